# Optimizing a Trainium2 kernel written in Bass

```python
import jax
import jax.numpy as jnp
from jax import lax
import numpy as np

D_MODEL = 1024
BATCH = 32
SEQ = 2048
DEPTH = 1

RW_HEAD_DIM = 64
RW_WIDTH = D_MODEL // 2
RW_HEADS = RW_WIDTH // RW_HEAD_DIM
RW_DECAY_RANK = 64
RW_ICLR_RANK = 64
RW_GATE_RANK = 128
RW_GN_EPS = 64e-5
RET_QK_DIM = 64
RET_QK_WIDTH = D_MODEL // 2
RET_HEADS = RET_QK_WIDTH // RET_QK_DIM
RET_V_DIM = 2 * RET_QK_DIM
RET_V_WIDTH = RET_HEADS * RET_V_DIM
RET_CHUNK = 128
ROPE_BASE = 10000.0
RET_GN_EPS = 1e-5
D_FF = 2816
CONV_WIDTH = 3
RMS_EPS = 1e-6

RW_SIZES = (RW_WIDTH, RW_WIDTH, RW_WIDTH, RW_DECAY_RANK, RW_ICLR_RANK, RW_GATE_RANK)
RET_SIZES = (RET_QK_WIDTH, RET_QK_WIDTH, RET_V_WIDTH, RET_V_WIDTH)
RW_COLS = sum(RW_SIZES)
RET_COLS = sum(RET_SIZES)
IN_SIZES = (RW_COLS, RET_COLS, D_MODEL, D_MODEL)
IN_COLS = sum(IN_SIZES)

kernel_name = "hybrid_rwkv7_retention_convffn_block"


def _split(z, sizes):
    return jnp.split(z, [int(s) for s in np.cumsum(sizes)[:-1]], axis=-1)


def rms_norm(x, g):
    xf = x.astype(jnp.float32)
    y = xf * lax.rsqrt(jnp.mean(xf * xf, axis=-1, keepdims=True) + RMS_EPS)
    return (y * g.astype(jnp.float32)).astype(x.dtype)


def group_norm_heads(z, eps):
    mu = jnp.mean(z, axis=-1, keepdims=True)
    var = jnp.mean(jnp.square(z - mu), axis=-1, keepdims=True)
    return (z - mu) * lax.rsqrt(var + eps)


def token_shift(p):
    return jnp.pad(p, ((0, 0), (1, 0), (0, 0)))[:, :-1]


def rwkv7_mixer(p, mu, w0, w2, a0, a2, g2, k_k, k_a, r_k, lnx_w, lnx_b):
    B, T, _ = p.shape
    H, N = RW_HEADS, RW_HEAD_DIM
    p = p.astype(jnp.float32)
    p = p + (token_shift(p) - p) * mu.astype(jnp.float32)
    r, k, v, wd, ad, gd = _split(p, RW_SIZES)
    w = -jax.nn.softplus(-(w0 + jnp.tanh(wd) @ w2)) - 0.5
    decay = jnp.exp(-jnp.exp(w))
    a = jax.nn.sigmoid(a0 + ad @ a2)
    g = jax.nn.sigmoid(gd) @ g2
    heads = lambda z: z.reshape(B, T, H, N).astype(jnp.float32)
    kk = heads(k * k_k)
    kk = kk / jnp.maximum(jnp.sqrt(jnp.sum(kk * kk, axis=-1, keepdims=True)), 1e-12)
    k = k * (1.0 + (a - 1.0) * k_a)
    r, k, v, decay, a = heads(r), heads(k), heads(v), heads(decay), heads(a)

    def step(S, inp):
        r_t, k_t, v_t, w_t, kk_t, a_t = inp
        sa = jnp.einsum('bhvk,bhk->bhv', S, -kk_t)
        S = (S * w_t[:, :, None, :]
             + sa[..., None] * (kk_t * a_t)[:, :, None, :]
             + v_t[..., None] * k_t[:, :, None, :])
        y = jnp.einsum('bhvk,bhk->bhv', S, r_t)
        return S, y

    xs = tuple(jnp.moveaxis(z, 1, 0) for z in (r, k, v, decay, kk, a))
    S0 = jnp.zeros((B, H, N, N), jnp.float32)
    _, y = lax.scan(step, S0, xs)
    y = jnp.moveaxis(y, 0, 1)
    y = group_norm_heads(y, RW_GN_EPS) * lnx_w.reshape(H, N) + lnx_b.reshape(H, N)
    bonus = jnp.sum(r * k * r_k.reshape(H, N), axis=-1, keepdims=True) * v
    return (y + bonus).reshape(B, T, RW_WIDTH) * g


def rotary(z, pos):
    half = z.shape[-1] // 2
    inv = ROPE_BASE ** (-jnp.arange(half, dtype=jnp.float32) / half)
    ang = pos.astype(jnp.float32)[..., None] * inv
    cos = jnp.cos(ang)[:, :, None, :]
    sin = jnp.sin(ang)[:, :, None, :]
    z1, z2 = z[..., :half], z[..., half:]
    return jnp.concatenate([z1 * cos - z2 * sin, z1 * sin + z2 * cos], axis=-1)


def retention_mixer(p, positions):
    B, T, _ = p.shape
    H, dk, dv, C = RET_HEADS, RET_QK_DIM, RET_V_DIM, RET_CHUNK
    q, k, v, g = _split(p.astype(jnp.float32), RET_SIZES)
    q = rotary(q.reshape(B, T, H, dk), positions)
    k = rotary(k.reshape(B, T, H, dk), positions) * (dk ** -0.5)
    v = v.reshape(B, T, H, dv)
    log_gamma = jnp.log1p(-jnp.exp2(-5.0 - jnp.arange(H, dtype=jnp.float32)))
    idx = jnp.arange(C, dtype=jnp.float32)
    rel = idx[:, None] - idx[None, :]
    decay_mask = jnp.where(rel[None] >= 0,
                           jnp.exp(jnp.maximum(rel, 0.0)[None] * log_gamma[:, None, None]), 0.0)
    q_decay = jnp.exp((idx + 1.0)[:, None] * log_gamma[None, :])[None, :, :, None]
    k_decay = jnp.exp((C - 1.0 - idx)[:, None] * log_gamma[None, :])[None, :, :, None]
    chunk_decay = jnp.exp(C * log_gamma)[None, :, None, None]
    n_chunks = T // C
    chunks = lambda z: jnp.moveaxis(z.reshape(B, n_chunks, C, H, z.shape[-1]), 1, 0)

    def step(R, inp):
        qc, kc, vc = inp
        s = jnp.einsum('bihd,bjhd->bhij', qc, kc) * decay_mask
        inner = jnp.einsum('bhij,bjhe->bihe', s, vc)
        cross = jnp.einsum('bihd,bhde->bihe', qc * q_decay, R)
        R = R * chunk_decay + jnp.einsum('bjhd,bjhe->bhde', kc * k_decay, vc)
        return R, inner + cross

    R0 = jnp.zeros((B, H, dk, dv), jnp.float32)
    _, o = lax.scan(step, R0, (chunks(q), chunks(k), chunks(v)))
    o = jnp.moveaxis(o, 0, 1).reshape(B, T, H, dv)
    o = group_norm_heads(o, RET_GN_EPS).reshape(B, T, RET_V_WIDTH)
    return jax.nn.silu(g) * o


def conv_ffn(h, w_up, conv_w, conv_b, w_down):
    u = h @ w_up
    T = u.shape[1]
    up = jnp.pad(u, ((0, 0), (CONV_WIDTH - 1, 0), (0, 0)))
    u = sum(up[:, i:i + T] * conv_w[i] for i in range(CONV_WIDTH)) + conv_b
    gate, val = jnp.split(u, 2, axis=-1)
    return (jax.nn.gelu(gate, approximate=True) * val) @ w_down


def setup_inputs(seed: int = 0) -> dict:
    key = jax.random.key(seed)
    ks = jax.random.split(key, 26)
    f32 = jnp.float32
    L = DEPTH
    nrm = lambda k, shape, s: s * jax.random.normal(k, shape, f32)
    x = jax.random.normal(ks[0], (BATCH, SEQ, D_MODEL), f32)
    offsets = jax.random.randint(ks[1], (BATCH, 1), 0, 4096, dtype=jnp.int32)
    positions = offsets + jnp.arange(SEQ, dtype=jnp.int32)[None, :]
    return {
        "x": x,
        "positions": positions,
        "norm_mix_pre": 1.0 + nrm(ks[2], (L, D_MODEL), 0.1),
        "norm_mix_post": 1.0 + nrm(ks[3], (L, D_MODEL), 0.1),
        "norm_ffn_pre": 1.0 + nrm(ks[4], (L, D_MODEL), 0.1),
        "norm_ffn_post": 1.0 + nrm(ks[5], (L, D_MODEL), 0.1),
        "w_in": nrm(ks[6], (L, D_MODEL, IN_COLS), D_MODEL ** -0.5),
        "rw_mu": jax.random.uniform(ks[7], (L, RW_COLS), f32, 0.0, 1.0),
        "rw_w0": jax.random.uniform(ks[8], (L, RW_WIDTH), f32, -6.0, 1.0),
        "rw_w2": nrm(ks[9], (L, RW_DECAY_RANK, RW_WIDTH), 0.1 * RW_DECAY_RANK ** -0.5),
        "rw_a0": nrm(ks[10], (L, RW_WIDTH), 0.1),
        "rw_a2": nrm(ks[11], (L, RW_ICLR_RANK, RW_WIDTH), 0.5 * RW_ICLR_RANK ** -0.5),
        "rw_g2": nrm(ks[12], (L, RW_GATE_RANK, RW_WIDTH), RW_GATE_RANK ** -0.5),
        "rw_k_k": 0.85 + nrm(ks[13], (L, RW_WIDTH), 0.1),
        "rw_k_a": 1.0 + nrm(ks[14], (L, RW_WIDTH), 0.1),
        "rw_r_k": nrm(ks[15], (L, RW_WIDTH), 0.1),
        "rw_lnx_w": 1.0 + nrm(ks[16], (L, RW_WIDTH), 0.1),
        "rw_lnx_b": nrm(ks[17], (L, RW_WIDTH), 0.01),
        "w_branch_rw": nrm(ks[18], (L, RW_WIDTH, D_MODEL), RW_WIDTH ** -0.5),
        "w_branch_ret": nrm(ks[19], (L, RET_V_WIDTH, D_MODEL), RET_V_WIDTH ** -0.5),
        "w_out": nrm(ks[20], (L, D_MODEL, D_MODEL), D_MODEL ** -0.5),
        "ffn_w_up": nrm(ks[21], (L, D_MODEL, 2 * D_FF), D_MODEL ** -0.5),
        "ffn_conv_w": nrm(ks[22], (L, CONV_WIDTH, 2 * D_FF), CONV_WIDTH ** -0.5),
        "ffn_conv_b": nrm(ks[23], (L, 2 * D_FF), 0.01),
        "ffn_w_down": nrm(ks[24], (L, D_FF, D_MODEL), D_FF ** -0.5),
    }


def reference(x, positions, norm_mix_pre, norm_mix_post, norm_ffn_pre, norm_ffn_post, w_in,
              rw_mu, rw_w0, rw_w2, rw_a0, rw_a2, rw_g2, rw_k_k, rw_k_a, rw_r_k, rw_lnx_w, rw_lnx_b,
              w_branch_rw, w_branch_ret, w_out, ffn_w_up, ffn_conv_w, ffn_conv_b, ffn_w_down):
    h = x
    for l in range(DEPTH):
        hn = rms_norm(h, norm_mix_pre[l])
        p = hn @ w_in[l]
        p_rw, p_ret, g_rw, g_ret = _split(p, IN_SIZES)
        y_rw = rwkv7_mixer(p_rw, rw_mu[l], rw_w0[l], rw_w2[l], rw_a0[l], rw_a2[l], rw_g2[l],
                           rw_k_k[l], rw_k_a[l], rw_r_k[l], rw_lnx_w[l], rw_lnx_b[l])
        y_ret = retention_mixer(p_ret, positions)
        merged = (jax.nn.sigmoid(g_rw.astype(jnp.float32)) * (y_rw @ w_branch_rw[l])
                  + jax.nn.sigmoid(g_ret.astype(jnp.float32)) * (y_ret @ w_branch_ret[l]))
        h = h + rms_norm(merged @ w_out[l], norm_mix_post[l])
        f = conv_ffn(rms_norm(h, norm_ffn_pre[l]), ffn_w_up[l], ffn_conv_w[l], ffn_conv_b[l], ffn_w_down[l])
        h = h + rms_norm(f, norm_ffn_post[l])
    return h.astype(x.dtype)
```

```python
import math
import numpy as np
from contextlib import ExitStack
import concourse.bass as bass
import concourse.mybir as mybir
from concourse.bass_utils import run_bass_kernel_spmd

F32 = mybir.dt.float32; BF16 = mybir.dt.bfloat16; I32 = mybir.dt.int32
AF = mybir.ActivationFunctionType; ALU = mybir.AluOpType; AX = mybir.AxisListType

D = 1024; RWC = 1792; INC = 6912; DFF = 2816
C0 = math.exp(-0.5)
G1, MU, W0, A0, KK, KA, RK, LW, LB, INV, SSC, CD, G3, CB, CW, QD, KDEC, NCONST = (
    0, 8, 22, 26, 30, 34, 38, 42, 46, 50, 51, 52, 56, 64, 108, 240, 752, 760)
T_MP, T_M0, T_MT, T_BM, T_ID, T_SW, NTAB = 0, 512, 640, 1664, 1792, 1920, 2048


class Buf:
    def __init__(self, t, name):
        self.t = t; self.name = name; self.writers = {}; self.readers = {}

    def __getitem__(self, k):
        return self.t[k]


class Sched:
    ENG = ['pe', 'act', 'dve', 'pool', 'sp']

    def __init__(self, nc, stack):
        self.nc = nc; self.stack = stack; self.semh = {}
        for e in self.ENG:
            self.semh[e] = stack.enter_context(nc.semaphore("s_" + e))
        self.cnt = {e: 0 for e in self.ENG}
        self.known = {e: {} for e in self.ENG}
        self.prog = {e: [] for e in self.ENG}
        self.dcnt = {}
        self.nbuf = 0

    def sb(self, st, shape, dt):
        self.nbuf += 1
        name = f"b{self.nbuf}"
        return Buf(st.enter_context(self.nc.sbuf_tensor(name, list(shape), dt)), name)

    def ps(self, st, shape, dt):
        self.nbuf += 1
        name = f"p{self.nbuf}"
        b = Buf(st.enter_context(self.nc.psum_tensor(name, list(shape), dt)), name)
        b.psum = True
        return b

    def _waits(self, eng, reads, writes):
        need = {}

        def add(k, v, raw):
            if k == eng and not raw and eng == 'pe':
                return
            if need.get(k, 0) < v:
                need[k] = v
        for b in reads:
            for k, v in b.writers.items():
                add(k, v, True)
        for b in writes:
            for k, v in b.writers.items():
                add(k, v, False)
            for k, v in b.readers.items():
                add(k, v, False)
        out = []
        for k, v in need.items():
            if k.startswith('d_'):
                v = self.dcnt[k]
            if self.known[eng].get(k, 0) >= v:
                continue
            self.known[eng][k] = v
            out.append((self.semh[k], v))
        return out

    def op(self, eng, fn, reads=(), writes=()):
        pr = [b for b in reads if getattr(b, 'psum', False)]
        if pr:
            writes = list(writes) + [b for b in pr if b not in writes]
            reads = [b for b in reads if not getattr(b, 'psum', False)]
        waits = self._waits(eng, reads, writes)
        self.cnt[eng] += 1
        n = self.cnt[eng]
        sem = self.semh[eng]

        def run(e, waits=waits, fn=fn, sem=sem):
            for s, v in waits:
                e.wait_ge(s, v)
            fn(e).then_inc(sem, 1)
        self.prog[eng].append(run)
        for b in reads:
            if b.readers.get(eng, 0) < n:
                b.readers[eng] = n
        for b in writes:
            b.writers = {eng: n}; b.readers = {}

    def dma(self, q, fn, semname, reads=(), writes=()):
        key = 'd_' + semname
        if key not in self.semh:
            self.semh[key] = self.stack.enter_context(self.nc.semaphore(key))
            self.dcnt[key] = 0
        waits = self._waits(q, reads, writes)
        self.dcnt[key] += 16
        n = self.dcnt[key]
        sem = self.semh[key]

        def run(e, waits=waits, fn=fn, sem=sem):
            for s, v in waits:
                e.wait_ge(s, v)
            fn(e).then_inc(sem, 16)
        self.prog[q].append(run)
        for b in reads:
            if b.readers.get(key, 0) < n:
                b.readers[key] = n
        for b in writes:
            b.writers = dict(b.writers); b.writers[key] = n
            b.readers = {}

    def barrier(self):
        allc = {}
        for e in self.ENG:
            if self.cnt[e] > 0:
                allc[e] = self.cnt[e]
        for k, v in self.dcnt.items():
            if v > 0:
                allc[k] = v
        for e in self.ENG:
            waits = []
            for k, v in allc.items():
                if self.known[e].get(k, 0) >= v:
                    continue
                self.known[e][k] = v
                waits.append((self.semh[k], v))

            def run(en, waits=waits):
                for s, v in waits:
                    en.wait_ge(s, v)
            self.prog[e].append(run)

    def emit(self):
        nc = self.nc
        with nc.Block() as block:
            @block.tensor
            def _(e):
                for f in self.prog['pe']:
                    f(e)

            @block.scalar
            def _(e):
                for f in self.prog['act']:
                    f(e)

            @block.vector
            def _(e):
                for f in self.prog['dve']:
                    f(e)

            @block.gpsimd
            def _(e):
                for f in self.prog['pool']:
                    f(e)

            @block.sync
            def _(e):
                for f in self.prog['sp']:
                    f(e)


def build(NSEQ, NCH, dbg=False, phases="ABC"):
    T = NCH * 128; NTOK = NSEQ * T; NCHUNK = NSEQ * NCH
    FT = min(256, T); NSUB = FT // 128; NTILE = T // FT
    nc = bass.Bass("TRN2", target_bir_lowering=False)

    def dr(name, shape, dt, kind="ExternalInput"):
        return nc.dram_tensor(name, list(shape), dt, kind=kind).ap()
    x_d = dr("x", [NTOK, D], F32)
    pos_d = dr("pos", [1, NTOK], I32)
    win_d = dr("w_in", [D, INC], F32)
    wbrw_d = dr("wbrw", [512, D], F32)
    wbret_d = dr("wbret", [D, D], F32)
    wout_d = dr("wout", [D, D], F32)
    wup_d = dr("wup", [D, 2 * DFF], F32)
    wdn_d = dr("wdn", [DFF, D], F32)
    lr_d = dr("lr", [128, 1024], F32)
    c_d = dr("c128", [128, NCONST], F32)
    rows_d = dr("rows", [2, D], F32)
    tab_d = dr("tab", [128, NTAB], F32)
    out_d = dr("out", [NTOK, D], F32, kind="ExternalOutput")
    sk = "ExternalOutput" if dbg else "Internal"
    hnT_s = dr("hnT_s", [NCHUNK, 128, 1024], BF16, kind=sk)
    yrw_s = dr("yrw_s", [NCHUNK, 128, 512], BF16, kind=sk)
    h_s = dr("h_s", [NTOK, D], F32, kind=sk)

    with ExitStack() as st0:
        S = Sched(nc, st0)
        banks = [S.ps(st0, [128, 512], F32) for _ in range(8)]
        bstate = [0]

        def nb():
            b = banks[bstate[0] % 8]; bstate[0] += 1
            return b

        def v3(b, a):
            return b.t[:, :].rearrange("p (a b) -> p a b", a=a)

        def vbf(b):
            return b.t[:, :].bitcast(BF16)

        cst = S.sb(st0, [128, NCONST], F32)
        tabf = S.sb(st0, [128, NTAB], F32)
        tabb = S.sb(st0, [128, NTAB], BF16)
        rows = S.sb(st0, [128, 2, D], F32)
        ones = S.sb(st0, [128, 128], F32)
        omka = S.sb(st0, [128, 4], F32)
        S.dma('sp', lambda e: e.dma_start(out=cst[:, :], in_=c_d), 'cst', writes=[cst])
        S.dma('sp', lambda e: e.dma_start(out=tabf[:, :], in_=tab_d), 'tabf', writes=[tabf])
        S.dma('pool', lambda e: e.dma_start(out=tabb[:, :], in_=tab_d), 'tabb', writes=[tabb])
        for r in range(2):
            S.dma('sp', lambda e, r=r: e.dma_start(out=rows[:, r, :], in_=rows_d[r:r + 1, :].partition_broadcast(128)),
                  'rows', writes=[rows])
        S.op('pool', lambda e: e.memset(ones[:, :], 1.0), writes=[ones])
        S.op('dve', lambda e: e.tensor_scalar(out=omka[:, :], in0=cst[:, KA:KA + 4], scalar1=-1.0, scalar2=1.0,
                                              op0=ALU.mult, op1=ALU.add), reads=[cst], writes=[omka])
        ident = tabb[:, T_ID:T_ID + 128]
        bones = tabb[:, T_BM:T_BM + 128]
        pswap = tabb[:, T_SW:T_SW + 128]

        def cb(off, n, shape):
            return cst[:, off:off + n].unsqueeze(2).to_broadcast(shape)

        def rmsnorm_T(stp, xs, dstT, col0, gcol, tmp_bf, ss, rs, xn):
            S.op('act', lambda e: e.activation(out=tmp_bf[:, :], in_=xs[:, :], func=AF.Square, accum_out=ss[:, 0:1]),
                 reads=[xs], writes=[tmp_bf, ss])
            S.op('act', lambda e: e.activation(out=rs[:, 0:1], in_=ss[:, 0:1], func=AF.Sqrt, scale=1.0 / D, bias=1e-6),
                 reads=[ss], writes=[rs])
            S.op('dve', lambda e: e.reciprocal(out=rs[:, 0:1], in_=rs[:, 0:1]), reads=[rs], writes=[rs])
            S.op('dve', lambda e: e.tensor_scalar(out=xn[:, :], in0=xs[:, :], scalar1=rs[:, 0:1], scalar2=None,
                                                  op0=ALU.mult), reads=[xs, rs], writes=[xn])
            bk = nb()
            for kc in range(8):
                S.op('pe', lambda e, kc=kc: e.transpose(out=vbf(bk)[:, kc * 128:(kc + 1) * 128],
                                                        in_=xn[:, kc * 128:(kc + 1) * 128], identity=ident),
                     reads=[xn, tabb], writes=[bk])
            S.op('dve', lambda e: e.tensor_tensor(
                out=dstT[:, :, col0:col0 + 128],
                in0=vbf(bk).rearrange("p (a b) -> p a b", a=8),
                in1=cb(gcol, 8, [128, 8, 128]), op=ALU.mult), reads=[bk, cst], writes=[dstT])

        if "A" in phases:
          with ExitStack() as st:
            w_rw = S.sb(st, [128, 8, RWC], BF16)
            lrb = S.sb(st, [128, 1024], BF16)
            for kc in range(8):
                S.dma('pool', lambda e, kc=kc: e.dma_start(out=w_rw[:, kc, :], in_=win_d[kc * 128:(kc + 1) * 128, 0:RWC]),
                      'wA', writes=[w_rw])
            S.dma('pool', lambda e: e.dma_start(out=lrb[:, :], in_=lr_d), 'wA', writes=[lrb])
            xs2 = [S.sb(st, [128, D], F32) for _ in range(2)]
            tmp_bf = S.sb(st, [128, D], BF16); xn = S.sb(st, [128, D], BF16)
            ss = S.sb(st, [128, 1], F32); rs = S.sb(st, [128, 1], F32)
            hnT = S.sb(st, [128, 8, 128], BF16)
            praw = S.sb(st, [128, 14, 129], F32)
            pl = S.sb(st, [128, 14, 128], F32)
            lr12 = S.sb(st, [128, 128], BF16); sg = S.sb(st, [128, 128], BF16)
            f = [S.sb(st, [128, 4, 128], F32) for _ in range(16)]
            (gT, sig, cs, csx, Epos, Eneg, Em, Ehat, al, kq, kkb, kp, bv, t0, t1, t2) = f
            sqb = S.sb(st, [128, 4, 128], BF16)
            SCin = S.sb(st, [128, 4, 2, 128], BF16)
            BtT = S.sb(st, [128, 4, 128], BF16); KtT = S.sb(st, [128, 4, 128], BF16)
            BhT = S.sb(st, [128, 4, 128], BF16); KhT = S.sb(st, [128, 4, 128], BF16)
            vTb = S.sb(st, [128, 4, 128], BF16)
            Vp = S.sb(st, [128, 4, 128], BF16); BH = S.sb(st, [128, 4, 128], BF16); KH = S.sb(st, [128, 4, 128], BF16)
            VZ = S.sb(st, [128, 4, 2, 128], BF16); AZ = S.sb(st, [128, 4, 2, 128], BF16); UVZ = S.sb(st, [128, 4, 2, 128], BF16)
            Ap = S.sb(st, [128, 4, 128], BF16); UVp = S.sb(st, [128, 4, 128], BF16)
            SCT = S.sb(st, [128, 4, 2, 512], BF16)
            Xs = S.sb(st, [128, 4, 2, 128], BF16)
            MN = S.sb(st, [128, 4, 2, 256], BF16)
            Xt = [Buf(Xs.t, 'Xt%d' % q_) for q_ in range(4)]
            MNt = [Buf(MN.t, 'MNt%d' % q_) for q_ in range(4)]
            RhT = S.sb(st, [128, 4, 128], BF16); GZ = S.sb(st, [128, 4, 128], BF16)
            HZ = S.sb(st, [128, 4, 128], BF16); Hf = S.sb(st, [128, 4, 128], F32)
            yb = S.sb(st, [128, 4, 128], BF16); ysq = S.sb(st, [128, 4, 128], BF16); rkb = S.sb(st, [128, 4, 128], BF16)
            yrwT = S.sb(st, [128, 4, 128], BF16)
            for z in (VZ, AZ, UVZ):
                S.op('pool', lambda e, z=z: e.memset(z[:, :, :, :], 0.0), writes=[z])
            bmf = tabf[:, T_BM:T_BM + 128].unsqueeze(1).to_broadcast([128, 4, 128])

            DBG.update({k_: v_.name for k_, v_ in list(locals().items()) if isinstance(v_, Buf)})

            def _chunk(ci):
                b_i, c_i = divmod(ci, NCH)
                tok0 = ci * 128
                xs = xs2[ci % 2]
                S.dma('sp', lambda e, xs=xs, tok0=tok0: e.dma_start(out=xs[:, :], in_=x_d[tok0:tok0 + 128, :]),
                      'xA%d' % (ci % 2), writes=[xs])
                if c_i == 0:
                    S.op('pool', lambda e: e.memset(praw[:, :, 0:1], 0.0), writes=[praw])
                    S.op('pool', lambda e: e.memset(Hf[:, :, :], 0.0), writes=[Hf])
                    S.op('pool', lambda e: e.memset(HZ[:, :, :], 0.0), writes=[HZ])
                rmsnorm_T(st, xs, hnT, 0, G1, tmp_bf, ss, rs, xn)
                S.dma('sp', lambda e, ci=ci: e.dma_start(out=hnT_s[ci].rearrange("p (a b) -> p a b", a=8), in_=hnT[:, :, :]),
                      'hst', reads=[hnT])
                if _stop(1):
                    return
                for g in range(4):
                    bk = nb(); n = 4 if g < 3 else 2
                    for jj in range(n):
                        j = g * 4 + jj
                        for kc in range(8):
                            S.op('pe', lambda e, bk=bk, jj=jj, j=j, kc=kc: e.matmul(
                                bk[:, jj * 128:(jj + 1) * 128], lhsT=w_rw[:, kc, j * 128:(j + 1) * 128], rhs=hnT[:, kc, :],
                                start=(kc == 0), stop=(kc == 7)), reads=[w_rw, hnT], writes=[bk])
                    S.op('act' if g % 2 == 0 else 'dve',
                         (lambda e, bk=bk, g=g, n=n: e.copy(out=praw[:, g * 4:g * 4 + n, 1:129], in_=v3(bk, 4)[:, 0:n, :])) if g % 2 == 0 else
                         (lambda e, bk=bk, g=g, n=n: e.tensor_copy(out=praw[:, g * 4:g * 4 + n, 1:129], in_=v3(bk, 4)[:, 0:n, :])),
                         reads=[bk], writes=[praw])
                S.op('dve', lambda e: e.tensor_tensor(out=pl[:, :, :], in0=praw[:, :, 0:128], in1=praw[:, :, 1:129],
                                                      op=ALU.subtract), reads=[praw], writes=[pl])
                S.op('pool', lambda e: e.tensor_tensor(out=pl[:, :, :], in0=pl[:, :, :], in1=cb(MU, 14, [128, 14, 128]),
                                                       op=ALU.mult), reads=[pl, cst], writes=[pl])
                S.op('dve', lambda e: e.tensor_tensor(out=pl[:, :, :], in0=pl[:, :, :], in1=praw[:, :, 1:129],
                                                      op=ALU.add), reads=[pl, praw], writes=[pl])
                S.op('act', lambda e: e.copy(out=praw[:, :, 0:1], in_=praw[:, :, 128:129]), reads=[praw], writes=[praw])
                if _stop(2):
                    return
                S.op('act', lambda e: e.activation(out=lr12[0:64, :], in_=pl[0:64, 12, :], func=AF.Tanh), reads=[pl], writes=[lr12])
                S.op('act', lambda e: e.copy(out=lr12[64:128, :], in_=pl[64:128, 12, :]), reads=[pl], writes=[lr12])
                S.op('act', lambda e: e.activation(out=sg[:, :], in_=pl[:, 13, :], func=AF.Sigmoid), reads=[pl], writes=[sg])
                bw = nb(); ba = nb(); bg = nb()
                for jc in range(4):
                    S.op('pe', lambda e, jc=jc: e.matmul(bw[:, jc * 128:(jc + 1) * 128], lhsT=lrb[0:64, jc * 128:(jc + 1) * 128],
                                                         rhs=lr12[0:64, :], start=True, stop=True), reads=[lrb, lr12], writes=[bw])
                for jc in range(4):
                    S.op('pe', lambda e, jc=jc: e.matmul(ba[:, jc * 128:(jc + 1) * 128], lhsT=lrb[64:128, jc * 128:(jc + 1) * 128],
                                                         rhs=lr12[64:128, :], start=True, stop=True), reads=[lrb, lr12], writes=[ba])
                for jc in range(4):
                    S.op('pe', lambda e, jc=jc: e.matmul(bg[:, jc * 128:(jc + 1) * 128], lhsT=lrb[:, 512 + jc * 128:512 + (jc + 1) * 128],
                                                         rhs=sg[:, :], start=True, stop=True), reads=[lrb, sg], writes=[bg])
                if _stop(3):
                    return
                S.op('dve', lambda e: e.tensor_tensor(out=t0[:, :, :], in0=v3(bw, 4), in1=cb(W0, 4, [128, 4, 128]), op=ALU.add),
                     reads=[bw, cst], writes=[t0])
                S.op('act', lambda e: e.activation(out=sig[:, :, :], in_=t0[:, :, :], func=AF.Sigmoid), reads=[t0], writes=[sig])
                S.op('dve', lambda e: e.tensor_tensor(out=t2[:, :, :], in0=v3(ba, 4), in1=cb(A0, 4, [128, 4, 128]), op=ALU.add),
                     reads=[ba, cst], writes=[t2])
                S.op('act', lambda e: e.activation(out=al[:, :, :], in_=t2[:, :, :], func=AF.Sigmoid), reads=[t2], writes=[al])
                S.op('act', lambda e: e.copy(out=gT[:, :, :], in_=v3(bg, 4)), reads=[bg], writes=[gT])
                for jc in range(4):
                    S.op('dve', lambda e, jc=jc: e.tensor_tensor_scan(out=cs[:, jc, :], data0=ones[:, :], data1=sig[:, jc, :],
                                                                      initial=0.0, op0=ALU.mult, op1=ALU.add),
                         reads=[ones, sig], writes=[cs])
                S.op('pool', lambda e: e.tensor_tensor(out=csx[:, :, :], in0=cs[:, :, :], in1=sig[:, :, :], op=ALU.subtract),
                     reads=[cs, sig], writes=[csx])
                S.op('act', lambda e: e.activation(out=Epos[:, :, :], in_=cs[:, :, :], func=AF.Exp, scale=-C0), reads=[cs], writes=[Epos])
                S.op('act', lambda e: e.activation(out=Eneg[:, :, :], in_=cs[:, :, :], func=AF.Exp, scale=C0), reads=[cs], writes=[Eneg])
                S.op('act', lambda e: e.activation(out=Em[:, :, :], in_=csx[:, :, :], func=AF.Exp, scale=-C0), reads=[csx], writes=[Em])
                S.op('dve', lambda e: e.tensor_tensor(out=t1[:, :, :], in0=cs[:, :, 127:128].to_broadcast([128, 4, 128]),
                                                      in1=cs[:, :, :], op=ALU.subtract), reads=[cs], writes=[t1])
                S.op('act', lambda e: e.activation(out=Ehat[:, :, :], in_=t1[:, :, :], func=AF.Exp, scale=-C0), reads=[t1], writes=[Ehat])
                S.op('pool', lambda e: e.tensor_tensor(out=kq[:, :, :], in0=pl[:, 4:8, :], in1=cb(KK, 4, [128, 4, 128]), op=ALU.mult),
                     reads=[pl, cst], writes=[kq])
                S.op('pool', lambda e: e.tensor_tensor(out=sqb[:, :, :], in0=kq[:, :, :], in1=kq[:, :, :], op=ALU.mult),
                     reads=[kq], writes=[sqb])
                bs = nb()
                for jc in range(4):
                    S.op('pe', lambda e, jc=jc: e.matmul(bs[:, jc * 128:(jc + 1) * 128], lhsT=bones, rhs=sqb[:, jc, :],
                                                         start=True, stop=True), reads=[tabb, sqb], writes=[bs])
                S.op('dve', lambda e: e.tensor_scalar(out=t0[:, :, :], in0=v3(bs, 4), scalar1=1e-24, scalar2=None, op0=ALU.max),
                     reads=[bs], writes=[t0])
                S.op('act', lambda e: e.activation(out=t0[:, :, :], in_=t0[:, :, :], func=AF.Sqrt), reads=[t0], writes=[t0])
                S.op('dve', lambda e: e.reciprocal(out=t0[:, :, :], in_=t0[:, :, :]), reads=[t0], writes=[t0])
                S.op('dve', lambda e: e.tensor_tensor(out=kkb[:, :, :], in0=kq[:, :, :], in1=t0[:, :, :], op=ALU.mult),
                     reads=[kq, t0], writes=[kkb])
                S.op('pool', lambda e: e.tensor_tensor(out=t2[:, :, :], in0=al[:, :, :], in1=cb(KA, 4, [128, 4, 128]), op=ALU.mult),
                     reads=[al, cst], writes=[t2])
                S.op('pool', lambda e: e.tensor_tensor(out=t2[:, :, :], in0=t2[:, :, :],
                                                       in1=omka[:, :].unsqueeze(2).to_broadcast([128, 4, 128]), op=ALU.add),
                     reads=[t2, omka], writes=[t2])
                S.op('dve', lambda e: e.tensor_tensor(out=kp[:, :, :], in0=pl[:, 4:8, :], in1=t2[:, :, :], op=ALU.mult),
                     reads=[pl, t2], writes=[kp])
                if _stop(4):
                    return
                S.op('pool', lambda e: e.tensor_tensor(out=t1[:, :, :], in0=kkb[:, :, :], in1=Em[:, :, :], op=ALU.mult),
                     reads=[kkb, Em], writes=[t1])
                S.op('act', lambda e: e.activation(out=SCin[:, :, 0, :], in_=t1[:, :, :], func=AF.Identity, scale=-1.0),
                     reads=[t1], writes=[SCin])
                S.op('pool', lambda e: e.tensor_tensor(out=bv[:, :, :], in0=kkb[:, :, :], in1=al[:, :, :], op=ALU.mult),
                     reads=[kkb, al], writes=[bv])
                S.op('dve', lambda e: e.tensor_tensor(out=BtT[:, :, :], in0=bv[:, :, :], in1=Eneg[:, :, :], op=ALU.mult),
                     reads=[bv, Eneg], writes=[BtT])
                S.op('pool', lambda e: e.tensor_tensor(out=KtT[:, :, :], in0=kp[:, :, :], in1=Eneg[:, :, :], op=ALU.mult),
                     reads=[kp, Eneg], writes=[KtT])
                S.op('dve', lambda e: e.tensor_tensor(out=SCin[:, :, 1, :], in0=pl[:, 0:4, :], in1=Epos[:, :, :], op=ALU.mult),
                     reads=[pl, Epos], writes=[SCin])
                S.op('pool', lambda e: e.tensor_tensor(out=BhT[:, :, :], in0=bv[:, :, :], in1=Ehat[:, :, :], op=ALU.mult),
                     reads=[bv, Ehat], writes=[BhT])
                S.op('dve', lambda e: e.tensor_tensor(out=KhT[:, :, :], in0=kp[:, :, :], in1=Ehat[:, :, :], op=ALU.mult),
                     reads=[kp, Ehat], writes=[KhT])
                S.op('act', lambda e: e.copy(out=vTb[:, :, :], in_=pl[:, 8:12, :]), reads=[pl], writes=[vTb])
                if _stop(4.3):
                    return
                bt1 = nb(); bt2 = nb()
                for jc in range(4):
                    S.op('pe', lambda e, jc=jc: e.transpose(out=vbf(bt1)[:, jc * 128:(jc + 1) * 128], in_=SCin[:, jc, 0, :], identity=ident),
                         reads=[SCin, tabb], writes=[bt1])
                    S.op('pe', lambda e, jc=jc: e.transpose(out=vbf(bt1)[:, 512 + jc * 128:512 + (jc + 1) * 128], in_=vTb[:, jc, :], identity=ident),
                         reads=[vTb, tabb], writes=[bt1])
                    S.op('pe', lambda e, jc=jc: e.transpose(out=vbf(bt2)[:, jc * 128:(jc + 1) * 128], in_=BhT[:, jc, :], identity=ident),
                         reads=[BhT, tabb], writes=[bt2])
                    S.op('pe', lambda e, jc=jc: e.transpose(out=vbf(bt2)[:, 512 + jc * 128:512 + (jc + 1) * 128], in_=KhT[:, jc, :], identity=ident),
                         reads=[KhT, tabb], writes=[bt2])
                if _stop(4.6):
                    return
                b1v = vbf(bt1).rearrange("p (k a h c) -> p k a h c", k=2, a=4, h=2)
                S.op('act', lambda e: e.copy(out=Xs[:, :, :, 0:64], in_=b1v[:, 0, :, :, :]), reads=[bt1], writes=Xt)
                if _stop(4.7):
                    return
                S.op('dve', lambda e: e.tensor_copy(out=Vp[:, :, :], in_=vbf(bt1)[:, 512:1024].rearrange("p (a b) -> p a b", a=4)),
                     reads=[bt1], writes=[Vp])
                if _stop(4.8):
                    return
                for hp in range(2):
                    S.op('act', lambda e, hp=hp: e.copy(out=VZ[:, :, hp, hp * 64:(hp + 1) * 64], in_=b1v[:, 1, :, hp, :]),
                         reads=[bt1], writes=[VZ])
                if _stop(4.85):
                    return
                S.op('dve', lambda e: e.tensor_copy(out=BH[:, :, :], in_=vbf(bt2)[:, 0:512].rearrange("p (a b) -> p a b", a=4)),
                     reads=[bt2], writes=[BH])
                if _stop(4.9):
                    return
                S.op('act', lambda e: e.copy(out=KH[:, :, :], in_=vbf(bt2)[:, 512:1024].rearrange("p (a b) -> p a b", a=4)),
                     reads=[bt2], writes=[KH])
                if _stop(5):
                    return
                b3 = [None, None]
                for jc in range(4):
                    for hp in range(2):
                        pb = 64 * hp; h = 2 * jc + hp
                        if h % 4 == 0:
                            b3 = [nb(), nb()]
                        bk = nb()
                        rhs2 = SCin[pb:pb + 64, jc, :, :].rearrange("p a b -> p (a b)")
                        S.op('pe', lambda e, bk=bk, jc=jc, pb=pb, rhs2=rhs2: e.matmul(bk[:, 0:256], lhsT=BtT[pb:pb + 64, jc, :], rhs=rhs2,
                                                                                      start=True, stop=True), reads=[BtT, SCin], writes=[bk])
                        S.op('pe', lambda e, bk=bk, jc=jc, pb=pb, rhs2=rhs2: e.matmul(bk[:, 256:512], lhsT=KtT[pb:pb + 64, jc, :], rhs=rhs2,
                                                                                      start=True, stop=True), reads=[KtT, SCin], writes=[bk])
                        S.op('dve', lambda e, bk=bk, jc=jc, hp=hp: e.tensor_tensor(out=SCT[:, jc, hp, :], in0=bk[:, :], in1=tabf[:, T_MP:T_MP + 512],
                                                                                   op=ALU.mult), reads=[bk, tabf], writes=[SCT])
                        b3k = b3[hp]
                        S.op('pe', lambda e, b3k=b3k, jc=jc, pb=pb: e.matmul(b3k[:, (jc % 2) * 128:(jc % 2 + 1) * 128], lhsT=SCin[pb:pb + 64, jc, 0, :],
                                                                             rhs=BtT[pb:pb + 64, jc, :], start=True, stop=True),
                             reads=[SCin, BtT], writes=[b3k])
                        if h % 4 == 3:
                            for hq in range(2):
                                S.op('dve', lambda e, g=h // 4, hq=hq, b3g=b3[hq]: e.tensor_tensor(
                                    out=MN[:, 2 * g:2 * g + 2, hq, 0:128],
                                    in0=b3g.t[:, 0:256].rearrange("p (a c) -> p a c", a=2),
                                    in1=tabf[:, T_M0:T_M0 + 128].unsqueeze(1).to_broadcast([128, 2, 128]), op=ALU.mult),
                                    reads=[b3[hq], tabf], writes=[MNt[2 * (h // 4)], MNt[2 * (h // 4) + 1]])
                if _stop(6):
                    return
                for g in range(2):
                    bk = nb()
                    for hh in range(4):
                        jc = 2 * g + hh // 2; hp = hh % 2
                        S.op('pe', lambda e, bk=bk, hh=hh, jc=jc, hp=hp: e.matmul(bk[:, hh * 128:(hh + 1) * 128], lhsT=SCT[:, jc, hp, 256:384],
                                                                                  rhs=Vp[:, jc, :], start=True, stop=True), reads=[SCT, Vp], writes=[bk])
                    bvw = bk.t[:, :].rearrange("p (a h g c) -> p a h g c", a=2, h=2, g=2)
                    for hp in range(2):
                        S.op('act', lambda e, bvw=bvw, g=g, hp=hp: e.copy(out=Xs[:, 2 * g:2 * g + 2, hp, 64:128], in_=bvw[:, :, hp, hp, :]),
                             reads=[bk], writes=[Xt[2 * g], Xt[2 * g + 1]])
                if _stop(7):
                    return
                for j in range(7):
                    for jc in range(4):
                        bP = nb(); bQ = nb() if j < 6 else None
                        for hp in range(2):
                            Nj = SCT[:, jc, hp, 0:128] if j == 0 else MN[:, jc, hp, 128:256]
                            nsrc = [SCT] if j == 0 else []
                            S.op('pe', lambda e, bP=bP, hp=hp, jc=jc, Nj=Nj: e.matmul(bP[:, hp * 128:(hp + 1) * 128], lhsT=Nj, rhs=Xs[:, jc, hp, :],
                                                                                     start=True, stop=True), reads=nsrc + [Xt[jc], MNt[jc]], writes=[bP])
                            if j < 6:
                                S.op('pe', lambda e, bQ=bQ, hp=hp, jc=jc, Nj=Nj: e.matmul(bQ[:, hp * 256:hp * 256 + 128], lhsT=Nj, rhs=MN[:, jc, hp, 0:128],
                                                                                         start=True, stop=True), reads=nsrc + [MNt[jc]], writes=[bQ])
                                S.op('pe', lambda e, bQ=bQ, hp=hp, jc=jc, Nj=Nj: e.matmul(bQ[:, hp * 256 + 128:hp * 256 + 256], lhsT=MN[:, jc, hp, 0:128], rhs=Nj,
                                                                                         start=True, stop=True), reads=nsrc + [MNt[jc]], writes=[bQ])
                        S.op('dve', lambda e, jc=jc, bP=bP: e.tensor_tensor(out=Xs[:, jc, :, :], in0=Xs[:, jc, :, :],
                                                                            in1=bP.t[:, 0:256].rearrange("p (h c) -> p h c", h=2), op=ALU.add),
                             reads=[Xt[jc], bP], writes=[Xt[jc]])
                        if j < 6:
                            S.op('act', lambda e, jc=jc, bQ=bQ: e.copy(out=MN[:, jc, :, :], in_=bQ.t[:, :].rearrange("p (h c) -> p h c", h=2)),
                                 reads=[bQ], writes=[MNt[jc]])
                if _stop(8):
                    return
                for hp in range(2):
                    S.op('act', lambda e, hp=hp: e.copy(out=AZ[:, :, hp, hp * 64:(hp + 1) * 64], in_=Xs[:, :, hp, 0:64]), reads=Xt, writes=[AZ])
                    S.op('pool', lambda e, hp=hp: e.tensor_copy(out=UVZ[:, :, hp, hp * 64:(hp + 1) * 64], in_=Xs[:, :, hp, 64:128]), reads=Xt, writes=[UVZ])
                S.op('pool', lambda e: e.tensor_copy(out=Ap[:, :, :].rearrange("p a (h c) -> p a h c", h=2), in_=Xs[:, :, :, 0:64]), reads=Xt, writes=[Ap])
                S.op('dve', lambda e: e.tensor_copy(out=UVp[:, :, :].rearrange("p a (h c) -> p a h c", h=2), in_=Xs[:, :, :, 64:128]), reads=Xt, writes=[UVp])
                bR = nb(); bG = nb()
                for jc in range(4):
                    for hp in range(2):
                        S.op('pe', lambda e, jc=jc, hp=hp: e.matmul(bR[:, jc * 128:(jc + 1) * 128], lhsT=AZ[:, jc, hp, :], rhs=SCT[:, jc, hp, 128:256],
                                                                    start=(hp == 0), stop=(hp == 1)), reads=[AZ, SCT], writes=[bR])
                for jc in range(4):
                    S.op('pe', lambda e, jc=jc: e.matmul(bG[:, jc * 128:(jc + 1) * 128], lhsT=Ap[:, jc, :], rhs=BH[:, jc, :], start=True, stop=True),
                         reads=[Ap, BH], writes=[bG])
                S.op('dve', lambda e: e.tensor_tensor(out=RhT[:, :, :], in0=v3(bR, 4), in1=SCin[:, :, 1, :], op=ALU.add), reads=[bR, SCin], writes=[RhT])
                S.op('dve', lambda e: e.tensor_tensor(out=GZ[:, :, :], in0=v3(bG, 4), in1=bmf, op=ALU.mult), reads=[bG, tabf], writes=[GZ])
                if _stop(9):
                    return
                bY = nb(); bH = nb()
                for jc in range(4):
                    for hp in range(2):
                        S.op('pe', lambda e, jc=jc, hp=hp: e.matmul(bY[:, jc * 128:(jc + 1) * 128], lhsT=UVZ[:, jc, hp, :], rhs=SCT[:, jc, hp, 128:256],
                                                                    start=(hp == 0), stop=False), reads=[UVZ, SCT], writes=[bY])
                        S.op('pe', lambda e, jc=jc, hp=hp: e.matmul(bY[:, jc * 128:(jc + 1) * 128], lhsT=VZ[:, jc, hp, :], rhs=SCT[:, jc, hp, 384:512],
                                                                    start=False, stop=False), reads=[VZ, SCT], writes=[bY])
                    S.op('pe', lambda e, jc=jc: e.matmul(bY[:, jc * 128:(jc + 1) * 128], lhsT=HZ[:, jc, :], rhs=RhT[:, jc, :], start=False, stop=True),
                         reads=[HZ, RhT], writes=[bY])
                for jc in range(4):
                    S.op('pe', lambda e, jc=jc: e.matmul(bH[:, jc * 128:(jc + 1) * 128], lhsT=BH[:, jc, :], rhs=UVp[:, jc, :], start=True, stop=False),
                         reads=[BH, UVp], writes=[bH])
                    S.op('pe', lambda e, jc=jc: e.matmul(bH[:, jc * 128:(jc + 1) * 128], lhsT=KH[:, jc, :], rhs=Vp[:, jc, :], start=False, stop=False),
                         reads=[KH, Vp], writes=[bH])
                    S.op('pe', lambda e, jc=jc: e.matmul(bH[:, jc * 128:(jc + 1) * 128], lhsT=GZ[:, jc, :], rhs=HZ[:, jc, :], start=False, stop=True),
                         reads=[GZ, HZ], writes=[bH])
                for jc in range(4):
                    S.op('dve', lambda e, jc=jc: e.scalar_tensor_tensor(out=Hf[:, jc, :], in0=Hf[:, jc, :], scalar=Epos[:, jc, 127:128],
                                                                        in1=bH[:, jc * 128:(jc + 1) * 128], op0=ALU.mult, op1=ALU.add),
                         reads=[Hf, Epos, bH], writes=[Hf])
                S.op('pool', lambda e: e.tensor_tensor(out=Hf[:, :, :], in0=Hf[:, :, :], in1=bmf, op=ALU.mult), reads=[Hf, tabf], writes=[Hf])
                S.op('act', lambda e: e.copy(out=HZ[:, :, :], in_=Hf[:, :, :]), reads=[Hf], writes=[HZ])
                if _stop(10):
                    return
                S.op('act', lambda e: e.copy(out=yb[:, :, :], in_=v3(bY, 4)), reads=[bY], writes=[yb])
                S.op('act', lambda e: e.activation(out=ysq[:, :, :], in_=v3(bY, 4), func=AF.Square), reads=[bY], writes=[ysq])
                S.op('pool', lambda e: e.tensor_tensor(out=t0[:, :, :], in0=pl[:, 0:4, :], in1=kp[:, :, :], op=ALU.mult), reads=[pl, kp], writes=[t0])
                S.op('pool', lambda e: e.tensor_tensor(out=rkb[:, :, :], in0=t0[:, :, :], in1=cb(RK, 4, [128, 4, 128]), op=ALU.mult),
                     reads=[t0, cst], writes=[rkb])
                bM = nb(); bQ = nb(); bO = nb()
                for jc in range(4):
                    S.op('pe', lambda e, jc=jc: e.matmul(bM[:, jc * 128:(jc + 1) * 128], lhsT=bones, rhs=yb[:, jc, :], start=True, stop=True),
                         reads=[tabb, yb], writes=[bM])
                    S.op('pe', lambda e, jc=jc: e.matmul(bQ[:, jc * 128:(jc + 1) * 128], lhsT=bones, rhs=ysq[:, jc, :], start=True, stop=True),
                         reads=[tabb, ysq], writes=[bQ])
                    S.op('pe', lambda e, jc=jc: e.matmul(bO[:, jc * 128:(jc + 1) * 128], lhsT=bones, rhs=rkb[:, jc, :], start=True, stop=True),
                         reads=[tabb, rkb], writes=[bO])
                S.op('act', lambda e: e.activation(out=t1[:, :, :], in_=v3(bM, 4), func=AF.Identity, scale=1.0 / 64), reads=[bM], writes=[t1])
                S.op('pool', lambda e: e.tensor_tensor(out=t2[:, :, :], in0=t1[:, :, :], in1=t1[:, :, :], op=ALU.mult), reads=[t1], writes=[t2])
                S.op('dve', lambda e: e.scalar_tensor_tensor(out=t2[:, :, :], in0=v3(bQ, 4), scalar=1.0 / 64, in1=t2[:, :, :],
                                                             op0=ALU.mult, op1=ALU.subtract), reads=[bQ, t2], writes=[t2])
                S.op('act', lambda e: e.activation(out=t2[:, :, :], in_=t2[:, :, :], func=AF.Sqrt, bias=64e-5), reads=[t2], writes=[t2])
                S.op('dve', lambda e: e.reciprocal(out=t2[:, :, :], in_=t2[:, :, :]), reads=[t2], writes=[t2])
                S.op('dve', lambda e: e.tensor_tensor(out=t0[:, :, :], in0=v3(bY, 4), in1=t1[:, :, :], op=ALU.subtract), reads=[bY, t1], writes=[t0])
                S.op('pool', lambda e: e.tensor_tensor(out=t0[:, :, :], in0=t0[:, :, :], in1=t2[:, :, :], op=ALU.mult), reads=[t0, t2], writes=[t0])
                S.op('pool', lambda e: e.tensor_tensor(out=t0[:, :, :], in0=t0[:, :, :], in1=cb(LW, 4, [128, 4, 128]), op=ALU.mult),
                     reads=[t0, cst], writes=[t0])
                S.op('pool', lambda e: e.tensor_tensor(out=t0[:, :, :], in0=t0[:, :, :], in1=cb(LB, 4, [128, 4, 128]), op=ALU.add),
                     reads=[t0, cst], writes=[t0])
                S.op('dve', lambda e: e.tensor_tensor(out=t1[:, :, :], in0=v3(bO, 4), in1=pl[:, 8:12, :], op=ALU.mult), reads=[bO, pl], writes=[t1])
                S.op('pool', lambda e: e.tensor_tensor(out=t0[:, :, :], in0=t0[:, :, :], in1=t1[:, :, :], op=ALU.add), reads=[t0, t1], writes=[t0])
                S.op('dve', lambda e: e.tensor_tensor(out=yrwT[:, :, :], in0=t0[:, :, :], in1=gT[:, :, :], op=ALU.mult), reads=[t0, gT], writes=[yrwT])
                S.dma('sp', lambda e, ci=ci: e.dma_start(out=yrw_s[ci].rearrange("p (a b) -> p a b", a=4), in_=yrwT[:, :, :]), 'yst', reads=[yrwT])
            for ci in range(NCHUNK):
                _chunk(ci)
            S.barrier()

        if "B" in phases:
          with ExitStack() as st:
            NBC = INC - RWC
            w_b = S.sb(st, [128, 8, NBC], BF16)
            wbrw = S.sb(st, [128, 4, D], BF16); wbret = S.sb(st, [128, 8, D], BF16); wout = S.sb(st, [128, 8, D], BF16)
            for kc in range(8):
                S.dma('pool', lambda e, kc=kc: e.dma_start(out=w_b[:, kc, :], in_=win_d[kc * 128:(kc + 1) * 128, RWC:INC]), 'wB', writes=[w_b])
                S.dma('pool', lambda e, kc=kc: e.dma_start(out=wbret[:, kc, :], in_=wbret_d[kc * 128:(kc + 1) * 128, :]), 'wB', writes=[wbret])
                S.dma('pool', lambda e, kc=kc: e.dma_start(out=wout[:, kc, :], in_=wout_d[kc * 128:(kc + 1) * 128, :]), 'wB', writes=[wout])
            for kc in range(4):
                S.dma('pool', lambda e, kc=kc: e.dma_start(out=wbrw[:, kc, :], in_=wbrw_d[kc * 128:(kc + 1) * 128, :]), 'wB', writes=[wbrw])
            _xb = S.sb(st, [128, D], F32); xs2 = [_xb, _xb]
            hnT2 = [S.sb(st, [128, 8, 128], BF16) for _ in range(2)]
            yrw2 = [S.sb(st, [128, 4, 128], BF16) for _ in range(2)]
            posi2 = [S.sb(st, [128, 128], I32) for _ in range(2)]
            posf = S.sb(st, [128, 128], F32); u0 = S.sb(st, [128, 128], F32); u1 = S.sb(st, [128, 128], F32)
            ti = S.sb(st, [128, 128], I32); tf = S.sb(st, [128, 128], F32)
            cosT = S.sb(st, [128, 128], F32); sinT = S.sb(st, [128, 128], F32)
            qk = S.sb(st, [128, 8, 128], F32); qkb = S.sb(st, [128, 8, 128], BF16)
            r1 = S.sb(st, [128, 8, 128], F32); r2 = S.sb(st, [128, 8, 128], F32)
            rot = S.sb(st, [128, 8, 128], BF16); qd = S.sb(st, [128, 4, 128], BF16)
            v_bf = S.sb(st, [128, D], BF16); sgb = S.sb(st, [128, D], BF16); sA = S.sb(st, [128, D], BF16); sB = S.sb(st, [128, D], BF16)
            kdZ = S.sb(st, [128, 4, 2, 128], BF16)
            sT = S.sb(st, [128, 8, 128], BF16)
            Rf = S.sb(st, [128, 4, 128], F32); Rb = S.sb(st, [128, 4, 128], BF16)
            of = qk; osq = r1
            stt = S.sb(st, [128, 16], F32); mean = S.sb(st, [128, 8], F32); var = S.sb(st, [128, 8], F32)
            yret = S.sb(st, [128, D], BF16); yretT = S.sb(st, [128, 8, 128], BF16)
            m1 = S.sb(st, [128, D], F32); m2 = S.sb(st, [128, D], F32); mg = S.sb(st, [128, D], BF16); mT = S.sb(st, [128, 8, 128], BF16)
            mo = m2; junk = mg; ss = S.sb(st, [128, 1], F32); rs = S.sb(st, [128, 1], F32)
            hh = m1
            S.op('pool', lambda e: e.memset(kdZ[:, :, :, :], 0.0), writes=[kdZ])
            DBG.update({k_: v_.name for k_, v_ in list(locals().items()) if isinstance(v_, Buf)})

            def _chunk(ci):
                b_i, c_i = divmod(ci, NCH)
                tok0 = ci * 128
                xs = xs2[ci % 2]; hnT = hnT2[ci % 2]; yrw = yrw2[ci % 2]; posi = posi2[ci % 2]
                S.dma('sp', lambda e, xs=xs, tok0=tok0: e.dma_start(out=xs[:, :], in_=x_d[tok0:tok0 + 128, :]), 'xB%d' % (ci % 2), writes=[xs])
                S.dma('sp', lambda e, hnT=hnT, ci=ci: e.dma_start(out=hnT[:, :, :], in_=hnT_s[ci].rearrange("p (a b) -> p a b", a=8)),
                      'hB%d' % (ci % 2), writes=[hnT])
                S.dma('sp', lambda e, yrw=yrw, ci=ci: e.dma_start(out=yrw[:, :, :], in_=yrw_s[ci].rearrange("p (a b) -> p a b", a=4)),
                      'yB%d' % (ci % 2), writes=[yrw])
                S.dma('sp', lambda e, posi=posi, tok0=tok0: e.dma_start(out=posi[:, :], in_=pos_d[0:1, tok0:tok0 + 128].partition_broadcast(128)),
                      'pB%d' % (ci % 2), writes=[posi])
                if c_i == 0:
                    S.op('pool', lambda e: e.memset(Rf[:, :, :], 0.0), writes=[Rf])
                    S.op('pool', lambda e: e.memset(Rb[:, :, :], 0.0), writes=[Rb])
                S.op('dve', lambda e, posi=posi: e.tensor_copy(out=posf[:, :], in_=posi[:, :]), reads=[posi], writes=[posf])
                S.op('dve', lambda e: e.tensor_scalar(out=u0[:, :], in0=posf[:, :], scalar1=cst[:, INV:INV + 1], scalar2=None, op0=ALU.mult),
                     reads=[posf, cst], writes=[u0])
                S.op('dve', lambda e: e.tensor_copy(out=ti[:, :], in_=u0[:, :]), reads=[u0], writes=[ti])
                S.op('dve', lambda e: e.tensor_copy(out=tf[:, :], in_=ti[:, :]), reads=[ti], writes=[tf])
                S.op('dve', lambda e: e.tensor_tensor(out=tf[:, :], in0=u0[:, :], in1=tf[:, :], op=ALU.subtract), reads=[u0, tf], writes=[tf])
                S.op('act', lambda e: e.activation(out=sinT[:, :], in_=tf[:, :], func=AF.Sin, scale=cst[:, SSC:SSC + 1]), reads=[tf, cst], writes=[sinT])
                S.op('pool', lambda e: e.tensor_scalar(out=u1[:, :], in0=u0[:, :], scalar1=0.25, scalar2=None, op0=ALU.add), reads=[u0], writes=[u1])
                S.op('dve', lambda e: e.tensor_copy(out=ti[:, :], in_=u1[:, :]), reads=[u1], writes=[ti])
                S.op('dve', lambda e: e.tensor_copy(out=tf[:, :], in_=ti[:, :]), reads=[ti], writes=[tf])
                S.op('dve', lambda e: e.tensor_tensor(out=tf[:, :], in0=u1[:, :], in1=tf[:, :], op=ALU.subtract), reads=[u1, tf], writes=[tf])
                S.op('act', lambda e: e.activation(out=cosT[:, :], in_=tf[:, :], func=AF.Sin, scale=2.0 * math.pi), reads=[tf], writes=[cosT])
                for g in range(2):
                    bk = nb()
                    for jj in range(4):
                        j = g * 4 + jj
                        for kc in range(8):
                            S.op('pe', lambda e, bk=bk, jj=jj, j=j, kc=kc, hnT=hnT: e.matmul(
                                bk[:, jj * 128:(jj + 1) * 128], lhsT=w_b[:, kc, j * 128:(j + 1) * 128], rhs=hnT[:, kc, :],
                                start=(kc == 0), stop=(kc == 7)), reads=[w_b, hnT], writes=[bk])
                    S.op('act', lambda e, bk=bk, g=g: e.copy(out=qk[:, g * 4:g * 4 + 4, :], in_=v3(bk, 4)), reads=[bk], writes=[qk])
                S.op('pool', lambda e: e.tensor_copy(out=qkb[:, :, :], in_=qk[:, :, :]), reads=[qk], writes=[qkb])
                for grp in range(4):
                    for half in range(2):
                        bk = nb(); c0 = 1024 + grp * 1024 + half * 512
                        for kc in range(8):
                            S.op('pe', lambda e, bk=bk, kc=kc, c0=c0, hnT=hnT: e.matmul(bk[:, :], lhsT=hnT[:, kc, :], rhs=w_b[:, kc, c0:c0 + 512],
                                                                                        start=(kc == 0), stop=(kc == 7)), reads=[w_b, hnT], writes=[bk])
                        dst = (v_bf, sgb, sA, sB)[grp]
                        fn = (AF.Copy, AF.Silu, AF.Sigmoid, AF.Sigmoid)[grp]
                        S.op('act', lambda e, bk=bk, dst=dst, fn=fn, half=half: e.activation(out=dst[:, half * 512:(half + 1) * 512], in_=bk[:, :], func=fn),
                             reads=[bk], writes=[dst])
                bsw = [nb(), nb()]
                for j in range(8):
                    S.op('pe', lambda e, j=j: e.matmul(bsw[j // 4][:, (j % 4) * 128:(j % 4 + 1) * 128], lhsT=pswap, rhs=qkb[:, j, :], start=True, stop=True),
                         reads=[tabb, qkb], writes=[bsw[j // 4]])
                S.op('pool', lambda e: e.tensor_tensor(out=r1[:, :, :], in0=qk[:, :, :], in1=cosT[:, :].unsqueeze(1).to_broadcast([128, 8, 128]), op=ALU.mult),
                     reads=[qk, cosT], writes=[r1])
                for g in range(2):
                    S.op('dve', lambda e, g=g: e.tensor_tensor(out=r2[:, g * 4:g * 4 + 4, :], in0=v3(bsw[g], 4),
                                                               in1=sinT[:, :].unsqueeze(1).to_broadcast([128, 4, 128]), op=ALU.mult),
                         reads=[bsw[g], sinT], writes=[r2])
                S.op('dve', lambda e: e.tensor_tensor(out=rot[:, :, :], in0=r1[:, :, :], in1=r2[:, :, :], op=ALU.add), reads=[r1, r2], writes=[rot])
                S.op('pool', lambda e: e.tensor_tensor(out=qd[:, :, :], in0=rot[:, 0:4, :], in1=cst[:, QD:QD + 512].rearrange("p (a b) -> p a b", a=4), op=ALU.mult),
                     reads=[rot, cst], writes=[qd])
                bk = nb()
                for jc in range(4):
                    S.op('pe', lambda e, bk=bk, jc=jc: e.transpose(out=vbf(bk)[:, jc * 128:(jc + 1) * 128], in_=rot[:, 4 + jc, :], identity=ident),
                         reads=[rot, tabb], writes=[bk])
                bkv = vbf(bk)[:, 0:512].rearrange("p (a h c) -> p a h c", a=4, h=2)
                kdv = cst[:, KDEC:KDEC + 8].rearrange("p (a h) -> p a h", a=4)
                for hp in range(2):
                    S.op('dve', lambda e, hp=hp, bkv=bkv: e.tensor_tensor(out=kdZ[:, :, hp, hp * 64:(hp + 1) * 64], in0=bkv[:, :, hp, :],
                                                                          in1=kdv[:, :, hp:hp + 1].to_broadcast([128, 4, 64]), op=ALU.mult),
                         reads=[bk, cst], writes=[kdZ])
                for hp in range(2):
                    bk = nb(); pb = 64 * hp
                    for jc in range(4):
                        S.op('pe', lambda e, bk=bk, jc=jc, pb=pb: e.matmul(bk[:, jc * 128:(jc + 1) * 128], lhsT=rot[pb:pb + 64, 4 + jc, :],
                                                                           rhs=rot[pb:pb + 64, jc, :], start=True, stop=True), reads=[rot], writes=[bk])
                    S.op('dve', lambda e, bk=bk, hp=hp: e.tensor_tensor(
                        out=sT[:, :, :].rearrange("p (a h) c -> p a h c", h=2)[:, :, hp, :], in0=v3(bk, 4),
                        in1=tabf[:, T_MT:T_MT + 1024].rearrange("p (a h c) -> p a h c", a=4, h=2)[:, :, hp, :], op=ALU.mult),
                         reads=[bk, tabf], writes=[sT])
                bo = [nb(), nb()]
                for h in range(8):
                    jc = h // 2; pb = 64 * (h % 2); bk = bo[h // 4]; cc = (h % 4) * 128
                    S.op('pe', lambda e, bk=bk, cc=cc, h=h: e.matmul(bk[:, cc:cc + 128], lhsT=sT[:, h, :], rhs=v_bf[:, h * 128:(h + 1) * 128],
                                                                     start=True, stop=False), reads=[sT, v_bf], writes=[bk])
                    S.op('pe', lambda e, bk=bk, cc=cc, jc=jc, pb=pb: e.matmul(bk[:, cc:cc + 128], lhsT=qd[pb:pb + 64, jc, :], rhs=Rb[pb:pb + 64, jc, :],
                                                                              start=False, stop=True), reads=[qd, Rb], writes=[bk])
                for g in range(2):
                    S.op('act', lambda e, g=g: e.copy(out=of[:, g * 4:g * 4 + 4, :], in_=v3(bo[g], 4)), reads=[bo[g]], writes=[of])
                bR = nb()
                for jc in range(4):
                    for hp in range(2):
                        h = 2 * jc + hp
                        S.op('pe', lambda e, jc=jc, hp=hp, h=h: e.matmul(bR[:, jc * 128:(jc + 1) * 128], lhsT=kdZ[:, jc, hp, :], rhs=v_bf[:, h * 128:(h + 1) * 128],
                                                                         start=(hp == 0), stop=(hp == 1)), reads=[kdZ, v_bf], writes=[bR])
                S.op('pool', lambda e: e.tensor_tensor(out=Rf[:, :, :], in0=Rf[:, :, :], in1=cb(CD, 4, [128, 4, 128]), op=ALU.mult), reads=[Rf, cst], writes=[Rf])
                S.op('dve', lambda e: e.tensor_tensor(out=Rf[:, :, :], in0=Rf[:, :, :], in1=v3(bR, 4), op=ALU.add), reads=[Rf, bR], writes=[Rf])
                S.op('act', lambda e: e.copy(out=Rb[:, :, :], in_=Rf[:, :, :]), reads=[Rf], writes=[Rb])
                S.op('dve', lambda e: e.tensor_reduce(out=stt[:, 0:8], in_=of[:, :, :], axis=AX.X, op=ALU.add), reads=[of], writes=[stt])
                S.op('pool', lambda e: e.tensor_tensor(out=osq[:, :, :], in0=of[:, :, :], in1=of[:, :, :], op=ALU.mult), reads=[of], writes=[osq])
                S.op('dve', lambda e: e.tensor_reduce(out=stt[:, 8:16], in_=osq[:, :, :], axis=AX.X, op=ALU.add), reads=[osq], writes=[stt])
                S.op('dve', lambda e: e.tensor_scalar(out=mean[:, :], in0=stt[:, 0:8], scalar1=1.0 / 128, scalar2=None, op0=ALU.mult), reads=[stt], writes=[mean])
                S.op('dve', lambda e: e.tensor_tensor(out=var[:, :], in0=mean[:, :], in1=mean[:, :], op=ALU.mult), reads=[mean], writes=[var])
                S.op('dve', lambda e: e.scalar_tensor_tensor(out=var[:, :], in0=stt[:, 8:16], scalar=1.0 / 128, in1=var[:, :], op0=ALU.mult, op1=ALU.subtract),
                     reads=[stt, var], writes=[var])
                S.op('act', lambda e: e.activation(out=var[:, :], in_=var[:, :], func=AF.Sqrt, bias=1e-5), reads=[var], writes=[var])
                S.op('dve', lambda e: e.reciprocal(out=var[:, :], in_=var[:, :]), reads=[var], writes=[var])
                S.op('dve', lambda e: e.tensor_tensor(out=of[:, :, :], in0=of[:, :, :], in1=mean[:, :].unsqueeze(2).to_broadcast([128, 8, 128]), op=ALU.subtract),
                     reads=[of, mean], writes=[of])
                S.op('pool', lambda e: e.tensor_tensor(out=of[:, :, :], in0=of[:, :, :], in1=var[:, :].unsqueeze(2).to_broadcast([128, 8, 128]), op=ALU.mult),
                     reads=[of, var], writes=[of])
                S.op('dve', lambda e: e.tensor_tensor(out=yret[:, :], in0=of[:, :, :].rearrange("p a b -> p (a b)"), in1=sgb[:, :], op=ALU.mult),
                     reads=[of, sgb], writes=[yret])
                bk = nb()
                for kc in range(8):
                    S.op('pe', lambda e, bk=bk, kc=kc: e.transpose(out=vbf(bk)[:, kc * 128:(kc + 1) * 128], in_=yret[:, kc * 128:(kc + 1) * 128], identity=ident),
                         reads=[yret, tabb], writes=[bk])
                S.op('act', lambda e, bk=bk: e.copy(out=yretT[:, :, :], in_=vbf(bk).rearrange("p (a b) -> p a b", a=8)), reads=[bk], writes=[yretT])
                for half in range(2):
                    b1 = nb(); b2 = nb(); hs = slice(half * 512, (half + 1) * 512)
                    for jc in range(4):
                        S.op('pe', lambda e, b1=b1, jc=jc, hs=hs, yrw=yrw: e.matmul(b1[:, :], lhsT=yrw[:, jc, :], rhs=wbrw[:, jc, hs], start=(jc == 0), stop=(jc == 3)),
                             reads=[yrw, wbrw], writes=[b1])
                    for kc in range(8):
                        S.op('pe', lambda e, b2=b2, kc=kc, hs=hs: e.matmul(b2[:, :], lhsT=yretT[:, kc, :], rhs=wbret[:, kc, hs], start=(kc == 0), stop=(kc == 7)),
                             reads=[yretT, wbret], writes=[b2])
                    S.op('dve', lambda e, b1=b1, hs=hs: e.tensor_tensor(out=m1[:, hs], in0=b1[:, :], in1=sA[:, hs], op=ALU.mult), reads=[b1, sA], writes=[m1])
                    S.op('dve', lambda e, b2=b2, hs=hs: e.tensor_tensor(out=m2[:, hs], in0=b2[:, :], in1=sB[:, hs], op=ALU.mult), reads=[b2, sB], writes=[m2])
                S.op('pool', lambda e: e.tensor_tensor(out=mg[:, :], in0=m1[:, :], in1=m2[:, :], op=ALU.add), reads=[m1, m2], writes=[mg])
                bk = nb()
                for kc in range(8):
                    S.op('pe', lambda e, bk=bk, kc=kc: e.transpose(out=vbf(bk)[:, kc * 128:(kc + 1) * 128], in_=mg[:, kc * 128:(kc + 1) * 128], identity=ident),
                         reads=[mg, tabb], writes=[bk])
                S.op('act', lambda e, bk=bk: e.copy(out=mT[:, :, :], in_=vbf(bk).rearrange("p (a b) -> p a b", a=8)), reads=[bk], writes=[mT])
                for half in range(2):
                    bk = nb(); hs = slice(half * 512, (half + 1) * 512)
                    for kc in range(8):
                        S.op('pe', lambda e, bk=bk, kc=kc, hs=hs: e.matmul(bk[:, :], lhsT=mT[:, kc, :], rhs=wout[:, kc, hs], start=(kc == 0), stop=(kc == 7)),
                             reads=[mT, wout], writes=[bk])
                    S.op('act', lambda e, bk=bk, hs=hs: e.copy(out=mo[:, hs], in_=bk[:, :]), reads=[bk], writes=[mo])
                S.op('act', lambda e: e.activation(out=junk[:, :], in_=mo[:, :], func=AF.Square, accum_out=ss[:, 0:1]), reads=[mo], writes=[junk, ss])
                S.op('act', lambda e: e.activation(out=rs[:, 0:1], in_=ss[:, 0:1], func=AF.Sqrt, scale=1.0 / D, bias=1e-6), reads=[ss], writes=[rs])
                S.op('dve', lambda e: e.reciprocal(out=rs[:, 0:1], in_=rs[:, 0:1]), reads=[rs], writes=[rs])
                S.op('dve', lambda e: e.scalar_tensor_tensor(out=hh[:, :], in0=mo[:, :], scalar=rs[:, 0:1], in1=rows[:, 0, :], op0=ALU.mult, op1=ALU.mult),
                     reads=[mo, rs, rows], writes=[hh])
                S.op('pool', lambda e, xs=xs: e.tensor_tensor(out=hh[:, :], in0=hh[:, :], in1=xs[:, :], op=ALU.add), reads=[hh, xs], writes=[hh])
                S.dma('sp', lambda e, tok0=tok0: e.dma_start(out=h_s[tok0:tok0 + 128, :], in_=hh[:, :]), 'hstore', reads=[hh])
            for ci in range(NCHUNK):
                _chunk(ci)
            S.barrier()

        if "C" in phases:
          with ExitStack() as st:
            wup = S.sb(st, [128, 8, 2 * DFF], BF16); wdn = S.sb(st, [128, 22, D], BF16)
            for kc in range(8):
                S.dma('pool', lambda e, kc=kc: e.dma_start(out=wup[:, kc, :], in_=wup_d[kc * 128:(kc + 1) * 128, :]), 'wC', writes=[wup])
            for j in range(22):
                S.dma('pool', lambda e, j=j: e.dma_start(out=wdn[:, j, :], in_=wdn_d[j * 128:(j + 1) * 128, :]), 'wC', writes=[wdn])
            hb = [S.sb(st, [128, D], F32) for _ in range(NSUB)]
            tmp_bf = S.sb(st, [128, D], BF16); xn = S.sb(st, [128, D], BF16)
            ss = S.sb(st, [128, 1], F32); rs = S.sb(st, [128, 1], F32)
            hn2T = S.sb(st, [128, 8, FT], BF16)
            actT = S.sb(st, [128, 22, FT], BF16)
            ub2 = [S.sb(st, [128, FT + 2], F32) for _ in range(4)]
            acc2 = [S.sb(st, [128, FT], F32) for _ in range(4)]
            gg2 = [S.sb(st, [128, FT], F32) for _ in range(2)]
            carry = S.sb(st, [128, 44, 2], F32)
            fo = S.sb(st, [128, D], F32); oo = S.sb(st, [128, D], F32)
            ucl = [0]

            def _tile(ti_):
                b_i, t_i = divmod(ti_, NTILE)
                tokb = ti_ * FT
                if t_i == 0:
                    S.op('pool', lambda e: e.memset(carry[:, :, :], 0.0), writes=[carry])
                for sc in range(NSUB):
                    S.dma('sp', lambda e, sc=sc, tokb=tokb: e.dma_start(out=hb[sc][:, :], in_=h_s[tokb + sc * 128:tokb + (sc + 1) * 128, :]),
                          'hC%d' % sc, writes=[hb[sc]])
                    rmsnorm_T(st, hb[sc], hn2T, sc * 128, G3, tmp_bf, ss, rs, xn)
                def _finish(jp):
                    ag = acc2[(2 * jp) % 4]; av = acc2[(2 * jp + 1) % 4]; gg = gg2[jp % 2]
                    S.op('act', lambda e: e.activation(out=gg[:, :], in_=ag[:, :], func=AF.Gelu_apprx_tanh), reads=[ag], writes=[gg])
                    S.op('pool', lambda e: e.tensor_tensor(out=actT[:, jp, :], in0=gg[:, :], in1=av[:, :], op=ALU.mult),
                         reads=[gg, av], writes=[actT])

                for jp in range(22):
                    if jp >= 1:
                        pass
                    for which in range(2):
                        j = jp + 22 * which
                        bk = nb(); ub = ub2[(2 * jp + which) % 4]; acc = acc2[(2 * jp + which) % 4]
                        for kc in range(8):
                            S.op('pe', lambda e, bk=bk, kc=kc, j=j: e.matmul(bk[:, 0:FT], lhsT=wup[:, kc, j * 128:(j + 1) * 128], rhs=hn2T[:, kc, :],
                                                                             start=(kc == 0), stop=(kc == 7)), reads=[wup, hn2T], writes=[bk])
                        S.op('act', lambda e, bk=bk, ub=ub: e.copy(out=ub[:, 2:FT + 2], in_=bk[:, 0:FT]), reads=[bk], writes=[ub])
                        S.op('pool', lambda e, ub=ub, j=j: e.tensor_copy(out=ub[:, 0:2], in_=carry[:, j, :]), reads=[carry], writes=[ub])
                        S.op('act', lambda e, bk=bk, acc=acc, j=j: e.activation(out=acc[:, :], in_=bk[:, 0:FT], func=AF.Identity,
                                                                                 scale=cst[:, CW + 2 * 44 + j:CW + 2 * 44 + j + 1], bias=cst[:, CB + j:CB + j + 1]),
                             reads=[bk, cst], writes=[acc])
                        S.op('dve', lambda e, ub=ub, acc=acc, j=j: e.scalar_tensor_tensor(out=acc[:, :], in0=ub[:, 1:FT + 1], scalar=cst[:, CW + 44 + j:CW + 44 + j + 1],
                                                                                         in1=acc[:, :], op0=ALU.mult, op1=ALU.add), reads=[ub, acc, cst], writes=[acc])
                        S.op('dve', lambda e, ub=ub, acc=acc, j=j: e.scalar_tensor_tensor(out=acc[:, :], in0=ub[:, 0:FT], scalar=cst[:, CW + j:CW + j + 1],
                                                                                         in1=acc[:, :], op0=ALU.mult, op1=ALU.add), reads=[ub, acc, cst], writes=[acc])
                        S.op('pool', lambda e, ub=ub, j=j: e.tensor_copy(out=carry[:, j, :], in_=ub[:, FT:FT + 2]), reads=[ub], writes=[carry])
                    if jp >= 1:
                        _finish(jp - 1)
                _finish(21)
                for sc in range(NSUB):
                    for half in range(2):
                        bk = nb(); hs = slice(half * 512, (half + 1) * 512)
                        for j in range(22):
                            S.op('pe', lambda e, bk=bk, j=j, sc=sc, hs=hs: e.matmul(bk[:, :], lhsT=actT[:, j, sc * 128:(sc + 1) * 128], rhs=wdn[:, j, hs],
                                                                                    start=(j == 0), stop=(j == 21)), reads=[actT, wdn], writes=[bk])
                        S.op('act', lambda e, bk=bk, hs=hs: e.copy(out=fo[:, hs], in_=bk[:, :]), reads=[bk], writes=[fo])
                    S.op('act', lambda e: e.activation(out=tmp_bf[:, :], in_=fo[:, :], func=AF.Square, accum_out=ss[:, 0:1]), reads=[fo], writes=[tmp_bf, ss])
                    S.op('act', lambda e: e.activation(out=rs[:, 0:1], in_=ss[:, 0:1], func=AF.Sqrt, scale=1.0 / D, bias=1e-6), reads=[ss], writes=[rs])
                    S.op('dve', lambda e: e.reciprocal(out=rs[:, 0:1], in_=rs[:, 0:1]), reads=[rs], writes=[rs])
                    S.op('dve', lambda e: e.scalar_tensor_tensor(out=oo[:, :], in0=fo[:, :], scalar=rs[:, 0:1], in1=rows[:, 1, :], op0=ALU.mult, op1=ALU.mult),
                         reads=[fo, rs, rows], writes=[oo])
                    S.op('pool', lambda e, sc=sc: e.tensor_tensor(out=oo[:, :], in0=oo[:, :], in1=hb[sc][:, :], op=ALU.add), reads=[oo, hb[sc]], writes=[oo])
                    S.dma('sp', lambda e, sc=sc, tokb=tokb: e.dma_start(out=out_d[tokb + sc * 128:tokb + (sc + 1) * 128, :], in_=oo[:, :]), 'ostore', reads=[oo])
            for ti_ in range(NSEQ * NTILE):
                _tile(ti_)
            S.barrier()
        S.barrier()
        S.emit()
    return nc


def host_consts(inp):
    f = np.float32
    c = np.zeros((128, NCONST), f)

    def pk(v, n):
        return np.asarray(v, f).reshape(n, 128).T
    c[:, G1:G1 + 8] = pk(inp["norm_mix_pre"][0], 8)
    c[:, MU:MU + 14] = pk(inp["rw_mu"][0], 14)
    c[:, W0:W0 + 4] = pk(inp["rw_w0"][0], 4)
    c[:, A0:A0 + 4] = pk(inp["rw_a0"][0], 4)
    c[:, KK:KK + 4] = pk(inp["rw_k_k"][0], 4)
    c[:, KA:KA + 4] = pk(inp["rw_k_a"][0], 4)
    c[:, RK:RK + 4] = pk(inp["rw_r_k"][0], 4)
    c[:, LW:LW + 4] = pk(inp["rw_lnx_w"][0], 4)
    c[:, LB:LB + 4] = pk(inp["rw_lnx_b"][0], 4)
    c[:, G3:G3 + 8] = pk(inp["norm_ffn_pre"][0], 8)
    c[:, CB:CB + 44] = pk(inp["ffn_conv_b"][0], 44)
    for tap in range(3):
        c[:, CW + tap * 44:CW + (tap + 1) * 44] = pk(inp["ffn_conv_w"][0, tap], 44)
    p = np.arange(128)
    inv = (10000.0 ** (-(np.arange(32, dtype=np.float32)) / np.float32(32))).astype(f)
    c[:, INV] = inv[p % 32] / f(2 * math.pi)
    c[:, SSC] = np.where((p % 64) < 32, -2 * math.pi, 2 * math.pi).astype(f)
    lg = np.log1p(-np.exp2(-5.0 - np.arange(8, dtype=np.float64)))
    for jc in range(4):
        hsel = 2 * jc + p // 64
        c[:, CD + jc] = np.exp(128 * lg[hsel])
        c[:, QD + jc * 128:QD + (jc + 1) * 128] = np.exp((np.arange(128)[None, :] + 1.0) * lg[hsel][:, None])
    for h in range(8):
        c[:, KDEC + h] = 0.125 * np.exp((127.0 - p) * lg[h])
    tab = np.zeros((128, NTAB), f)
    s = np.arange(128)[:, None]; t = np.arange(128)[None, :]
    strict = (t > s).astype(f); incl = (t >= s).astype(f)
    tab[:, T_MP:T_MP + 512] = np.concatenate([strict, incl, strict, incl], axis=1)
    tab[:, T_M0:T_M0 + 128] = (t < s).astype(f)
    for h in range(8):
        tab[:, T_MT + h * 128:T_MT + (h + 1) * 128] = np.where(t >= s, 0.125 * np.exp(np.maximum(t - s, 0) * lg[h]), 0.0)
    tab[:, T_BM:T_BM + 128] = ((s // 64) == (t // 64)).astype(f)
    tab[:, T_ID:T_ID + 128] = np.eye(128, dtype=f)
    tab[:, T_SW:T_SW + 128] = (t == (s ^ 32)).astype(f)
    lr = np.concatenate([np.concatenate([inp["rw_w2"][0], inp["rw_a2"][0]], axis=0), inp["rw_g2"][0]], axis=1).astype(f)
    rows = np.stack([inp["norm_mix_post"][0], inp["norm_ffn_post"][0]], axis=0).astype(f)
    return c, tab, lr, rows


_NC_CACHE = {}
STOP = [None]


def _stop(n):
    return STOP[0] is not None and n >= STOP[0]

DBG = {}


def run(inp, NSEQ, NCH, ncores, dbg=False, phases="ABC"):
    key = (NSEQ, NCH, dbg, phases, STOP[0])
    if key not in _NC_CACHE:
        _NC_CACHE[key] = build(NSEQ, NCH, dbg, phases)
    nc = _NC_CACHE[key]
    c, tab, lr, rows = host_consts(inp)
    T = NCH * 128
    shared = {
        "w_in": np.ascontiguousarray(inp["w_in"][0]), "wbrw": np.ascontiguousarray(inp["w_branch_rw"][0]),
        "wbret": np.ascontiguousarray(inp["w_branch_ret"][0]), "wout": np.ascontiguousarray(inp["w_out"][0]),
        "wup": np.ascontiguousarray(inp["ffn_w_up"][0]), "wdn": np.ascontiguousarray(inp["ffn_w_down"][0]),
        "lr": lr, "c128": c, "rows": rows, "tab": tab,
    }
    in_maps = []
    for i in range(ncores):
        xs = np.ascontiguousarray(inp["x"][i * NSEQ:(i + 1) * NSEQ, :T, :]).reshape(NSEQ * T, D)
        ps = np.ascontiguousarray(inp["positions"][i * NSEQ:(i + 1) * NSEQ, :T]).reshape(1, NSEQ * T).astype(np.int32)
        m = dict(shared); m["x"] = xs; m["pos"] = ps
        in_maps.append(m)
    res = run_bass_kernel_spmd(nc, in_maps, core_ids=list(range(ncores)))
    return res


def kernel(**inputs):
    inp = {k: np.asarray(v) for k, v in inputs.items()}
    B, T, _ = inp["x"].shape
    ncores = 8
    NSEQ = B // ncores
    res = run(inp, NSEQ, T // 128, ncores)
    out = np.concatenate([r["out"].reshape(NSEQ, T, D) for r in res.results], axis=0)
    return out.astype(np.float32)
```

```python
import math
import numpy as np
from contextlib import ExitStack
import concourse.bass as bass
import concourse.mybir as mybir
from concourse.bass_utils import run_bass_kernel_spmd

F32 = mybir.dt.float32; BF16 = mybir.dt.bfloat16; I32 = mybir.dt.int32
AF = mybir.ActivationFunctionType; ALU = mybir.AluOpType; AX = mybir.AxisListType

D = 1024; RWC = 1792; INC = 6912; DFF = 2816
C0 = math.exp(-0.5)
G1, MU, W0, A0, KK, KA, RK, LW, LB, INV, SSC, CD, G3, CB, CW, QD, KDEC, NCONST = (
    0, 8, 22, 26, 30, 34, 38, 42, 46, 50, 51, 52, 56, 64, 108, 240, 752, 760)
T_MP, T_M0, T_MT, T_BM, T_ID, T_SW, NTAB = 0, 512, 640, 1664, 1792, 1920, 2048


class Buf:
    def __init__(self, t, name):
        self.t = t; self.name = name; self.writers = {}; self.readers = {}

    def __getitem__(self, k):
        return self.t[k]


class Sched:
    ENG = ['pe', 'act', 'dve', 'pool', 'sp']

    def __init__(self, nc, stack):
        self.nc = nc; self.stack = stack; self.semh = {}
        for e in self.ENG:
            self.semh[e] = stack.enter_context(nc.semaphore("s_" + e))
        self.cnt = {e: 0 for e in self.ENG}
        self.known = {e: {} for e in self.ENG}
        self.prog = {e: [] for e in self.ENG}
        self.dcnt = {}
        self.nbuf = 0

    def sb(self, st, shape, dt):
        self.nbuf += 1
        name = f"b{self.nbuf}"
        return Buf(st.enter_context(self.nc.sbuf_tensor(name, list(shape), dt)), name)

    def ps(self, st, shape, dt):
        self.nbuf += 1
        name = f"p{self.nbuf}"
        b = Buf(st.enter_context(self.nc.psum_tensor(name, list(shape), dt)), name)
        b.psum = True
        return b

    def _waits(self, eng, reads, writes):
        need = {}

        def add(k, v, raw):
            if k == eng and not raw and eng == 'pe':
                return
            if need.get(k, 0) < v:
                need[k] = v
        for b in reads:
            for k, v in b.writers.items():
                add(k, v, True)
        for b in writes:
            for k, v in b.writers.items():
                add(k, v, False)
            for k, v in b.readers.items():
                add(k, v, False)
        out = []
        for k, v in need.items():
            if k.startswith('d_'):
                v = self.dcnt[k]
            if self.known[eng].get(k, 0) >= v:
                continue
            self.known[eng][k] = v
            out.append((self.semh[k], v))
        return out

    def op(self, eng, fn, reads=(), writes=()):
        pr = [b for b in reads if getattr(b, 'psum', False)]
        if pr:
            writes = list(writes) + [b for b in pr if b not in writes]
            reads = [b for b in reads if not getattr(b, 'psum', False)]
        waits = self._waits(eng, reads, writes)
        self.cnt[eng] += 1
        n = self.cnt[eng]
        sem = self.semh[eng]

        def run(e, waits=waits, fn=fn, sem=sem):
            for s, v in waits:
                e.wait_ge(s, v)
            fn(e).then_inc(sem, 1)
        self.prog[eng].append(run)
        for b in reads:
            if b.readers.get(eng, 0) < n:
                b.readers[eng] = n
        for b in writes:
            b.writers = {eng: n}; b.readers = {}

    def dma(self, q, fn, semname, reads=(), writes=()):
        key = 'd_' + semname
        if key not in self.semh:
            self.semh[key] = self.stack.enter_context(self.nc.semaphore(key))
            self.dcnt[key] = 0
        waits = self._waits(q, reads, writes)
        self.dcnt[key] += 16
        n = self.dcnt[key]
        sem = self.semh[key]

        def run(e, waits=waits, fn=fn, sem=sem):
            for s, v in waits:
                e.wait_ge(s, v)
            fn(e).then_inc(sem, 16)
        self.prog[q].append(run)
        for b in reads:
            if b.readers.get(key, 0) < n:
                b.readers[key] = n
        for b in writes:
            b.writers = dict(b.writers); b.writers[key] = n
            b.readers = {}

    def barrier(self):
        allc = {}
        for e in self.ENG:
            if self.cnt[e] > 0:
                allc[e] = self.cnt[e]
        for k, v in self.dcnt.items():
            if v > 0:
                allc[k] = v
        for e in self.ENG:
            waits = []
            for k, v in allc.items():
                if self.known[e].get(k, 0) >= v:
                    continue
                self.known[e][k] = v
                waits.append((self.semh[k], v))

            def run(en, waits=waits):
                for s, v in waits:
                    en.wait_ge(s, v)
            self.prog[e].append(run)

    def emit(self):
        nc = self.nc
        with nc.Block() as block:
            @block.tensor
            def _(e):
                for f in self.prog['pe']:
                    f(e)

            @block.scalar
            def _(e):
                for f in self.prog['act']:
                    f(e)

            @block.vector
            def _(e):
                for f in self.prog['dve']:
                    f(e)

            @block.gpsimd
            def _(e):
                for f in self.prog['pool']:
                    f(e)

            @block.sync
            def _(e):
                for f in self.prog['sp']:
                    f(e)


def build(NSEQ, NCH, dbg=False, phases="ABC"):
    T = NCH * 128; NTOK = NSEQ * T; NCHUNK = NSEQ * NCH
    FT = min(256, T); NSUB = FT // 128; NTILE = T // FT
    nc = bass.Bass("TRN2", target_bir_lowering=False)

    def dr(name, shape, dt, kind="ExternalInput"):
        return nc.dram_tensor(name, list(shape), dt, kind=kind).ap()
    x_d = dr("x", [NTOK, D], F32)
    pos_d = dr("pos", [1, NTOK], I32)
    win_d = dr("w_in", [D, INC], F32)
    wbrw_d = dr("wbrw", [512, D], F32)
    wbret_d = dr("wbret", [D, D], F32)
    wout_d = dr("wout", [D, D], F32)
    wup_d = dr("wup", [D, 2 * DFF], F32)
    wdn_d = dr("wdn", [DFF, D], F32)
    lr_d = dr("lr", [128, 1024], F32)
    c_d = dr("c128", [128, NCONST], F32)
    rows_d = dr("rows", [2, D], F32)
    tab_d = dr("tab", [128, NTAB], F32)
    out_d = dr("out", [NTOK, D], F32, kind="ExternalOutput")
    sk = "ExternalOutput" if dbg else "Internal"
    hnT_s = dr("hnT_s", [NCHUNK, 128, 1024], BF16, kind=sk)
    yrw_s = dr("yrw_s", [NCHUNK, 128, 512], BF16, kind=sk)
    h_s = dr("h_s", [NTOK, D], F32, kind=sk)

    with ExitStack() as st0:
        S = Sched(nc, st0)
        banks = [S.ps(st0, [128, 512], F32) for _ in range(8)]
        bstate = [0]

        def nb():
            b = banks[bstate[0] % 8]; bstate[0] += 1
            return b

        def v3(b, a):
            return b.t[:, :].rearrange("p (a b) -> p a b", a=a)

        def vbf(b):
            return b.t[:, :].bitcast(BF16)

        cst = S.sb(st0, [128, NCONST], F32)
        tabf = S.sb(st0, [128, NTAB], F32)
        tabb = S.sb(st0, [128, NTAB], BF16)
        rows = S.sb(st0, [128, 2, D], F32)
        ones = S.sb(st0, [128, 128], F32)
        omka = S.sb(st0, [128, 4], F32)
        S.dma('sp', lambda e: e.dma_start(out=cst[:, :], in_=c_d), 'cst', writes=[cst])
        S.dma('sp', lambda e: e.dma_start(out=tabf[:, :], in_=tab_d), 'tabf', writes=[tabf])
        S.dma('pool', lambda e: e.dma_start(out=tabb[:, :], in_=tab_d), 'tabb', writes=[tabb])
        for r in range(2):
            S.dma('sp', lambda e, r=r: e.dma_start(out=rows[:, r, :], in_=rows_d[r:r + 1, :].partition_broadcast(128)),
                  'rows', writes=[rows])
        S.op('pool', lambda e: e.memset(ones[:, :], 1.0), writes=[ones])
        S.op('dve', lambda e: e.tensor_scalar(out=omka[:, :], in0=cst[:, KA:KA + 4], scalar1=-1.0, scalar2=1.0,
                                              op0=ALU.mult, op1=ALU.add), reads=[cst], writes=[omka])
        ident = tabb[:, T_ID:T_ID + 128]
        bones = tabb[:, T_BM:T_BM + 128]
        pswap = tabb[:, T_SW:T_SW + 128]

        def cb(off, n, shape):
            return cst[:, off:off + n].unsqueeze(2).to_broadcast(shape)

        def rmsnorm_T(stp, xs, dstT, col0, gcol, tmp_bf, ss, rs, xn, use_ln=False):
            S.op('act', lambda e: e.activation(out=tmp_bf[:, :], in_=xs[:, :], func=AF.Square, accum_out=ss[:, 0:1]),
                 reads=[xs], writes=[tmp_bf, ss])
            if use_ln:
                S.op('act', lambda e: e.activation(out=rs[:, 0:1], in_=ss[:, 0:1], func=AF.Ln, scale=1.0 / D, bias=1e-6),
                     reads=[ss], writes=[rs])
                S.op('act', lambda e: e.activation(out=rs[:, 0:1], in_=rs[:, 0:1], func=AF.Exp, scale=-0.5), reads=[rs], writes=[rs])
            else:
                S.op('act', lambda e: e.activation(out=rs[:, 0:1], in_=ss[:, 0:1], func=AF.Sqrt, scale=1.0 / D, bias=1e-6),
                     reads=[ss], writes=[rs])
                S.op('dve', lambda e: e.reciprocal(out=rs[:, 0:1], in_=rs[:, 0:1]), reads=[rs], writes=[rs])
            S.op('dve', lambda e: e.tensor_scalar(out=xn[:, :], in0=xs[:, :], scalar1=rs[:, 0:1], scalar2=None,
                                                  op0=ALU.mult), reads=[xs, rs], writes=[xn])
            bk = nb()
            for kc in range(8):
                S.op('pe', lambda e, kc=kc: e.transpose(out=vbf(bk)[:, kc * 128:(kc + 1) * 128],
                                                        in_=xn[:, kc * 128:(kc + 1) * 128], identity=ident),
                     reads=[xn, tabb], writes=[bk])
            S.op('dve', lambda e: e.tensor_tensor(
                out=dstT[:, :, col0:col0 + 128],
                in0=vbf(bk).rearrange("p (a b) -> p a b", a=8),
                in1=cb(gcol, 8, [128, 8, 128]), op=ALU.mult), reads=[bk, cst], writes=[dstT])

        if "A" in phases:
          with ExitStack() as st:
            w_rw = S.sb(st, [128, 8, RWC], BF16)
            lrb = S.sb(st, [128, 1024], BF16)
            for kc in range(8):
                S.dma('pool', lambda e, kc=kc: e.dma_start(out=w_rw[:, kc, :], in_=win_d[kc * 128:(kc + 1) * 128, 0:RWC]),
                      'wA', writes=[w_rw])
            S.dma('pool', lambda e: e.dma_start(out=lrb[:, :], in_=lr_d), 'wA', writes=[lrb])
            xs2 = [S.sb(st, [128, D], F32) for _ in range(2)]
            tmp_bf = S.sb(st, [128, D], BF16); xn = S.sb(st, [128, D], BF16)
            ss = S.sb(st, [128, 1], F32); rs = S.sb(st, [128, 1], F32)
            hnT = S.sb(st, [128, 8, 128], BF16)
            praw = S.sb(st, [128, 14, 129], F32)
            pl = S.sb(st, [128, 14, 128], F32)
            lr12 = S.sb(st, [128, 128], BF16); sg = S.sb(st, [128, 128], BF16)
            f = [S.sb(st, [128, 4, 128], F32) for _ in range(16)]
            (gT, sig, cs, csx, Epos, Eneg, Em, Ehat, al, kq, kkb, kp, bv, t0, t1, t2) = f
            sqb = S.sb(st, [128, 4, 128], BF16)
            SCin = S.sb(st, [128, 4, 2, 128], BF16)
            BtT = S.sb(st, [128, 4, 128], BF16); KtT = S.sb(st, [128, 4, 128], BF16)
            BhT = S.sb(st, [128, 4, 128], BF16); KhT = S.sb(st, [128, 4, 128], BF16)
            vTb = S.sb(st, [128, 4, 128], BF16)
            Vp = S.sb(st, [128, 4, 128], BF16); BH = S.sb(st, [128, 4, 128], BF16); KH = S.sb(st, [128, 4, 128], BF16)
            VZ = S.sb(st, [128, 4, 2, 128], BF16); AZ = S.sb(st, [128, 4, 2, 128], BF16); UVZ = S.sb(st, [128, 4, 2, 128], BF16)
            Ap = S.sb(st, [128, 4, 128], BF16); UVp = S.sb(st, [128, 4, 128], BF16)
            SCT = S.sb(st, [128, 4, 2, 512], BF16)
            Xs = S.sb(st, [128, 4, 2, 128], BF16)
            MN = S.sb(st, [128, 4, 2, 256], BF16)
            Xt = [Buf(Xs.t, 'Xt%d' % q_) for q_ in range(4)]
            MNt = [Buf(MN.t, 'MNt%d' % q_) for q_ in range(4)]
            RhT = S.sb(st, [128, 4, 128], BF16); GZ = S.sb(st, [128, 4, 128], BF16)
            HZ = S.sb(st, [128, 4, 128], BF16); Hf = S.sb(st, [128, 4, 128], F32)
            yb = S.sb(st, [128, 4, 128], BF16); ysq = S.sb(st, [128, 4, 128], BF16); rkb = S.sb(st, [128, 4, 128], BF16)
            yrwT = S.sb(st, [128, 4, 128], BF16)
            for z in (VZ, AZ, UVZ):
                S.op('pool', lambda e, z=z: e.memset(z[:, :, :, :], 0.0), writes=[z])
            bmf = tabf[:, T_BM:T_BM + 128].unsqueeze(1).to_broadcast([128, 4, 128])
            prt = [Buf(praw.t, 'prt%d' % q_) for q_ in range(4)]
            plt = [Buf(pl.t, 'plt%d' % q_) for q_ in range(4)]
            tmpS = [S.sb(st, [128, 512], BF16) for _ in range(2)]

            DBG.update({k_: v_.name for k_, v_ in list(locals().items()) if isinstance(v_, Buf)})

            def _chunk(ci):
                b_i, c_i = divmod(ci, NCH)
                tok0 = ci * 128
                xs = xs2[ci % 2]
                S.dma('sp', lambda e, xs=xs, tok0=tok0: e.dma_start(out=xs[:, :], in_=x_d[tok0:tok0 + 128, :]),
                      'xA%d' % (ci % 2), writes=[xs])
                if c_i == 0:
                    S.op('pool', lambda e: e.memset(praw[:, :, 0:1], 0.0), writes=prt)
                    S.op('pool', lambda e: e.memset(Hf[:, :, :], 0.0), writes=[Hf])
                    S.op('pool', lambda e: e.memset(HZ[:, :, :], 0.0), writes=[HZ])
                rmsnorm_T(st, xs, hnT, 0, G1, tmp_bf, ss, rs, xn, use_ln=True)
                S.dma('sp', lambda e, ci=ci: e.dma_start(out=hnT_s[ci].rearrange("p (a b) -> p a b", a=8), in_=hnT[:, :, :]),
                      'hst', reads=[hnT])
                if _stop(1):
                    return
                def proj_group(g):
                    bk = nb(); n = 4 if g < 3 else 2
                    j0 = g * 4
                    for jj in range(n):
                        j = j0 + jj
                        for kc in range(8):
                            S.op('pe', lambda e, jj=jj, j=j, kc=kc: e.matmul(
                                bk[:, jj * 128:(jj + 1) * 128], lhsT=w_rw[:, kc, j * 128:(j + 1) * 128], rhs=hnT[:, kc, :],
                                start=(kc == 0), stop=(kc == 7)), reads=[w_rw, hnT], writes=[bk])
                    S.op('act', lambda e: e.copy(out=praw[:, j0:j0 + n, 1:129], in_=v3(bk, 4)[:, 0:n, :]), reads=[bk], writes=[prt[g]])
                    S.op('dve', lambda e: e.tensor_tensor(out=pl[:, j0:j0 + n, :], in0=praw[:, j0:j0 + n, 0:128], in1=praw[:, j0:j0 + n, 1:129],
                                                          op=ALU.subtract), reads=[prt[g]], writes=[plt[g]])
                    S.op('pool', lambda e: e.tensor_tensor(out=pl[:, j0:j0 + n, :], in0=pl[:, j0:j0 + n, :], in1=cb(MU + j0, n, [128, n, 128]),
                                                           op=ALU.mult), reads=[plt[g], cst], writes=[plt[g]])
                    S.op('dve', lambda e: e.tensor_tensor(out=pl[:, j0:j0 + n, :], in0=pl[:, j0:j0 + n, :], in1=praw[:, j0:j0 + n, 1:129],
                                                          op=ALU.add), reads=[plt[g], prt[g]], writes=[plt[g]])
                    S.op('act', lambda e: e.copy(out=praw[:, j0:j0 + n, 0:1], in_=praw[:, j0:j0 + n, 128:129]), reads=[prt[g]], writes=[prt[g]])

                proj_group(3)
                proj_group(1)
                if _stop(2):
                    return
                S.op('act', lambda e: e.activation(out=lr12[0:64, :], in_=pl[0:64, 12, :], func=AF.Tanh), reads=[plt[3]], writes=[lr12])
                S.op('act', lambda e: e.copy(out=lr12[64:128, :], in_=pl[64:128, 12, :]), reads=[plt[3]], writes=[lr12])
                S.op('act', lambda e: e.activation(out=sg[:, :], in_=pl[:, 13, :], func=AF.Sigmoid), reads=[plt[3]], writes=[sg])
                proj_group(0)
                bw = nb(); ba = nb(); bg = nb()
                for jc in range(4):
                    S.op('pe', lambda e, jc=jc: e.matmul(bw[:, jc * 128:(jc + 1) * 128], lhsT=lrb[0:64, jc * 128:(jc + 1) * 128],
                                                         rhs=lr12[0:64, :], start=True, stop=True), reads=[lrb, lr12], writes=[bw])
                for jc in range(4):
                    S.op('pe', lambda e, jc=jc: e.matmul(ba[:, jc * 128:(jc + 1) * 128], lhsT=lrb[64:128, jc * 128:(jc + 1) * 128],
                                                         rhs=lr12[64:128, :], start=True, stop=True), reads=[lrb, lr12], writes=[ba])
                for jc in range(4):
                    S.op('pe', lambda e, jc=jc: e.matmul(bg[:, jc * 128:(jc + 1) * 128], lhsT=lrb[:, 512 + jc * 128:512 + (jc + 1) * 128],
                                                         rhs=sg[:, :], start=True, stop=True), reads=[lrb, sg], writes=[bg])
                proj_group(2)
                if _stop(3):
                    return
                S.op('dve', lambda e: e.tensor_tensor(out=t0[:, :, :], in0=v3(bw, 4), in1=cb(W0, 4, [128, 4, 128]), op=ALU.add),
                     reads=[bw, cst], writes=[t0])
                S.op('act', lambda e: e.activation(out=sig[:, :, :], in_=t0[:, :, :], func=AF.Sigmoid), reads=[t0], writes=[sig])
                S.op('dve', lambda e: e.tensor_tensor(out=t2[:, :, :], in0=v3(ba, 4), in1=cb(A0, 4, [128, 4, 128]), op=ALU.add),
                     reads=[ba, cst], writes=[t2])
                S.op('act', lambda e: e.activation(out=al[:, :, :], in_=t2[:, :, :], func=AF.Sigmoid), reads=[t2], writes=[al])
                S.op('act', lambda e: e.copy(out=gT[:, :, :], in_=v3(bg, 4)), reads=[bg], writes=[gT])
                for jc in range(4):
                    S.op('dve', lambda e, jc=jc: e.tensor_tensor_scan(out=cs[:, jc, :], data0=ones[:, :], data1=sig[:, jc, :],
                                                                      initial=0.0, op0=ALU.mult, op1=ALU.add),
                         reads=[ones, sig], writes=[cs])
                S.op('pool', lambda e: e.tensor_tensor(out=csx[:, :, :], in0=cs[:, :, :], in1=sig[:, :, :], op=ALU.subtract),
                     reads=[cs, sig], writes=[csx])
                S.op('act', lambda e: e.activation(out=Epos[:, :, :], in_=cs[:, :, :], func=AF.Exp, scale=-C0), reads=[cs], writes=[Epos])
                S.op('act', lambda e: e.activation(out=Eneg[:, :, :], in_=cs[:, :, :], func=AF.Exp, scale=C0), reads=[cs], writes=[Eneg])
                S.op('act', lambda e: e.activation(out=Em[:, :, :], in_=csx[:, :, :], func=AF.Exp, scale=-C0), reads=[csx], writes=[Em])
                S.op('dve', lambda e: e.tensor_tensor(out=t1[:, :, :], in0=cs[:, :, 127:128].to_broadcast([128, 4, 128]),
                                                      in1=cs[:, :, :], op=ALU.subtract), reads=[cs], writes=[t1])
                S.op('act', lambda e: e.activation(out=Ehat[:, :, :], in_=t1[:, :, :], func=AF.Exp, scale=-C0), reads=[t1], writes=[Ehat])
                S.op('pool', lambda e: e.tensor_tensor(out=kq[:, :, :], in0=pl[:, 4:8, :], in1=cb(KK, 4, [128, 4, 128]), op=ALU.mult),
                     reads=[plt[1], cst], writes=[kq])
                S.op('pool', lambda e: e.tensor_tensor(out=sqb[:, :, :], in0=kq[:, :, :], in1=kq[:, :, :], op=ALU.mult),
                     reads=[kq], writes=[sqb])
                bs = nb()
                for jc in range(4):
                    S.op('pe', lambda e, jc=jc: e.matmul(bs[:, jc * 128:(jc + 1) * 128], lhsT=bones, rhs=sqb[:, jc, :],
                                                         start=True, stop=True), reads=[tabb, sqb], writes=[bs])
                S.op('dve', lambda e: e.tensor_scalar(out=t0[:, :, :], in0=v3(bs, 4), scalar1=1e-24, scalar2=None, op0=ALU.max),
                     reads=[bs], writes=[t0])
                S.op('act', lambda e: e.activation(out=t0[:, :, :], in_=t0[:, :, :], func=AF.Ln, scale=float(2.0 ** 40)), reads=[t0], writes=[t0])
                S.op('act', lambda e: e.activation(out=t0[:, :, :], in_=t0[:, :, :], func=AF.Exp, scale=-0.5, bias=20.0 * math.log(2.0)),
                     reads=[t0], writes=[t0])
                S.op('dve', lambda e: e.tensor_tensor(out=kkb[:, :, :], in0=kq[:, :, :], in1=t0[:, :, :], op=ALU.mult),
                     reads=[kq, t0], writes=[kkb])
                S.op('pool', lambda e: e.tensor_tensor(out=t2[:, :, :], in0=al[:, :, :], in1=cb(KA, 4, [128, 4, 128]), op=ALU.mult),
                     reads=[al, cst], writes=[t2])
                S.op('pool', lambda e: e.tensor_tensor(out=t2[:, :, :], in0=t2[:, :, :],
                                                       in1=omka[:, :].unsqueeze(2).to_broadcast([128, 4, 128]), op=ALU.add),
                     reads=[t2, omka], writes=[t2])
                S.op('dve', lambda e: e.tensor_tensor(out=kp[:, :, :], in0=pl[:, 4:8, :], in1=t2[:, :, :], op=ALU.mult),
                     reads=[plt[1], t2], writes=[kp])
                if _stop(4):
                    return
                S.op('pool', lambda e: e.tensor_tensor(out=t1[:, :, :], in0=kkb[:, :, :], in1=Em[:, :, :], op=ALU.mult),
                     reads=[kkb, Em], writes=[t1])
                S.op('act', lambda e: e.activation(out=SCin[:, :, 0, :], in_=t1[:, :, :], func=AF.Identity, scale=-1.0),
                     reads=[t1], writes=[SCin])
                S.op('pool', lambda e: e.tensor_tensor(out=bv[:, :, :], in0=kkb[:, :, :], in1=al[:, :, :], op=ALU.mult),
                     reads=[kkb, al], writes=[bv])
                S.op('dve', lambda e: e.tensor_tensor(out=BtT[:, :, :], in0=bv[:, :, :], in1=Eneg[:, :, :], op=ALU.mult),
                     reads=[bv, Eneg], writes=[BtT])
                S.op('pool', lambda e: e.tensor_tensor(out=KtT[:, :, :], in0=kp[:, :, :], in1=Eneg[:, :, :], op=ALU.mult),
                     reads=[kp, Eneg], writes=[KtT])
                S.op('dve', lambda e: e.tensor_tensor(out=SCin[:, :, 1, :], in0=pl[:, 0:4, :], in1=Epos[:, :, :], op=ALU.mult),
                     reads=[plt[0], Epos], writes=[SCin])
                S.op('pool', lambda e: e.tensor_tensor(out=BhT[:, :, :], in0=bv[:, :, :], in1=Ehat[:, :, :], op=ALU.mult),
                     reads=[bv, Ehat], writes=[BhT])
                S.op('dve', lambda e: e.tensor_tensor(out=KhT[:, :, :], in0=kp[:, :, :], in1=Ehat[:, :, :], op=ALU.mult),
                     reads=[kp, Ehat], writes=[KhT])
                S.op('act', lambda e: e.copy(out=vTb[:, :, :], in_=pl[:, 8:12, :]), reads=[plt[2]], writes=[vTb])
                if _stop(4.3):
                    return
                bt1 = nb(); bt2 = nb()
                for jc in range(4):
                    S.op('pe', lambda e, jc=jc: e.transpose(out=vbf(bt1)[:, jc * 128:(jc + 1) * 128], in_=SCin[:, jc, 0, :], identity=ident),
                         reads=[SCin, tabb], writes=[bt1])
                    S.op('pe', lambda e, jc=jc: e.transpose(out=vbf(bt1)[:, 512 + jc * 128:512 + (jc + 1) * 128], in_=vTb[:, jc, :], identity=ident),
                         reads=[vTb, tabb], writes=[bt1])
                    S.op('pe', lambda e, jc=jc: e.transpose(out=vbf(bt2)[:, jc * 128:(jc + 1) * 128], in_=BhT[:, jc, :], identity=ident),
                         reads=[BhT, tabb], writes=[bt2])
                    S.op('pe', lambda e, jc=jc: e.transpose(out=vbf(bt2)[:, 512 + jc * 128:512 + (jc + 1) * 128], in_=KhT[:, jc, :], identity=ident),
                         reads=[KhT, tabb], writes=[bt2])
                if _stop(4.6):
                    return
                b1v = vbf(bt1).rearrange("p (k a h c) -> p k a h c", k=2, a=4, h=2)
                S.op('act', lambda e: e.copy(out=Xs[:, :, :, 0:64], in_=b1v[:, 0, :, :, :]), reads=[bt1], writes=Xt)
                if _stop(4.7):
                    return
                S.op('dve', lambda e: e.tensor_copy(out=Vp[:, :, :], in_=vbf(bt1)[:, 512:1024].rearrange("p (a b) -> p a b", a=4)),
                     reads=[bt1], writes=[Vp])
                if _stop(4.8):
                    return
                for hp in range(2):
                    S.op('act', lambda e, hp=hp: e.copy(out=VZ[:, :, hp, hp * 64:(hp + 1) * 64], in_=b1v[:, 1, :, hp, :]),
                         reads=[bt1], writes=[VZ])
                if _stop(4.85):
                    return
                S.op('dve', lambda e: e.tensor_copy(out=BH[:, :, :], in_=vbf(bt2)[:, 0:512].rearrange("p (a b) -> p a b", a=4)),
                     reads=[bt2], writes=[BH])
                if _stop(4.9):
                    return
                S.op('act', lambda e: e.copy(out=KH[:, :, :], in_=vbf(bt2)[:, 512:1024].rearrange("p (a b) -> p a b", a=4)),
                     reads=[bt2], writes=[KH])
                if _stop(5):
                    return
                b3 = [None, None]
                for jc in range(4):
                    for hp in range(2):
                        pb = 64 * hp; h = 2 * jc + hp
                        if h % 4 == 0:
                            b3 = [nb(), nb()]
                        bk = nb()
                        rhs2 = SCin[pb:pb + 64, jc, :, :].rearrange("p a b -> p (a b)")
                        S.op('pe', lambda e, bk=bk, jc=jc, pb=pb, rhs2=rhs2: e.matmul(bk[:, 0:256], lhsT=BtT[pb:pb + 64, jc, :], rhs=rhs2,
                                                                                      start=True, stop=True), reads=[BtT, SCin], writes=[bk])
                        S.op('pe', lambda e, bk=bk, jc=jc, pb=pb, rhs2=rhs2: e.matmul(bk[:, 256:512], lhsT=KtT[pb:pb + 64, jc, :], rhs=rhs2,
                                                                                      start=True, stop=True), reads=[KtT, SCin], writes=[bk])
                        if hp == 0:
                            S.op('dve', lambda e, bk=bk, jc=jc, hp=hp: e.tensor_tensor(out=SCT[:, jc, hp, :], in0=bk[:, :], in1=tabf[:, T_MP:T_MP + 512],
                                                                                       op=ALU.mult), reads=[bk, tabf], writes=[SCT])
                        else:
                            tS = tmpS[jc % 2]
                            S.op('act', lambda e, bk=bk, tS=tS: e.copy(out=tS[:, :], in_=bk[:, :]), reads=[bk], writes=[tS])
                            S.op('pool', lambda e, tS=tS, jc=jc, hp=hp: e.tensor_tensor(out=SCT[:, jc, hp, :], in0=tS[:, :], in1=tabb[:, T_MP:T_MP + 512],
                                                                                        op=ALU.mult), reads=[tS, tabb], writes=[SCT])
                        b3k = b3[hp]
                        S.op('pe', lambda e, b3k=b3k, jc=jc, pb=pb: e.matmul(b3k[:, (jc % 2) * 128:(jc % 2 + 1) * 128], lhsT=SCin[pb:pb + 64, jc, 0, :],
                                                                             rhs=BtT[pb:pb + 64, jc, :], start=True, stop=True),
                             reads=[SCin, BtT], writes=[b3k])
                        if h % 4 == 3:
                            for hq in range(2):
                                S.op('dve', lambda e, g=h // 4, hq=hq, b3g=b3[hq]: e.tensor_tensor(
                                    out=MN[:, 2 * g:2 * g + 2, hq, 0:128],
                                    in0=b3g.t[:, 0:256].rearrange("p (a c) -> p a c", a=2),
                                    in1=tabf[:, T_M0:T_M0 + 128].unsqueeze(1).to_broadcast([128, 2, 128]), op=ALU.mult),
                                    reads=[b3[hq], tabf], writes=[MNt[2 * (h // 4)], MNt[2 * (h // 4) + 1]])
                if _stop(6):
                    return
                for g in range(2):
                    bk = nb()
                    for hh in range(4):
                        jc = 2 * g + hh // 2; hp = hh % 2
                        S.op('pe', lambda e, bk=bk, hh=hh, jc=jc, hp=hp: e.matmul(bk[:, hh * 128:(hh + 1) * 128], lhsT=SCT[:, jc, hp, 256:384],
                                                                                  rhs=Vp[:, jc, :], start=True, stop=True), reads=[SCT, Vp], writes=[bk])
                    bvw = bk.t[:, :].rearrange("p (a h g c) -> p a h g c", a=2, h=2, g=2)
                    for hp in range(2):
                        S.op('act', lambda e, bvw=bvw, g=g, hp=hp: e.copy(out=Xs[:, 2 * g:2 * g + 2, hp, 64:128], in_=bvw[:, :, hp, hp, :]),
                             reads=[bk], writes=[Xt[2 * g], Xt[2 * g + 1]])
                if _stop(7):
                    return
                for j in range(7):
                    for jc in range(4):
                        bP = nb(); bQ = nb() if j < 6 else None
                        for hp in range(2):
                            Nj = SCT[:, jc, hp, 0:128] if j == 0 else MN[:, jc, hp, 128:256]
                            nsrc = [SCT] if j == 0 else []
                            S.op('pe', lambda e, bP=bP, hp=hp, jc=jc, Nj=Nj: e.matmul(bP[:, hp * 128:(hp + 1) * 128], lhsT=Nj, rhs=Xs[:, jc, hp, :],
                                                                                     start=True, stop=True), reads=nsrc + [Xt[jc], MNt[jc]], writes=[bP])
                            if j < 6:
                                S.op('pe', lambda e, bQ=bQ, hp=hp, jc=jc, Nj=Nj: e.matmul(bQ[:, hp * 256:hp * 256 + 128], lhsT=Nj, rhs=MN[:, jc, hp, 0:128],
                                                                                         start=True, stop=True), reads=nsrc + [MNt[jc]], writes=[bQ])
                                S.op('pe', lambda e, bQ=bQ, hp=hp, jc=jc, Nj=Nj: e.matmul(bQ[:, hp * 256 + 128:hp * 256 + 256], lhsT=MN[:, jc, hp, 0:128], rhs=Nj,
                                                                                         start=True, stop=True), reads=nsrc + [MNt[jc]], writes=[bQ])
                        S.op('dve', lambda e, jc=jc, bP=bP: e.tensor_tensor(out=Xs[:, jc, :, :], in0=Xs[:, jc, :, :],
                                                                            in1=bP.t[:, 0:256].rearrange("p (h c) -> p h c", h=2), op=ALU.add),
                             reads=[Xt[jc], bP], writes=[Xt[jc]])
                        if j < 6:
                            S.op('act', lambda e, jc=jc, bQ=bQ: e.copy(out=MN[:, jc, :, :], in_=bQ.t[:, :].rearrange("p (h c) -> p h c", h=2)),
                                 reads=[bQ], writes=[MNt[jc]])
                if _stop(8):
                    return
                for hp in range(2):
                    S.op('act', lambda e, hp=hp: e.copy(out=AZ[:, :, hp, hp * 64:(hp + 1) * 64], in_=Xs[:, :, hp, 0:64]), reads=Xt, writes=[AZ])
                    S.op('pool', lambda e, hp=hp: e.tensor_copy(out=UVZ[:, :, hp, hp * 64:(hp + 1) * 64], in_=Xs[:, :, hp, 64:128]), reads=Xt, writes=[UVZ])
                S.op('pool', lambda e: e.tensor_copy(out=Ap[:, :, :].rearrange("p a (h c) -> p a h c", h=2), in_=Xs[:, :, :, 0:64]), reads=Xt, writes=[Ap])
                S.op('dve', lambda e: e.tensor_copy(out=UVp[:, :, :].rearrange("p a (h c) -> p a h c", h=2), in_=Xs[:, :, :, 64:128]), reads=Xt, writes=[UVp])
                bR = nb(); bG = nb()
                for jc in range(4):
                    for hp in range(2):
                        S.op('pe', lambda e, jc=jc, hp=hp: e.matmul(bR[:, jc * 128:(jc + 1) * 128], lhsT=AZ[:, jc, hp, :], rhs=SCT[:, jc, hp, 128:256],
                                                                    start=(hp == 0), stop=(hp == 1)), reads=[AZ, SCT], writes=[bR])
                for jc in range(4):
                    S.op('pe', lambda e, jc=jc: e.matmul(bG[:, jc * 128:(jc + 1) * 128], lhsT=Ap[:, jc, :], rhs=BH[:, jc, :], start=True, stop=True),
                         reads=[Ap, BH], writes=[bG])
                S.op('dve', lambda e: e.tensor_tensor(out=RhT[:, :, :], in0=v3(bR, 4), in1=SCin[:, :, 1, :], op=ALU.add), reads=[bR, SCin], writes=[RhT])
                S.op('dve', lambda e: e.tensor_tensor(out=GZ[:, :, :], in0=v3(bG, 4), in1=bmf, op=ALU.mult), reads=[bG, tabf], writes=[GZ])
                if _stop(9):
                    return
                bY = nb(); bH = nb()
                for jc in range(4):
                    for hp in range(2):
                        S.op('pe', lambda e, jc=jc, hp=hp: e.matmul(bY[:, jc * 128:(jc + 1) * 128], lhsT=UVZ[:, jc, hp, :], rhs=SCT[:, jc, hp, 128:256],
                                                                    start=(hp == 0), stop=False), reads=[UVZ, SCT], writes=[bY])
                        S.op('pe', lambda e, jc=jc, hp=hp: e.matmul(bY[:, jc * 128:(jc + 1) * 128], lhsT=VZ[:, jc, hp, :], rhs=SCT[:, jc, hp, 384:512],
                                                                    start=False, stop=False), reads=[VZ, SCT], writes=[bY])
                    S.op('pe', lambda e, jc=jc: e.matmul(bY[:, jc * 128:(jc + 1) * 128], lhsT=HZ[:, jc, :], rhs=RhT[:, jc, :], start=False, stop=True),
                         reads=[HZ, RhT], writes=[bY])
                for jc in range(4):
                    S.op('pe', lambda e, jc=jc: e.matmul(bH[:, jc * 128:(jc + 1) * 128], lhsT=BH[:, jc, :], rhs=UVp[:, jc, :], start=True, stop=False),
                         reads=[BH, UVp], writes=[bH])
                    S.op('pe', lambda e, jc=jc: e.matmul(bH[:, jc * 128:(jc + 1) * 128], lhsT=KH[:, jc, :], rhs=Vp[:, jc, :], start=False, stop=False),
                         reads=[KH, Vp], writes=[bH])
                    S.op('pe', lambda e, jc=jc: e.matmul(bH[:, jc * 128:(jc + 1) * 128], lhsT=GZ[:, jc, :], rhs=HZ[:, jc, :], start=False, stop=True),
                         reads=[GZ, HZ], writes=[bH])
                for jc in range(4):
                    S.op('dve', lambda e, jc=jc: e.scalar_tensor_tensor(out=Hf[:, jc, :], in0=Hf[:, jc, :], scalar=Epos[:, jc, 127:128],
                                                                        in1=bH[:, jc * 128:(jc + 1) * 128], op0=ALU.mult, op1=ALU.add),
                         reads=[Hf, Epos, bH], writes=[Hf])
                S.op('pool', lambda e: e.tensor_tensor(out=Hf[:, :, :], in0=Hf[:, :, :], in1=bmf, op=ALU.mult), reads=[Hf, tabf], writes=[Hf])
                S.op('act', lambda e: e.copy(out=HZ[:, :, :], in_=Hf[:, :, :]), reads=[Hf], writes=[HZ])
                if _stop(10):
                    return
                S.op('act', lambda e: e.copy(out=yb[:, :, :], in_=v3(bY, 4)), reads=[bY], writes=[yb])
                S.op('act', lambda e: e.activation(out=ysq[:, :, :], in_=v3(bY, 4), func=AF.Square), reads=[bY], writes=[ysq])
                S.op('pool', lambda e: e.tensor_tensor(out=t0[:, :, :], in0=pl[:, 0:4, :], in1=kp[:, :, :], op=ALU.mult), reads=[plt[0], kp], writes=[t0])
                S.op('pool', lambda e: e.tensor_tensor(out=rkb[:, :, :], in0=t0[:, :, :], in1=cb(RK, 4, [128, 4, 128]), op=ALU.mult),
                     reads=[t0, cst], writes=[rkb])
                bM = nb(); bQ = nb(); bO = nb()
                for jc in range(4):
                    S.op('pe', lambda e, jc=jc: e.matmul(bM[:, jc * 128:(jc + 1) * 128], lhsT=bones, rhs=yb[:, jc, :], start=True, stop=True),
                         reads=[tabb, yb], writes=[bM])
                    S.op('pe', lambda e, jc=jc: e.matmul(bQ[:, jc * 128:(jc + 1) * 128], lhsT=bones, rhs=ysq[:, jc, :], start=True, stop=True),
                         reads=[tabb, ysq], writes=[bQ])
                    S.op('pe', lambda e, jc=jc: e.matmul(bO[:, jc * 128:(jc + 1) * 128], lhsT=bones, rhs=rkb[:, jc, :], start=True, stop=True),
                         reads=[tabb, rkb], writes=[bO])
                S.op('act', lambda e: e.activation(out=t1[:, :, :], in_=v3(bM, 4), func=AF.Identity, scale=1.0 / 64), reads=[bM], writes=[t1])
                S.op('pool', lambda e: e.tensor_tensor(out=t2[:, :, :], in0=t1[:, :, :], in1=t1[:, :, :], op=ALU.mult), reads=[t1], writes=[t2])
                S.op('dve', lambda e: e.scalar_tensor_tensor(out=t2[:, :, :], in0=v3(bQ, 4), scalar=1.0 / 64, in1=t2[:, :, :],
                                                             op0=ALU.mult, op1=ALU.subtract), reads=[bQ, t2], writes=[t2])
                S.op('act', lambda e: e.activation(out=t2[:, :, :], in_=t2[:, :, :], func=AF.Ln, bias=64e-5), reads=[t2], writes=[t2])
                S.op('act', lambda e: e.activation(out=t2[:, :, :], in_=t2[:, :, :], func=AF.Exp, scale=-0.5), reads=[t2], writes=[t2])
                S.op('dve', lambda e: e.tensor_tensor(out=t0[:, :, :], in0=v3(bY, 4), in1=t1[:, :, :], op=ALU.subtract), reads=[bY, t1], writes=[t0])
                S.op('pool', lambda e: e.tensor_tensor(out=t0[:, :, :], in0=t0[:, :, :], in1=t2[:, :, :], op=ALU.mult), reads=[t0, t2], writes=[t0])
                S.op('pool', lambda e: e.tensor_tensor(out=t0[:, :, :], in0=t0[:, :, :], in1=cb(LW, 4, [128, 4, 128]), op=ALU.mult),
                     reads=[t0, cst], writes=[t0])
                S.op('pool', lambda e: e.tensor_tensor(out=t0[:, :, :], in0=t0[:, :, :], in1=cb(LB, 4, [128, 4, 128]), op=ALU.add),
                     reads=[t0, cst], writes=[t0])
                S.op('dve', lambda e: e.tensor_tensor(out=t1[:, :, :], in0=v3(bO, 4), in1=pl[:, 8:12, :], op=ALU.mult), reads=[bO, plt[2]], writes=[t1])
                S.op('pool', lambda e: e.tensor_tensor(out=t0[:, :, :], in0=t0[:, :, :], in1=t1[:, :, :], op=ALU.add), reads=[t0, t1], writes=[t0])
                S.op('pool', lambda e: e.tensor_tensor(out=yrwT[:, :, :], in0=t0[:, :, :], in1=gT[:, :, :], op=ALU.mult), reads=[t0, gT], writes=[yrwT])
                S.dma('pool', lambda e, ci=ci: e.dma_start(out=yrw_s[ci].rearrange("p (a b) -> p a b", a=4), in_=yrwT[:, :, :]), 'yst', reads=[yrwT])
            for ci in range(NCHUNK):
                _chunk(ci)
            S.barrier()

        if "B" in phases:
          with ExitStack() as st:
            NBC = INC - RWC
            w_b = S.sb(st, [128, 8, NBC], BF16)
            wbrw = S.sb(st, [128, 4, D], BF16); wbret = S.sb(st, [128, 8, D], BF16); wout = S.sb(st, [128, 8, D], BF16)
            for kc in range(8):
                S.dma('pool', lambda e, kc=kc: e.dma_start(out=w_b[:, kc, :], in_=win_d[kc * 128:(kc + 1) * 128, RWC:INC]), 'wB', writes=[w_b])
                S.dma('pool', lambda e, kc=kc: e.dma_start(out=wbret[:, kc, :], in_=wbret_d[kc * 128:(kc + 1) * 128, :]), 'wB', writes=[wbret])
                S.dma('pool', lambda e, kc=kc: e.dma_start(out=wout[:, kc, :], in_=wout_d[kc * 128:(kc + 1) * 128, :]), 'wB', writes=[wout])
            for kc in range(4):
                S.dma('pool', lambda e, kc=kc: e.dma_start(out=wbrw[:, kc, :], in_=wbrw_d[kc * 128:(kc + 1) * 128, :]), 'wB', writes=[wbrw])
            _xb = S.sb(st, [128, D], F32); xs2 = [_xb, _xb]
            hnT2 = [S.sb(st, [128, 8, 128], BF16) for _ in range(2)]
            yrw2 = [S.sb(st, [128, 4, 128], BF16) for _ in range(2)]
            posi2 = [S.sb(st, [128, 128], I32) for _ in range(2)]
            posf = S.sb(st, [128, 128], F32); u0 = S.sb(st, [128, 128], F32); u1 = S.sb(st, [128, 128], F32)
            ti = S.sb(st, [128, 128], I32); tf = S.sb(st, [128, 128], F32)
            cosT = S.sb(st, [128, 128], F32); sinT = S.sb(st, [128, 128], F32)
            qk = S.sb(st, [128, 8, 128], F32); qkb = S.sb(st, [128, 8, 128], BF16)
            r1 = S.sb(st, [128, 8, 128], F32); r2 = S.sb(st, [128, 8, 128], F32)
            rot = S.sb(st, [128, 8, 128], BF16); qd = S.sb(st, [128, 4, 128], BF16)
            v_bf = S.sb(st, [128, D], BF16); sgb = S.sb(st, [128, D], BF16); sA = S.sb(st, [128, D], BF16); sB = S.sb(st, [128, D], BF16)
            kdZ = S.sb(st, [128, 4, 2, 128], BF16)
            sT = S.sb(st, [128, 8, 128], BF16)
            Rf = S.sb(st, [128, 4, 128], F32); Rb = S.sb(st, [128, 4, 128], BF16)
            of = qk; osq = r1
            stt = S.sb(st, [128, 16], F32); mean = S.sb(st, [128, 8], F32); var = S.sb(st, [128, 8], F32)
            yret = S.sb(st, [128, D], BF16); yretT = S.sb(st, [128, 8, 128], BF16)
            m1 = S.sb(st, [128, D], F32); m2 = S.sb(st, [128, D], F32); mg = S.sb(st, [128, D], BF16); mT = S.sb(st, [128, 8, 128], BF16)
            mo = m2; junk = mg; ss = S.sb(st, [128, 1], F32); rs = S.sb(st, [128, 1], F32)
            hh = m1
            S.op('pool', lambda e: e.memset(kdZ[:, :, :, :], 0.0), writes=[kdZ])
            DBG.update({k_: v_.name for k_, v_ in list(locals().items()) if isinstance(v_, Buf)})

            def _chunk(ci):
                b_i, c_i = divmod(ci, NCH)
                tok0 = ci * 128
                xs = xs2[ci % 2]; hnT = hnT2[ci % 2]; yrw = yrw2[ci % 2]; posi = posi2[ci % 2]
                S.dma('sp', lambda e, hnT=hnT, ci=ci: e.dma_start(out=hnT[:, :, :], in_=hnT_s[ci].rearrange("p (a b) -> p a b", a=8)),
                      'hB%d' % (ci % 2), writes=[hnT])
                S.dma('sp', lambda e, yrw=yrw, ci=ci: e.dma_start(out=yrw[:, :, :], in_=yrw_s[ci].rearrange("p (a b) -> p a b", a=4)),
                      'yB%d' % (ci % 2), writes=[yrw])
                S.dma('sp', lambda e, posi=posi, tok0=tok0: e.dma_start(out=posi[:, :], in_=pos_d[0:1, tok0:tok0 + 128].partition_broadcast(128)),
                      'pB%d' % (ci % 2), writes=[posi])
                S.dma('sp', lambda e, xs=xs, tok0=tok0: e.dma_start(out=xs[:, :], in_=x_d[tok0:tok0 + 128, :]), 'xB%d' % (ci % 2), writes=[xs])
                if c_i == 0:
                    S.op('pool', lambda e: e.memset(Rf[:, :, :], 0.0), writes=[Rf])
                    S.op('pool', lambda e: e.memset(Rb[:, :, :], 0.0), writes=[Rb])
                S.op('dve', lambda e, posi=posi: e.tensor_copy(out=posf[:, :], in_=posi[:, :]), reads=[posi], writes=[posf])
                S.op('dve', lambda e: e.tensor_scalar(out=u0[:, :], in0=posf[:, :], scalar1=cst[:, INV:INV + 1], scalar2=None, op0=ALU.mult),
                     reads=[posf, cst], writes=[u0])
                S.op('dve', lambda e: e.tensor_copy(out=ti[:, :], in_=u0[:, :]), reads=[u0], writes=[ti])
                S.op('dve', lambda e: e.tensor_copy(out=tf[:, :], in_=ti[:, :]), reads=[ti], writes=[tf])
                S.op('dve', lambda e: e.tensor_tensor(out=tf[:, :], in0=u0[:, :], in1=tf[:, :], op=ALU.subtract), reads=[u0, tf], writes=[tf])
                S.op('act', lambda e: e.activation(out=sinT[:, :], in_=tf[:, :], func=AF.Sin, scale=cst[:, SSC:SSC + 1]), reads=[tf, cst], writes=[sinT])
                S.op('pool', lambda e: e.tensor_scalar(out=u1[:, :], in0=u0[:, :], scalar1=0.25, scalar2=None, op0=ALU.add), reads=[u0], writes=[u1])
                S.op('dve', lambda e: e.tensor_copy(out=ti[:, :], in_=u1[:, :]), reads=[u1], writes=[ti])
                S.op('dve', lambda e: e.tensor_copy(out=tf[:, :], in_=ti[:, :]), reads=[ti], writes=[tf])
                S.op('dve', lambda e: e.tensor_tensor(out=tf[:, :], in0=u1[:, :], in1=tf[:, :], op=ALU.subtract), reads=[u1, tf], writes=[tf])
                S.op('act', lambda e: e.activation(out=cosT[:, :], in_=tf[:, :], func=AF.Sin, scale=2.0 * math.pi), reads=[tf], writes=[cosT])
                for g in range(2):
                    bk = nb()
                    for jj in range(4):
                        j = g * 4 + jj
                        for kc in range(8):
                            S.op('pe', lambda e, bk=bk, jj=jj, j=j, kc=kc, hnT=hnT: e.matmul(
                                bk[:, jj * 128:(jj + 1) * 128], lhsT=w_b[:, kc, j * 128:(j + 1) * 128], rhs=hnT[:, kc, :],
                                start=(kc == 0), stop=(kc == 7)), reads=[w_b, hnT], writes=[bk])
                    S.op('act', lambda e, bk=bk, g=g: e.copy(out=qk[:, g * 4:g * 4 + 4, :], in_=v3(bk, 4)), reads=[bk], writes=[qk])
                S.op('pool', lambda e: e.tensor_copy(out=qkb[:, :, :], in_=qk[:, :, :]), reads=[qk], writes=[qkb])
                for grp in range(4):
                    for half in range(2):
                        bk = nb(); c0 = 1024 + grp * 1024 + half * 512
                        for kc in range(8):
                            S.op('pe', lambda e, bk=bk, kc=kc, c0=c0, hnT=hnT: e.matmul(bk[:, :], lhsT=hnT[:, kc, :], rhs=w_b[:, kc, c0:c0 + 512],
                                                                                        start=(kc == 0), stop=(kc == 7)), reads=[w_b, hnT], writes=[bk])
                        dst = (v_bf, sgb, sA, sB)[grp]
                        fn = (AF.Copy, AF.Silu, AF.Sigmoid, AF.Sigmoid)[grp]
                        S.op('act', lambda e, bk=bk, dst=dst, fn=fn, half=half: e.activation(out=dst[:, half * 512:(half + 1) * 512], in_=bk[:, :], func=fn),
                             reads=[bk], writes=[dst])
                bsw = [nb(), nb()]
                for j in range(8):
                    S.op('pe', lambda e, j=j: e.matmul(bsw[j // 4][:, (j % 4) * 128:(j % 4 + 1) * 128], lhsT=pswap, rhs=qkb[:, j, :], start=True, stop=True),
                         reads=[tabb, qkb], writes=[bsw[j // 4]])
                S.op('pool', lambda e: e.tensor_tensor(out=r1[:, :, :], in0=qk[:, :, :], in1=cosT[:, :].unsqueeze(1).to_broadcast([128, 8, 128]), op=ALU.mult),
                     reads=[qk, cosT], writes=[r1])
                for g in range(2):
                    S.op('dve', lambda e, g=g: e.tensor_tensor(out=r2[:, g * 4:g * 4 + 4, :], in0=v3(bsw[g], 4),
                                                               in1=sinT[:, :].unsqueeze(1).to_broadcast([128, 4, 128]), op=ALU.mult),
                         reads=[bsw[g], sinT], writes=[r2])
                S.op('dve', lambda e: e.tensor_tensor(out=rot[:, :, :], in0=r1[:, :, :], in1=r2[:, :, :], op=ALU.add), reads=[r1, r2], writes=[rot])
                S.op('pool', lambda e: e.tensor_tensor(out=qd[:, :, :], in0=rot[:, 0:4, :], in1=cst[:, QD:QD + 512].rearrange("p (a b) -> p a b", a=4), op=ALU.mult),
                     reads=[rot, cst], writes=[qd])
                bk = nb()
                for jc in range(4):
                    S.op('pe', lambda e, bk=bk, jc=jc: e.transpose(out=vbf(bk)[:, jc * 128:(jc + 1) * 128], in_=rot[:, 4 + jc, :], identity=ident),
                         reads=[rot, tabb], writes=[bk])
                bkv = vbf(bk)[:, 0:512].rearrange("p (a h c) -> p a h c", a=4, h=2)
                kdv = cst[:, KDEC:KDEC + 8].rearrange("p (a h) -> p a h", a=4)
                for hp in range(2):
                    S.op('dve', lambda e, hp=hp, bkv=bkv: e.tensor_tensor(out=kdZ[:, :, hp, hp * 64:(hp + 1) * 64], in0=bkv[:, :, hp, :],
                                                                          in1=kdv[:, :, hp:hp + 1].to_broadcast([128, 4, 64]), op=ALU.mult),
                         reads=[bk, cst], writes=[kdZ])
                for hp in range(2):
                    bk = nb(); pb = 64 * hp
                    for jc in range(4):
                        S.op('pe', lambda e, bk=bk, jc=jc, pb=pb: e.matmul(bk[:, jc * 128:(jc + 1) * 128], lhsT=rot[pb:pb + 64, 4 + jc, :],
                                                                           rhs=rot[pb:pb + 64, jc, :], start=True, stop=True), reads=[rot], writes=[bk])
                    S.op('dve', lambda e, bk=bk, hp=hp: e.tensor_tensor(
                        out=sT[:, :, :].rearrange("p (a h) c -> p a h c", h=2)[:, :, hp, :], in0=v3(bk, 4),
                        in1=tabf[:, T_MT:T_MT + 1024].rearrange("p (a h c) -> p a h c", a=4, h=2)[:, :, hp, :], op=ALU.mult),
                         reads=[bk, tabf], writes=[sT])
                bo = [nb(), nb()]
                for h in range(8):
                    jc = h // 2; pb = 64 * (h % 2); bk = bo[h // 4]; cc = (h % 4) * 128
                    S.op('pe', lambda e, bk=bk, cc=cc, h=h: e.matmul(bk[:, cc:cc + 128], lhsT=sT[:, h, :], rhs=v_bf[:, h * 128:(h + 1) * 128],
                                                                     start=True, stop=False), reads=[sT, v_bf], writes=[bk])
                    S.op('pe', lambda e, bk=bk, cc=cc, jc=jc, pb=pb: e.matmul(bk[:, cc:cc + 128], lhsT=qd[pb:pb + 64, jc, :], rhs=Rb[pb:pb + 64, jc, :],
                                                                              start=False, stop=True), reads=[qd, Rb], writes=[bk])
                for g in range(2):
                    S.op('act', lambda e, g=g: e.copy(out=of[:, g * 4:g * 4 + 4, :], in_=v3(bo[g], 4)), reads=[bo[g]], writes=[of])
                bR = nb()
                for jc in range(4):
                    for hp in range(2):
                        h = 2 * jc + hp
                        S.op('pe', lambda e, jc=jc, hp=hp, h=h: e.matmul(bR[:, jc * 128:(jc + 1) * 128], lhsT=kdZ[:, jc, hp, :], rhs=v_bf[:, h * 128:(h + 1) * 128],
                                                                         start=(hp == 0), stop=(hp == 1)), reads=[kdZ, v_bf], writes=[bR])
                S.op('pool', lambda e: e.tensor_tensor(out=Rf[:, :, :], in0=Rf[:, :, :], in1=cb(CD, 4, [128, 4, 128]), op=ALU.mult), reads=[Rf, cst], writes=[Rf])
                S.op('dve', lambda e: e.tensor_tensor(out=Rf[:, :, :], in0=Rf[:, :, :], in1=v3(bR, 4), op=ALU.add), reads=[Rf, bR], writes=[Rf])
                S.op('act', lambda e: e.copy(out=Rb[:, :, :], in_=Rf[:, :, :]), reads=[Rf], writes=[Rb])
                S.op('dve', lambda e: e.tensor_reduce(out=stt[:, 0:8], in_=of[:, :, :], axis=AX.X, op=ALU.add), reads=[of], writes=[stt])
                S.op('pool', lambda e: e.tensor_tensor(out=osq[:, :, :], in0=of[:, :, :], in1=of[:, :, :], op=ALU.mult), reads=[of], writes=[osq])
                S.op('dve', lambda e: e.tensor_reduce(out=stt[:, 8:16], in_=osq[:, :, :], axis=AX.X, op=ALU.add), reads=[osq], writes=[stt])
                S.op('dve', lambda e: e.tensor_scalar(out=mean[:, :], in0=stt[:, 0:8], scalar1=1.0 / 128, scalar2=None, op0=ALU.mult), reads=[stt], writes=[mean])
                S.op('dve', lambda e: e.tensor_tensor(out=var[:, :], in0=mean[:, :], in1=mean[:, :], op=ALU.mult), reads=[mean], writes=[var])
                S.op('dve', lambda e: e.scalar_tensor_tensor(out=var[:, :], in0=stt[:, 8:16], scalar=1.0 / 128, in1=var[:, :], op0=ALU.mult, op1=ALU.subtract),
                     reads=[stt, var], writes=[var])
                S.op('act', lambda e: e.activation(out=var[:, :], in_=var[:, :], func=AF.Sqrt, bias=1e-5), reads=[var], writes=[var])
                S.op('dve', lambda e: e.reciprocal(out=var[:, :], in_=var[:, :]), reads=[var], writes=[var])
                S.op('dve', lambda e: e.tensor_tensor(out=of[:, :, :], in0=of[:, :, :], in1=mean[:, :].unsqueeze(2).to_broadcast([128, 8, 128]), op=ALU.subtract),
                     reads=[of, mean], writes=[of])
                S.op('pool', lambda e: e.tensor_tensor(out=of[:, :, :], in0=of[:, :, :], in1=var[:, :].unsqueeze(2).to_broadcast([128, 8, 128]), op=ALU.mult),
                     reads=[of, var], writes=[of])
                S.op('dve', lambda e: e.tensor_tensor(out=yret[:, :], in0=of[:, :, :].rearrange("p a b -> p (a b)"), in1=sgb[:, :], op=ALU.mult),
                     reads=[of, sgb], writes=[yret])
                bk = nb()
                for kc in range(8):
                    S.op('pe', lambda e, bk=bk, kc=kc: e.transpose(out=vbf(bk)[:, kc * 128:(kc + 1) * 128], in_=yret[:, kc * 128:(kc + 1) * 128], identity=ident),
                         reads=[yret, tabb], writes=[bk])
                S.op('act', lambda e, bk=bk: e.copy(out=yretT[:, :, :], in_=vbf(bk).rearrange("p (a b) -> p a b", a=8)), reads=[bk], writes=[yretT])
                for half in range(2):
                    b1 = nb(); b2 = nb(); hs = slice(half * 512, (half + 1) * 512)
                    for jc in range(4):
                        S.op('pe', lambda e, b1=b1, jc=jc, hs=hs, yrw=yrw: e.matmul(b1[:, :], lhsT=yrw[:, jc, :], rhs=wbrw[:, jc, hs], start=(jc == 0), stop=(jc == 3)),
                             reads=[yrw, wbrw], writes=[b1])
                    for kc in range(8):
                        S.op('pe', lambda e, b2=b2, kc=kc, hs=hs: e.matmul(b2[:, :], lhsT=yretT[:, kc, :], rhs=wbret[:, kc, hs], start=(kc == 0), stop=(kc == 7)),
                             reads=[yretT, wbret], writes=[b2])
                    S.op('dve', lambda e, b1=b1, hs=hs: e.tensor_tensor(out=m1[:, hs], in0=b1[:, :], in1=sA[:, hs], op=ALU.mult), reads=[b1, sA], writes=[m1])
                    S.op('dve', lambda e, b2=b2, hs=hs: e.tensor_tensor(out=m2[:, hs], in0=b2[:, :], in1=sB[:, hs], op=ALU.mult), reads=[b2, sB], writes=[m2])
                S.op('pool', lambda e: e.tensor_tensor(out=mg[:, :], in0=m1[:, :], in1=m2[:, :], op=ALU.add), reads=[m1, m2], writes=[mg])
                bk = nb()
                for kc in range(8):
                    S.op('pe', lambda e, bk=bk, kc=kc: e.transpose(out=vbf(bk)[:, kc * 128:(kc + 1) * 128], in_=mg[:, kc * 128:(kc + 1) * 128], identity=ident),
                         reads=[mg, tabb], writes=[bk])
                S.op('act', lambda e, bk=bk: e.copy(out=mT[:, :, :], in_=vbf(bk).rearrange("p (a b) -> p a b", a=8)), reads=[bk], writes=[mT])
                for half in range(2):
                    bk = nb(); hs = slice(half * 512, (half + 1) * 512)
                    for kc in range(8):
                        S.op('pe', lambda e, bk=bk, kc=kc, hs=hs: e.matmul(bk[:, :], lhsT=mT[:, kc, :], rhs=wout[:, kc, hs], start=(kc == 0), stop=(kc == 7)),
                             reads=[mT, wout], writes=[bk])
                    S.op('act', lambda e, bk=bk, hs=hs: e.copy(out=mo[:, hs], in_=bk[:, :]), reads=[bk], writes=[mo])
                S.op('act', lambda e: e.activation(out=junk[:, :], in_=mo[:, :], func=AF.Square, accum_out=ss[:, 0:1]), reads=[mo], writes=[junk, ss])
                S.op('act', lambda e: e.activation(out=rs[:, 0:1], in_=ss[:, 0:1], func=AF.Sqrt, scale=1.0 / D, bias=1e-6), reads=[ss], writes=[rs])
                S.op('dve', lambda e: e.reciprocal(out=rs[:, 0:1], in_=rs[:, 0:1]), reads=[rs], writes=[rs])
                S.op('dve', lambda e: e.scalar_tensor_tensor(out=hh[:, :], in0=mo[:, :], scalar=rs[:, 0:1], in1=rows[:, 0, :], op0=ALU.mult, op1=ALU.mult),
                     reads=[mo, rs, rows], writes=[hh])
                S.op('pool', lambda e, xs=xs: e.tensor_tensor(out=hh[:, :], in0=hh[:, :], in1=xs[:, :], op=ALU.add), reads=[hh, xs], writes=[hh])
                S.dma('pool', lambda e, tok0=tok0: e.dma_start(out=h_s[tok0:tok0 + 128, :], in_=hh[:, :]), 'hstore', reads=[hh])
            for ci in range(NCHUNK):
                _chunk(ci)
            S.barrier()

        if "C" in phases:
          with ExitStack() as st:
            wup = S.sb(st, [128, 8, 2 * DFF], BF16); wdn = S.sb(st, [128, 22, D], BF16)
            for kc in range(8):
                S.dma('pool', lambda e, kc=kc: e.dma_start(out=wup[:, kc, :], in_=wup_d[kc * 128:(kc + 1) * 128, :]), 'wC', writes=[wup])
            for j in range(22):
                S.dma('pool', lambda e, j=j: e.dma_start(out=wdn[:, j, :], in_=wdn_d[j * 128:(j + 1) * 128, :]), 'wC', writes=[wdn])
            hb = [S.sb(st, [128, D], F32) for _ in range(NSUB)]
            tmp_bf = S.sb(st, [128, D], BF16); xn = S.sb(st, [128, D], BF16)
            ss = S.sb(st, [128, 1], F32); rs = S.sb(st, [128, 1], F32)
            hn2T = S.sb(st, [128, 8, FT], BF16)
            actT = S.sb(st, [128, 22, FT], BF16)
            ub2 = [S.sb(st, [128, FT + 2], F32) for _ in range(4)]
            acc2 = [S.sb(st, [128, FT], F32) for _ in range(4)]
            gg2 = [S.sb(st, [128, FT], F32) for _ in range(2)]
            carry = S.sb(st, [128, 44, 2], F32)
            fo = S.sb(st, [128, D], F32); oo = S.sb(st, [128, D], F32)
            ucl = [0]

            def _tile(ti_):
                b_i, t_i = divmod(ti_, NTILE)
                tokb = ti_ * FT
                if t_i == 0:
                    S.op('pool', lambda e: e.memset(carry[:, :, :], 0.0), writes=[carry])
                for sc in range(NSUB):
                    S.dma('sp', lambda e, sc=sc, tokb=tokb: e.dma_start(out=hb[sc][:, :], in_=h_s[tokb + sc * 128:tokb + (sc + 1) * 128, :]),
                          'hC%d' % sc, writes=[hb[sc]])
                    rmsnorm_T(st, hb[sc], hn2T, sc * 128, G3, tmp_bf, ss, rs, xn)
                def _finish(jp):
                    ag = acc2[(2 * jp) % 4]; av = acc2[(2 * jp + 1) % 4]; gg = gg2[jp % 2]
                    S.op('act', lambda e: e.activation(out=gg[:, :], in_=ag[:, :], func=AF.Gelu_apprx_tanh), reads=[ag], writes=[gg])
                    S.op('pool', lambda e: e.tensor_tensor(out=actT[:, jp, :], in0=gg[:, :], in1=av[:, :], op=ALU.mult),
                         reads=[gg, av], writes=[actT])

                for jp in range(22):
                    if jp >= 1:
                        pass
                    for which in range(2):
                        j = jp + 22 * which
                        bk = nb(); ub = ub2[(2 * jp + which) % 4]; acc = acc2[(2 * jp + which) % 4]
                        for kc in range(8):
                            S.op('pe', lambda e, bk=bk, kc=kc, j=j: e.matmul(bk[:, 0:FT], lhsT=wup[:, kc, j * 128:(j + 1) * 128], rhs=hn2T[:, kc, :],
                                                                             start=(kc == 0), stop=(kc == 7)), reads=[wup, hn2T], writes=[bk])
                        S.op('act', lambda e, bk=bk, ub=ub: e.copy(out=ub[:, 2:FT + 2], in_=bk[:, 0:FT]), reads=[bk], writes=[ub])
                        S.op('pool', lambda e, ub=ub, j=j: e.tensor_copy(out=ub[:, 0:2], in_=carry[:, j, :]), reads=[carry], writes=[ub])
                        S.op('act', lambda e, bk=bk, acc=acc, j=j: e.activation(out=acc[:, :], in_=bk[:, 0:FT], func=AF.Identity,
                                                                                 scale=cst[:, CW + 2 * 44 + j:CW + 2 * 44 + j + 1], bias=cst[:, CB + j:CB + j + 1]),
                             reads=[bk, cst], writes=[acc])
                        S.op('dve', lambda e, ub=ub, acc=acc, j=j: e.scalar_tensor_tensor(out=acc[:, :], in0=ub[:, 1:FT + 1], scalar=cst[:, CW + 44 + j:CW + 44 + j + 1],
                                                                                         in1=acc[:, :], op0=ALU.mult, op1=ALU.add), reads=[ub, acc, cst], writes=[acc])
                        S.op('dve', lambda e, ub=ub, acc=acc, j=j: e.scalar_tensor_tensor(out=acc[:, :], in0=ub[:, 0:FT], scalar=cst[:, CW + j:CW + j + 1],
                                                                                         in1=acc[:, :], op0=ALU.mult, op1=ALU.add), reads=[ub, acc, cst], writes=[acc])
                        S.op('pool', lambda e, ub=ub, j=j: e.tensor_copy(out=carry[:, j, :], in_=ub[:, FT:FT + 2]), reads=[ub], writes=[carry])
                    if jp >= 1:
                        _finish(jp - 1)
                _finish(21)
                for sc in range(NSUB):
                    for half in range(2):
                        bk = nb(); hs = slice(half * 512, (half + 1) * 512)
                        for j in range(22):
                            S.op('pe', lambda e, bk=bk, j=j, sc=sc, hs=hs: e.matmul(bk[:, :], lhsT=actT[:, j, sc * 128:(sc + 1) * 128], rhs=wdn[:, j, hs],
                                                                                    start=(j == 0), stop=(j == 21)), reads=[actT, wdn], writes=[bk])
                        S.op('act', lambda e, bk=bk, hs=hs: e.copy(out=fo[:, hs], in_=bk[:, :]), reads=[bk], writes=[fo])
                    S.op('act', lambda e: e.activation(out=tmp_bf[:, :], in_=fo[:, :], func=AF.Square, accum_out=ss[:, 0:1]), reads=[fo], writes=[tmp_bf, ss])
                    S.op('act', lambda e: e.activation(out=rs[:, 0:1], in_=ss[:, 0:1], func=AF.Sqrt, scale=1.0 / D, bias=1e-6), reads=[ss], writes=[rs])
                    S.op('dve', lambda e: e.reciprocal(out=rs[:, 0:1], in_=rs[:, 0:1]), reads=[rs], writes=[rs])
                    S.op('dve', lambda e: e.scalar_tensor_tensor(out=oo[:, :], in0=fo[:, :], scalar=rs[:, 0:1], in1=rows[:, 1, :], op0=ALU.mult, op1=ALU.mult),
                         reads=[fo, rs, rows], writes=[oo])
                    S.op('pool', lambda e, sc=sc: e.tensor_tensor(out=oo[:, :], in0=oo[:, :], in1=hb[sc][:, :], op=ALU.add), reads=[oo, hb[sc]], writes=[oo])
                    S.dma('pool', lambda e, sc=sc, tokb=tokb: e.dma_start(out=out_d[tokb + sc * 128:tokb + (sc + 1) * 128, :], in_=oo[:, :]), 'ostore', reads=[oo])
            for ti_ in range(NSEQ * NTILE):
                _tile(ti_)
            S.barrier()
        S.barrier()
        S.emit()
    return nc


def host_consts(inp):
    f = np.float32
    c = np.zeros((128, NCONST), f)

    def pk(v, n):
        return np.asarray(v, f).reshape(n, 128).T
    c[:, G1:G1 + 8] = pk(inp["norm_mix_pre"][0], 8)
    c[:, MU:MU + 14] = pk(inp["rw_mu"][0], 14)
    c[:, W0:W0 + 4] = pk(inp["rw_w0"][0], 4)
    c[:, A0:A0 + 4] = pk(inp["rw_a0"][0], 4)
    c[:, KK:KK + 4] = pk(inp["rw_k_k"][0], 4)
    c[:, KA:KA + 4] = pk(inp["rw_k_a"][0], 4)
    c[:, RK:RK + 4] = pk(inp["rw_r_k"][0], 4)
    c[:, LW:LW + 4] = pk(inp["rw_lnx_w"][0], 4)
    c[:, LB:LB + 4] = pk(inp["rw_lnx_b"][0], 4)
    c[:, G3:G3 + 8] = pk(inp["norm_ffn_pre"][0], 8)
    c[:, CB:CB + 44] = pk(inp["ffn_conv_b"][0], 44)
    for tap in range(3):
        c[:, CW + tap * 44:CW + (tap + 1) * 44] = pk(inp["ffn_conv_w"][0, tap], 44)
    p = np.arange(128)
    inv = (10000.0 ** (-(np.arange(32, dtype=np.float32)) / np.float32(32))).astype(f)
    c[:, INV] = inv[p % 32] / f(2 * math.pi)
    c[:, SSC] = np.where((p % 64) < 32, -2 * math.pi, 2 * math.pi).astype(f)
    lg = np.log1p(-np.exp2(-5.0 - np.arange(8, dtype=np.float64)))
    for jc in range(4):
        hsel = 2 * jc + p // 64
        c[:, CD + jc] = np.exp(128 * lg[hsel])
        c[:, QD + jc * 128:QD + (jc + 1) * 128] = np.exp((np.arange(128)[None, :] + 1.0) * lg[hsel][:, None])
    for h in range(8):
        c[:, KDEC + h] = 0.125 * np.exp((127.0 - p) * lg[h])
    tab = np.zeros((128, NTAB), f)
    s = np.arange(128)[:, None]; t = np.arange(128)[None, :]
    strict = (t > s).astype(f); incl = (t >= s).astype(f)
    tab[:, T_MP:T_MP + 512] = np.concatenate([strict, incl, strict, incl], axis=1)
    tab[:, T_M0:T_M0 + 128] = (t < s).astype(f)
    for h in range(8):
        tab[:, T_MT + h * 128:T_MT + (h + 1) * 128] = np.where(t >= s, 0.125 * np.exp(np.maximum(t - s, 0) * lg[h]), 0.0)
    tab[:, T_BM:T_BM + 128] = ((s // 64) == (t // 64)).astype(f)
    tab[:, T_ID:T_ID + 128] = np.eye(128, dtype=f)
    tab[:, T_SW:T_SW + 128] = (t == (s ^ 32)).astype(f)
    lr = np.concatenate([np.concatenate([inp["rw_w2"][0], inp["rw_a2"][0]], axis=0), inp["rw_g2"][0]], axis=1).astype(f)
    rows = np.stack([inp["norm_mix_post"][0], inp["norm_ffn_post"][0]], axis=0).astype(f)
    return c, tab, lr, rows


_NC_CACHE = {}
STOP = [None]


def _stop(n):
    return STOP[0] is not None and n >= STOP[0]

DBG = {}


def run(inp, NSEQ, NCH, ncores, dbg=False, phases="ABC"):
    key = (NSEQ, NCH, dbg, phases, STOP[0])
    if key not in _NC_CACHE:
        _NC_CACHE[key] = build(NSEQ, NCH, dbg, phases)
    nc = _NC_CACHE[key]
    c, tab, lr, rows = host_consts(inp)
    T = NCH * 128
    shared = {
        "w_in": np.ascontiguousarray(inp["w_in"][0]), "wbrw": np.ascontiguousarray(inp["w_branch_rw"][0]),
        "wbret": np.ascontiguousarray(inp["w_branch_ret"][0]), "wout": np.ascontiguousarray(inp["w_out"][0]),
        "wup": np.ascontiguousarray(inp["ffn_w_up"][0]), "wdn": np.ascontiguousarray(inp["ffn_w_down"][0]),
        "lr": lr, "c128": c, "rows": rows, "tab": tab,
    }
    in_maps = []
    for i in range(ncores):
        xs = np.ascontiguousarray(inp["x"][i * NSEQ:(i + 1) * NSEQ, :T, :]).reshape(NSEQ * T, D)
        ps = np.ascontiguousarray(inp["positions"][i * NSEQ:(i + 1) * NSEQ, :T]).reshape(1, NSEQ * T).astype(np.int32)
        m = dict(shared); m["x"] = xs; m["pos"] = ps
        in_maps.append(m)
    res = run_bass_kernel_spmd(nc, in_maps, core_ids=list(range(ncores)))
    return res


def kernel(**inputs):
    inp = {k: np.asarray(v) for k, v in inputs.items()}
    B, T, _ = inp["x"].shape
    ncores = 8
    NSEQ = B // ncores
    res = run(inp, NSEQ, T // 128, ncores)
    out = np.concatenate([r["out"].reshape(NSEQ, T, D) for r in res.results], axis=0)
    return out.astype(np.float32)
```

```python
import math
import numpy as np
from contextlib import ExitStack
import concourse.bass as bass
import concourse.mybir as mybir
from concourse.bass_utils import run_bass_kernel_spmd

F32 = mybir.dt.float32; BF16 = mybir.dt.bfloat16; I32 = mybir.dt.int32
AF = mybir.ActivationFunctionType; ALU = mybir.AluOpType; AX = mybir.AxisListType

D = 1024; RWC = 1792; INC = 6912; DFF = 2816
C0 = math.exp(-0.5)
G1, MU, W0, A0, KK, KA, RK, LW, LB, INV, SSC, CD, G3, CB, CW, QD, KDEC, NCONST = (
    0, 8, 22, 26, 30, 34, 38, 42, 46, 50, 51, 52, 56, 64, 108, 240, 752, 760)
T_MP, T_M0, T_MT, T_BM, T_ID, T_SW, NTAB = 0, 512, 640, 1664, 1792, 1920, 2048


class Buf:
    def __init__(self, t, name):
        self.t = t; self.name = name; self.writers = {}; self.readers = {}

    def __getitem__(self, k):
        return self.t[k]


class Sched:
    ENG = ['pe', 'act', 'dve', 'pool', 'sp']

    def __init__(self, nc, stack):
        self.nc = nc; self.stack = stack; self.semh = {}
        for e in self.ENG:
            self.semh[e] = stack.enter_context(nc.semaphore("s_" + e))
        self.cnt = {e: 0 for e in self.ENG}
        self.known = {e: {} for e in self.ENG}
        self.prog = {e: [] for e in self.ENG}
        self.dcnt = {}
        self.nbuf = 0

    def sb(self, st, shape, dt):
        self.nbuf += 1
        name = f"b{self.nbuf}"
        return Buf(st.enter_context(self.nc.sbuf_tensor(name, list(shape), dt)), name)

    def ps(self, st, shape, dt):
        self.nbuf += 1
        name = f"p{self.nbuf}"
        b = Buf(st.enter_context(self.nc.psum_tensor(name, list(shape), dt)), name)
        b.psum = True
        return b

    def _waits(self, eng, reads, writes):
        need = {}

        def add(k, v, raw):
            if k == eng and not raw and eng == 'pe':
                return
            if need.get(k, 0) < v:
                need[k] = v
        for b in reads:
            for k, v in b.writers.items():
                add(k, v, True)
        for b in writes:
            for k, v in b.writers.items():
                add(k, v, False)
            for k, v in b.readers.items():
                add(k, v, False)
        out = []
        for k, v in need.items():
            if k.startswith('d_'):
                v = self.dcnt[k]
            if self.known[eng].get(k, 0) >= v:
                continue
            self.known[eng][k] = v
            out.append((self.semh[k], v))
        return out

    def op(self, eng, fn, reads=(), writes=()):
        pr = [b for b in reads if getattr(b, 'psum', False)]
        if pr:
            writes = list(writes) + [b for b in pr if b not in writes]
            reads = [b for b in reads if not getattr(b, 'psum', False)]
        waits = self._waits(eng, reads, writes)
        self.cnt[eng] += 1
        n = self.cnt[eng]
        sem = self.semh[eng]

        def run(e, waits=waits, fn=fn, sem=sem):
            for s, v in waits:
                e.wait_ge(s, v)
            fn(e).then_inc(sem, 1)
        self.prog[eng].append(run)
        for b in reads:
            if b.readers.get(eng, 0) < n:
                b.readers[eng] = n
        for b in writes:
            b.writers = {eng: n}; b.readers = {}

    def dma(self, q, fn, semname, reads=(), writes=()):
        key = 'd_' + semname
        if key not in self.semh:
            self.semh[key] = self.stack.enter_context(self.nc.semaphore(key))
            self.dcnt[key] = 0
        waits = self._waits(q, reads, writes)
        self.dcnt[key] += 16
        n = self.dcnt[key]
        sem = self.semh[key]

        def run(e, waits=waits, fn=fn, sem=sem):
            for s, v in waits:
                e.wait_ge(s, v)
            fn(e).then_inc(sem, 16)
        self.prog[q].append(run)
        for b in reads:
            if b.readers.get(key, 0) < n:
                b.readers[key] = n
        for b in writes:
            b.writers = dict(b.writers); b.writers[key] = n
            b.readers = {}

    def barrier(self):
        allc = {}
        for e in self.ENG:
            if self.cnt[e] > 0:
                allc[e] = self.cnt[e]
        for k, v in self.dcnt.items():
            if v > 0:
                allc[k] = v
        for e in self.ENG:
            waits = []
            for k, v in allc.items():
                if self.known[e].get(k, 0) >= v:
                    continue
                self.known[e][k] = v
                waits.append((self.semh[k], v))

            def run(en, waits=waits):
                for s, v in waits:
                    en.wait_ge(s, v)
            self.prog[e].append(run)

    def emit(self):
        nc = self.nc
        with nc.Block() as block:
            @block.tensor
            def _(e):
                for f in self.prog['pe']:
                    f(e)

            @block.scalar
            def _(e):
                for f in self.prog['act']:
                    f(e)

            @block.vector
            def _(e):
                for f in self.prog['dve']:
                    f(e)

            @block.gpsimd
            def _(e):
                for f in self.prog['pool']:
                    f(e)

            @block.sync
            def _(e):
                for f in self.prog['sp']:
                    f(e)


def build(NSEQ, NCH, dbg=False, phases="ABC"):
    T = NCH * 128; NTOK = NSEQ * T; NCHUNK = NSEQ * NCH
    FT = min(256, T); NSUB = FT // 128; NTILE = T // FT
    nc = bass.Bass("TRN2", target_bir_lowering=False)

    def dr(name, shape, dt, kind="ExternalInput"):
        return nc.dram_tensor(name, list(shape), dt, kind=kind).ap()
    x_d = dr("x", [NTOK, D], F32)
    pos_d = dr("pos", [1, NTOK], I32)
    win_d = dr("w_in", [D, INC], F32)
    wbrw_d = dr("wbrw", [512, D], F32)
    wbret_d = dr("wbret", [D, D], F32)
    wout_d = dr("wout", [D, D], F32)
    wup_d = dr("wup", [D, 2 * DFF], F32)
    wdn_d = dr("wdn", [DFF, D], F32)
    lr_d = dr("lr", [128, 1024], F32)
    c_d = dr("c128", [128, NCONST], F32)
    rows_d = dr("rows", [2, D], F32)
    tab_d = dr("tab", [128, NTAB], F32)
    out_d = dr("out", [NTOK, D], F32, kind="ExternalOutput")
    sk = "ExternalOutput" if dbg else "Internal"
    hnT_s = dr("hnT_s", [NCHUNK, 128, 1024], BF16, kind=sk)
    yrw_s = dr("yrw_s", [NCHUNK, 128, 512], BF16, kind=sk)
    h_s = dr("h_s", [NTOK, D], F32, kind=sk)

    with ExitStack() as st0:
        S = Sched(nc, st0)
        banks = [S.ps(st0, [128, 512], F32) for _ in range(8)]
        bstate = [0]

        def nb():
            b = banks[bstate[0] % 8]; bstate[0] += 1
            return b

        def v3(b, a):
            return b.t[:, :].rearrange("p (a b) -> p a b", a=a)

        def vbf(b):
            return b.t[:, :].bitcast(BF16)

        cst = S.sb(st0, [128, NCONST], F32)
        tabf = S.sb(st0, [128, NTAB], F32)
        tabb = S.sb(st0, [128, NTAB], BF16)
        rows = S.sb(st0, [128, 2, D], F32)
        ones = S.sb(st0, [128, 128], F32)
        omka = S.sb(st0, [128, 4], F32)
        S.dma('sp', lambda e: e.dma_start(out=cst[:, :], in_=c_d), 'cst', writes=[cst])
        S.dma('sp', lambda e: e.dma_start(out=tabf[:, :], in_=tab_d), 'tabf', writes=[tabf])
        S.dma('pool', lambda e: e.dma_start(out=tabb[:, :], in_=tab_d), 'tabb', writes=[tabb])
        for r in range(2):
            S.dma('sp', lambda e, r=r: e.dma_start(out=rows[:, r, :], in_=rows_d[r:r + 1, :].partition_broadcast(128)),
                  'rows', writes=[rows])
        S.op('pool', lambda e: e.memset(ones[:, :], 1.0), writes=[ones])
        S.op('dve', lambda e: e.tensor_scalar(out=omka[:, :], in0=cst[:, KA:KA + 4], scalar1=-1.0, scalar2=1.0,
                                              op0=ALU.mult, op1=ALU.add), reads=[cst], writes=[omka])
        ident = tabb[:, T_ID:T_ID + 128]
        bones = tabb[:, T_BM:T_BM + 128]
        pswap = tabb[:, T_SW:T_SW + 128]

        def cb(off, n, shape):
            return cst[:, off:off + n].unsqueeze(2).to_broadcast(shape)

        def rmsnorm_T(stp, xs, dstT, col0, gcol, tmp_bf, ss, rs, xn, use_ln=False):
            S.op('act', lambda e: e.activation(out=tmp_bf[:, :], in_=xs[:, :], func=AF.Square, accum_out=ss[:, 0:1]),
                 reads=[xs], writes=[tmp_bf, ss])
            if use_ln:
                S.op('act', lambda e: e.activation(out=rs[:, 0:1], in_=ss[:, 0:1], func=AF.Ln, scale=1.0 / D, bias=1e-6),
                     reads=[ss], writes=[rs])
                S.op('act', lambda e: e.activation(out=rs[:, 0:1], in_=rs[:, 0:1], func=AF.Exp, scale=-0.5), reads=[rs], writes=[rs])
            else:
                S.op('act', lambda e: e.activation(out=rs[:, 0:1], in_=ss[:, 0:1], func=AF.Sqrt, scale=1.0 / D, bias=1e-6),
                     reads=[ss], writes=[rs])
                S.op('dve', lambda e: e.reciprocal(out=rs[:, 0:1], in_=rs[:, 0:1]), reads=[rs], writes=[rs])
            S.op('dve', lambda e: e.tensor_scalar(out=xn[:, :], in0=xs[:, :], scalar1=rs[:, 0:1], scalar2=None,
                                                  op0=ALU.mult), reads=[xs, rs], writes=[xn])
            bk = nb()
            for kc in range(8):
                S.op('pe', lambda e, kc=kc: e.transpose(out=vbf(bk)[:, kc * 128:(kc + 1) * 128],
                                                        in_=xn[:, kc * 128:(kc + 1) * 128], identity=ident),
                     reads=[xn, tabb], writes=[bk])
            S.op('dve', lambda e: e.tensor_tensor(
                out=dstT[:, :, col0:col0 + 128],
                in0=vbf(bk).rearrange("p (a b) -> p a b", a=8),
                in1=cb(gcol, 8, [128, 8, 128]), op=ALU.mult), reads=[bk, cst], writes=[dstT])

        if "A" in phases:
          with ExitStack() as st:
            w_rw = S.sb(st, [128, 8, RWC], BF16)
            lrb = S.sb(st, [128, 1024], BF16)
            for kc in range(8):
                S.dma('pool', lambda e, kc=kc: e.dma_start(out=w_rw[:, kc, :], in_=win_d[kc * 128:(kc + 1) * 128, 0:RWC]),
                      'wA', writes=[w_rw])
            S.dma('pool', lambda e: e.dma_start(out=lrb[:, :], in_=lr_d), 'wA', writes=[lrb])
            xs2 = [S.sb(st, [128, D], F32) for _ in range(2)]
            tmp_bf = S.sb(st, [128, D], BF16); xn = S.sb(st, [128, D], BF16)
            ss = S.sb(st, [128, 1], F32); rs = S.sb(st, [128, 1], F32)
            hnT = S.sb(st, [128, 8, 128], BF16)
            praw = S.sb(st, [128, 14, 129], F32)
            pl = S.sb(st, [128, 14, 128], F32)
            lr12 = S.sb(st, [128, 128], BF16); sg = S.sb(st, [128, 128], BF16)
            f = [S.sb(st, [128, 4, 128], F32) for _ in range(16)]
            (gT, sig, cs, csx, Epos, Eneg, Em, Ehat, al, kq, kkb, kp, bv, t0, t1, t2) = f
            sqb = S.sb(st, [128, 4, 128], BF16)
            SCin = S.sb(st, [128, 4, 2, 128], BF16)
            BtT = S.sb(st, [128, 4, 128], BF16); KtT = S.sb(st, [128, 4, 128], BF16)
            BhT = S.sb(st, [128, 4, 128], BF16); KhT = S.sb(st, [128, 4, 128], BF16)
            vTb = S.sb(st, [128, 4, 128], BF16)
            Vp = S.sb(st, [128, 4, 128], BF16); BH = S.sb(st, [128, 4, 128], BF16); KH = S.sb(st, [128, 4, 128], BF16)
            VZ = S.sb(st, [128, 4, 2, 128], BF16); AZ = S.sb(st, [128, 4, 2, 128], BF16); UVZ = S.sb(st, [128, 4, 2, 128], BF16)
            Ap = S.sb(st, [128, 4, 128], BF16); UVp = S.sb(st, [128, 4, 128], BF16)
            SCT = S.sb(st, [128, 4, 2, 512], BF16)
            Xs = S.sb(st, [128, 4, 2, 128], BF16)
            MN = S.sb(st, [128, 4, 2, 256], BF16)
            Xt = [Buf(Xs.t, 'Xt%d' % q_) for q_ in range(4)]
            MNt = [Buf(MN.t, 'MNt%d' % q_) for q_ in range(4)]
            RhT = S.sb(st, [128, 4, 128], BF16); GZ = S.sb(st, [128, 4, 128], BF16)
            HZ = S.sb(st, [128, 4, 128], BF16); Hf = S.sb(st, [128, 4, 128], F32)
            yb = S.sb(st, [128, 4, 128], BF16); ysq = S.sb(st, [128, 4, 128], BF16); rkb = S.sb(st, [128, 4, 128], BF16)
            yrwT = S.sb(st, [128, 4, 128], BF16)
            for z in (VZ, AZ, UVZ):
                S.op('pool', lambda e, z=z: e.memset(z[:, :, :, :], 0.0), writes=[z])
            bmf = tabf[:, T_BM:T_BM + 128].unsqueeze(1).to_broadcast([128, 4, 128])
            prt = [Buf(praw.t, 'prt%d' % q_) for q_ in range(4)]
            plt = [Buf(pl.t, 'plt%d' % q_) for q_ in range(4)]
            tmpS = [S.sb(st, [128, 512], BF16) for _ in range(2)]

            DBG.update({k_: v_.name for k_, v_ in list(locals().items()) if isinstance(v_, Buf)})

            def _chunk(ci):
                b_i, c_i = divmod(ci, NCH)
                tok0 = ci * 128
                xs = xs2[ci % 2]
                S.dma('sp', lambda e, xs=xs, tok0=tok0: e.dma_start(out=xs[:, :], in_=x_d[tok0:tok0 + 128, :]),
                      'xA%d' % (ci % 2), writes=[xs])
                if c_i == 0:
                    S.op('pool', lambda e: e.memset(praw[:, :, 0:1], 0.0), writes=prt)
                    S.op('pool', lambda e: e.memset(Hf[:, :, :], 0.0), writes=[Hf])
                    S.op('pool', lambda e: e.memset(HZ[:, :, :], 0.0), writes=[HZ])
                rmsnorm_T(st, xs, hnT, 0, G1, tmp_bf, ss, rs, xn, use_ln=True)
                S.dma('sp', lambda e, ci=ci: e.dma_start(out=hnT_s[ci].rearrange("p (a b) -> p a b", a=8), in_=hnT[:, :, :]),
                      'hst', reads=[hnT])
                if _stop(1):
                    return
                def proj_group(g):
                    bk = nb(); n = 4 if g < 3 else 2
                    j0 = g * 4
                    for jj in range(n):
                        j = j0 + jj
                        for kc in range(8):
                            S.op('pe', lambda e, jj=jj, j=j, kc=kc: e.matmul(
                                bk[:, jj * 128:(jj + 1) * 128], lhsT=w_rw[:, kc, j * 128:(j + 1) * 128], rhs=hnT[:, kc, :],
                                start=(kc == 0), stop=(kc == 7)), reads=[w_rw, hnT], writes=[bk])
                    S.op('act', lambda e: e.copy(out=praw[:, j0:j0 + n, 1:129], in_=v3(bk, 4)[:, 0:n, :]), reads=[bk], writes=[prt[g]])
                    S.op('dve', lambda e: e.tensor_tensor(out=pl[:, j0:j0 + n, :], in0=praw[:, j0:j0 + n, 0:128], in1=praw[:, j0:j0 + n, 1:129],
                                                          op=ALU.subtract), reads=[prt[g]], writes=[plt[g]])
                    S.op('pool', lambda e: e.tensor_tensor(out=pl[:, j0:j0 + n, :], in0=pl[:, j0:j0 + n, :], in1=cb(MU + j0, n, [128, n, 128]),
                                                           op=ALU.mult), reads=[plt[g], cst], writes=[plt[g]])
                    S.op('dve', lambda e: e.tensor_tensor(out=pl[:, j0:j0 + n, :], in0=pl[:, j0:j0 + n, :], in1=praw[:, j0:j0 + n, 1:129],
                                                          op=ALU.add), reads=[plt[g], prt[g]], writes=[plt[g]])
                    S.op('act', lambda e: e.copy(out=praw[:, j0:j0 + n, 0:1], in_=praw[:, j0:j0 + n, 128:129]), reads=[prt[g]], writes=[prt[g]])

                proj_group(3)
                proj_group(1)
                if _stop(2):
                    return
                S.op('act', lambda e: e.activation(out=lr12[0:64, :], in_=pl[0:64, 12, :], func=AF.Tanh), reads=[plt[3]], writes=[lr12])
                S.op('act', lambda e: e.copy(out=lr12[64:128, :], in_=pl[64:128, 12, :]), reads=[plt[3]], writes=[lr12])
                S.op('act', lambda e: e.activation(out=sg[:, :], in_=pl[:, 13, :], func=AF.Sigmoid), reads=[plt[3]], writes=[sg])
                proj_group(0)
                bw = nb(); ba = nb(); bg = nb()
                for jc in range(4):
                    S.op('pe', lambda e, jc=jc: e.matmul(bw[:, jc * 128:(jc + 1) * 128], lhsT=lrb[0:64, jc * 128:(jc + 1) * 128],
                                                         rhs=lr12[0:64, :], start=True, stop=True), reads=[lrb, lr12], writes=[bw])
                for jc in range(4):
                    S.op('pe', lambda e, jc=jc: e.matmul(ba[:, jc * 128:(jc + 1) * 128], lhsT=lrb[64:128, jc * 128:(jc + 1) * 128],
                                                         rhs=lr12[64:128, :], start=True, stop=True), reads=[lrb, lr12], writes=[ba])
                for jc in range(4):
                    S.op('pe', lambda e, jc=jc: e.matmul(bg[:, jc * 128:(jc + 1) * 128], lhsT=lrb[:, 512 + jc * 128:512 + (jc + 1) * 128],
                                                         rhs=sg[:, :], start=True, stop=True), reads=[lrb, sg], writes=[bg])
                proj_group(2)
                if _stop(3):
                    return
                S.op('dve', lambda e: e.tensor_tensor(out=t0[:, :, :], in0=v3(bw, 4), in1=cb(W0, 4, [128, 4, 128]), op=ALU.add),
                     reads=[bw, cst], writes=[t0])
                S.op('act', lambda e: e.activation(out=sig[:, :, :], in_=t0[:, :, :], func=AF.Sigmoid), reads=[t0], writes=[sig])
                S.op('dve', lambda e: e.tensor_tensor(out=t2[:, :, :], in0=v3(ba, 4), in1=cb(A0, 4, [128, 4, 128]), op=ALU.add),
                     reads=[ba, cst], writes=[t2])
                S.op('act', lambda e: e.activation(out=al[:, :, :], in_=t2[:, :, :], func=AF.Sigmoid), reads=[t2], writes=[al])
                S.op('act', lambda e: e.copy(out=gT[:, :, :], in_=v3(bg, 4)), reads=[bg], writes=[gT])
                for jc in range(4):
                    S.op('dve', lambda e, jc=jc: e.tensor_tensor_scan(out=cs[:, jc, :], data0=ones[:, :], data1=sig[:, jc, :],
                                                                      initial=0.0, op0=ALU.mult, op1=ALU.add),
                         reads=[ones, sig], writes=[cs])
                S.op('pool', lambda e: e.tensor_tensor(out=csx[:, :, :], in0=cs[:, :, :], in1=sig[:, :, :], op=ALU.subtract),
                     reads=[cs, sig], writes=[csx])
                S.op('act', lambda e: e.activation(out=Epos[:, :, :], in_=cs[:, :, :], func=AF.Exp, scale=-C0), reads=[cs], writes=[Epos])
                S.op('act', lambda e: e.activation(out=Eneg[:, :, :], in_=cs[:, :, :], func=AF.Exp, scale=C0), reads=[cs], writes=[Eneg])
                S.op('act', lambda e: e.activation(out=Em[:, :, :], in_=csx[:, :, :], func=AF.Exp, scale=-C0), reads=[csx], writes=[Em])
                S.op('dve', lambda e: e.tensor_tensor(out=t1[:, :, :], in0=cs[:, :, 127:128].to_broadcast([128, 4, 128]),
                                                      in1=cs[:, :, :], op=ALU.subtract), reads=[cs], writes=[t1])
                S.op('act', lambda e: e.activation(out=Ehat[:, :, :], in_=t1[:, :, :], func=AF.Exp, scale=-C0), reads=[t1], writes=[Ehat])
                S.op('pool', lambda e: e.tensor_tensor(out=kq[:, :, :], in0=pl[:, 4:8, :], in1=cb(KK, 4, [128, 4, 128]), op=ALU.mult),
                     reads=[plt[1], cst], writes=[kq])
                S.op('pool', lambda e: e.tensor_tensor(out=sqb[:, :, :], in0=kq[:, :, :], in1=kq[:, :, :], op=ALU.mult),
                     reads=[kq], writes=[sqb])
                bs = nb()
                for jc in range(4):
                    S.op('pe', lambda e, jc=jc: e.matmul(bs[:, jc * 128:(jc + 1) * 128], lhsT=bones, rhs=sqb[:, jc, :],
                                                         start=True, stop=True), reads=[tabb, sqb], writes=[bs])
                S.op('dve', lambda e: e.tensor_scalar(out=t0[:, :, :], in0=v3(bs, 4), scalar1=1e-24, scalar2=None, op0=ALU.max),
                     reads=[bs], writes=[t0])
                S.op('act', lambda e: e.activation(out=t0[:, :, :], in_=t0[:, :, :], func=AF.Ln, scale=float(2.0 ** 40)), reads=[t0], writes=[t0])
                S.op('act', lambda e: e.activation(out=t0[:, :, :], in_=t0[:, :, :], func=AF.Exp, scale=-0.5, bias=20.0 * math.log(2.0)),
                     reads=[t0], writes=[t0])
                S.op('dve', lambda e: e.tensor_tensor(out=kkb[:, :, :], in0=kq[:, :, :], in1=t0[:, :, :], op=ALU.mult),
                     reads=[kq, t0], writes=[kkb])
                S.op('pool', lambda e: e.tensor_tensor(out=t2[:, :, :], in0=al[:, :, :], in1=cb(KA, 4, [128, 4, 128]), op=ALU.mult),
                     reads=[al, cst], writes=[t2])
                S.op('pool', lambda e: e.tensor_tensor(out=t2[:, :, :], in0=t2[:, :, :],
                                                       in1=omka[:, :].unsqueeze(2).to_broadcast([128, 4, 128]), op=ALU.add),
                     reads=[t2, omka], writes=[t2])
                S.op('dve', lambda e: e.tensor_tensor(out=kp[:, :, :], in0=pl[:, 4:8, :], in1=t2[:, :, :], op=ALU.mult),
                     reads=[plt[1], t2], writes=[kp])
                if _stop(4):
                    return
                S.op('pool', lambda e: e.tensor_tensor(out=t1[:, :, :], in0=kkb[:, :, :], in1=Em[:, :, :], op=ALU.mult),
                     reads=[kkb, Em], writes=[t1])
                S.op('act', lambda e: e.activation(out=SCin[:, :, 0, :], in_=t1[:, :, :], func=AF.Identity, scale=-1.0),
                     reads=[t1], writes=[SCin])
                S.op('pool', lambda e: e.tensor_tensor(out=bv[:, :, :], in0=kkb[:, :, :], in1=al[:, :, :], op=ALU.mult),
                     reads=[kkb, al], writes=[bv])
                S.op('dve', lambda e: e.tensor_tensor(out=BtT[:, :, :], in0=bv[:, :, :], in1=Eneg[:, :, :], op=ALU.mult),
                     reads=[bv, Eneg], writes=[BtT])
                S.op('pool', lambda e: e.tensor_tensor(out=KtT[:, :, :], in0=kp[:, :, :], in1=Eneg[:, :, :], op=ALU.mult),
                     reads=[kp, Eneg], writes=[KtT])
                S.op('dve', lambda e: e.tensor_tensor(out=SCin[:, :, 1, :], in0=pl[:, 0:4, :], in1=Epos[:, :, :], op=ALU.mult),
                     reads=[plt[0], Epos], writes=[SCin])
                S.op('pool', lambda e: e.tensor_tensor(out=BhT[:, :, :], in0=bv[:, :, :], in1=Ehat[:, :, :], op=ALU.mult),
                     reads=[bv, Ehat], writes=[BhT])
                S.op('dve', lambda e: e.tensor_tensor(out=KhT[:, :, :], in0=kp[:, :, :], in1=Ehat[:, :, :], op=ALU.mult),
                     reads=[kp, Ehat], writes=[KhT])
                S.op('act', lambda e: e.copy(out=vTb[:, :, :], in_=pl[:, 8:12, :]), reads=[plt[2]], writes=[vTb])
                if _stop(4.3):
                    return
                bt1 = nb(); bt2 = nb()
                for jc in range(4):
                    S.op('pe', lambda e, jc=jc: e.transpose(out=vbf(bt1)[:, jc * 128:(jc + 1) * 128], in_=SCin[:, jc, 0, :], identity=ident),
                         reads=[SCin, tabb], writes=[bt1])
                    S.op('pe', lambda e, jc=jc: e.transpose(out=vbf(bt1)[:, 512 + jc * 128:512 + (jc + 1) * 128], in_=vTb[:, jc, :], identity=ident),
                         reads=[vTb, tabb], writes=[bt1])
                    S.op('pe', lambda e, jc=jc: e.transpose(out=vbf(bt2)[:, jc * 128:(jc + 1) * 128], in_=BhT[:, jc, :], identity=ident),
                         reads=[BhT, tabb], writes=[bt2])
                    S.op('pe', lambda e, jc=jc: e.transpose(out=vbf(bt2)[:, 512 + jc * 128:512 + (jc + 1) * 128], in_=KhT[:, jc, :], identity=ident),
                         reads=[KhT, tabb], writes=[bt2])
                if _stop(4.6):
                    return
                b1v = vbf(bt1).rearrange("p (k a h c) -> p k a h c", k=2, a=4, h=2)
                S.op('act', lambda e: e.copy(out=Xs[:, :, :, 0:64], in_=b1v[:, 0, :, :, :]), reads=[bt1], writes=Xt)
                if _stop(4.7):
                    return
                S.op('dve', lambda e: e.tensor_copy(out=Vp[:, :, :], in_=vbf(bt1)[:, 512:1024].rearrange("p (a b) -> p a b", a=4)),
                     reads=[bt1], writes=[Vp])
                if _stop(4.8):
                    return
                for hp in range(2):
                    S.op('act', lambda e, hp=hp: e.copy(out=VZ[:, :, hp, hp * 64:(hp + 1) * 64], in_=b1v[:, 1, :, hp, :]),
                         reads=[bt1], writes=[VZ])
                if _stop(4.85):
                    return
                S.op('dve', lambda e: e.tensor_copy(out=BH[:, :, :], in_=vbf(bt2)[:, 0:512].rearrange("p (a b) -> p a b", a=4)),
                     reads=[bt2], writes=[BH])
                if _stop(4.9):
                    return
                S.op('act', lambda e: e.copy(out=KH[:, :, :], in_=vbf(bt2)[:, 512:1024].rearrange("p (a b) -> p a b", a=4)),
                     reads=[bt2], writes=[KH])
                if _stop(5):
                    return
                b3 = [None, None]
                for jc in range(4):
                    for hp in range(2):
                        pb = 64 * hp; h = 2 * jc + hp
                        if h % 4 == 0:
                            b3 = [nb(), nb()]
                        bk = nb()
                        rhs2 = SCin[pb:pb + 64, jc, :, :].rearrange("p a b -> p (a b)")
                        S.op('pe', lambda e, bk=bk, jc=jc, pb=pb, rhs2=rhs2: e.matmul(bk[:, 0:256], lhsT=BtT[pb:pb + 64, jc, :], rhs=rhs2,
                                                                                      start=True, stop=True), reads=[BtT, SCin], writes=[bk])
                        S.op('pe', lambda e, bk=bk, jc=jc, pb=pb, rhs2=rhs2: e.matmul(bk[:, 256:512], lhsT=KtT[pb:pb + 64, jc, :], rhs=rhs2,
                                                                                      start=True, stop=True), reads=[KtT, SCin], writes=[bk])
                        if hp == 0:
                            S.op('dve', lambda e, bk=bk, jc=jc, hp=hp: e.tensor_tensor(out=SCT[:, jc, hp, :], in0=bk[:, :], in1=tabf[:, T_MP:T_MP + 512],
                                                                                       op=ALU.mult), reads=[bk, tabf], writes=[SCT])
                        else:
                            tS = tmpS[jc % 2]
                            S.op('act', lambda e, bk=bk, tS=tS: e.copy(out=tS[:, :], in_=bk[:, :]), reads=[bk], writes=[tS])
                            S.op('pool', lambda e, tS=tS, jc=jc, hp=hp: e.tensor_tensor(out=SCT[:, jc, hp, :], in0=tS[:, :], in1=tabb[:, T_MP:T_MP + 512],
                                                                                        op=ALU.mult), reads=[tS, tabb], writes=[SCT])
                        b3k = b3[hp]
                        S.op('pe', lambda e, b3k=b3k, jc=jc, pb=pb: e.matmul(b3k[:, (jc % 2) * 128:(jc % 2 + 1) * 128], lhsT=SCin[pb:pb + 64, jc, 0, :],
                                                                             rhs=BtT[pb:pb + 64, jc, :], start=True, stop=True),
                             reads=[SCin, BtT], writes=[b3k])
                        if h % 4 == 3:
                            for hq in range(2):
                                S.op('dve', lambda e, g=h // 4, hq=hq, b3g=b3[hq]: e.tensor_tensor(
                                    out=MN[:, 2 * g:2 * g + 2, hq, 0:128],
                                    in0=b3g.t[:, 0:256].rearrange("p (a c) -> p a c", a=2),
                                    in1=tabf[:, T_M0:T_M0 + 128].unsqueeze(1).to_broadcast([128, 2, 128]), op=ALU.mult),
                                    reads=[b3[hq], tabf], writes=[MNt[2 * (h // 4)], MNt[2 * (h // 4) + 1]])
                if _stop(6):
                    return
                for g in range(2):
                    bk = nb()
                    for hh in range(4):
                        jc = 2 * g + hh // 2; hp = hh % 2
                        S.op('pe', lambda e, bk=bk, hh=hh, jc=jc, hp=hp: e.matmul(bk[:, hh * 128:(hh + 1) * 128], lhsT=SCT[:, jc, hp, 256:384],
                                                                                  rhs=Vp[:, jc, :], start=True, stop=True), reads=[SCT, Vp], writes=[bk])
                    bvw = bk.t[:, :].rearrange("p (a h g c) -> p a h g c", a=2, h=2, g=2)
                    for hp in range(2):
                        S.op('act', lambda e, bvw=bvw, g=g, hp=hp: e.copy(out=Xs[:, 2 * g:2 * g + 2, hp, 64:128], in_=bvw[:, :, hp, hp, :]),
                             reads=[bk], writes=[Xt[2 * g], Xt[2 * g + 1]])
                if _stop(7):
                    return
                for j in range(7):
                    for jc in range(4):
                        bP = nb(); bQ = nb() if j < 6 else None
                        for hp in range(2):
                            Nj = SCT[:, jc, hp, 0:128] if j == 0 else MN[:, jc, hp, 128:256]
                            nsrc = [SCT] if j == 0 else []
                            S.op('pe', lambda e, bP=bP, hp=hp, jc=jc, Nj=Nj: e.matmul(bP[:, hp * 128:(hp + 1) * 128], lhsT=Nj, rhs=Xs[:, jc, hp, :],
                                                                                     start=True, stop=True), reads=nsrc + [Xt[jc], MNt[jc]], writes=[bP])
                            if j < 6:
                                S.op('pe', lambda e, bQ=bQ, hp=hp, jc=jc, Nj=Nj: e.matmul(bQ[:, hp * 256:hp * 256 + 128], lhsT=Nj, rhs=MN[:, jc, hp, 0:128],
                                                                                         start=True, stop=True), reads=nsrc + [MNt[jc]], writes=[bQ])
                                S.op('pe', lambda e, bQ=bQ, hp=hp, jc=jc, Nj=Nj: e.matmul(bQ[:, hp * 256 + 128:hp * 256 + 256], lhsT=MN[:, jc, hp, 0:128], rhs=Nj,
                                                                                         start=True, stop=True), reads=nsrc + [MNt[jc]], writes=[bQ])
                        S.op('dve', lambda e, jc=jc, bP=bP: e.tensor_tensor(out=Xs[:, jc, :, :], in0=Xs[:, jc, :, :],
                                                                            in1=bP.t[:, 0:256].rearrange("p (h c) -> p h c", h=2), op=ALU.add),
                             reads=[Xt[jc], bP], writes=[Xt[jc]])
                        if j < 6:
                            S.op('act', lambda e, jc=jc, bQ=bQ: e.copy(out=MN[:, jc, :, :], in_=bQ.t[:, :].rearrange("p (h c) -> p h c", h=2)),
                                 reads=[bQ], writes=[MNt[jc]])
                if _stop(8):
                    return
                for hp in range(2):
                    S.op('act', lambda e, hp=hp: e.copy(out=AZ[:, :, hp, hp * 64:(hp + 1) * 64], in_=Xs[:, :, hp, 0:64]), reads=Xt, writes=[AZ])
                    S.op('pool', lambda e, hp=hp: e.tensor_copy(out=UVZ[:, :, hp, hp * 64:(hp + 1) * 64], in_=Xs[:, :, hp, 64:128]), reads=Xt, writes=[UVZ])
                S.op('pool', lambda e: e.tensor_copy(out=Ap[:, :, :].rearrange("p a (h c) -> p a h c", h=2), in_=Xs[:, :, :, 0:64]), reads=Xt, writes=[Ap])
                S.op('dve', lambda e: e.tensor_copy(out=UVp[:, :, :].rearrange("p a (h c) -> p a h c", h=2), in_=Xs[:, :, :, 64:128]), reads=Xt, writes=[UVp])
                bR = nb(); bG = nb()
                for jc in range(4):
                    for hp in range(2):
                        S.op('pe', lambda e, jc=jc, hp=hp: e.matmul(bR[:, jc * 128:(jc + 1) * 128], lhsT=AZ[:, jc, hp, :], rhs=SCT[:, jc, hp, 128:256],
                                                                    start=(hp == 0), stop=(hp == 1)), reads=[AZ, SCT], writes=[bR])
                for jc in range(4):
                    S.op('pe', lambda e, jc=jc: e.matmul(bG[:, jc * 128:(jc + 1) * 128], lhsT=Ap[:, jc, :], rhs=BH[:, jc, :], start=True, stop=True),
                         reads=[Ap, BH], writes=[bG])
                S.op('dve', lambda e: e.tensor_tensor(out=RhT[:, :, :], in0=v3(bR, 4), in1=SCin[:, :, 1, :], op=ALU.add), reads=[bR, SCin], writes=[RhT])
                S.op('dve', lambda e: e.tensor_tensor(out=GZ[:, :, :], in0=v3(bG, 4), in1=bmf, op=ALU.mult), reads=[bG, tabf], writes=[GZ])
                if _stop(9):
                    return
                bY = nb(); bH = nb()
                for jc in range(4):
                    for hp in range(2):
                        S.op('pe', lambda e, jc=jc, hp=hp: e.matmul(bY[:, jc * 128:(jc + 1) * 128], lhsT=UVZ[:, jc, hp, :], rhs=SCT[:, jc, hp, 128:256],
                                                                    start=(hp == 0), stop=False), reads=[UVZ, SCT], writes=[bY])
                        S.op('pe', lambda e, jc=jc, hp=hp: e.matmul(bY[:, jc * 128:(jc + 1) * 128], lhsT=VZ[:, jc, hp, :], rhs=SCT[:, jc, hp, 384:512],
                                                                    start=False, stop=False), reads=[VZ, SCT], writes=[bY])
                    S.op('pe', lambda e, jc=jc: e.matmul(bY[:, jc * 128:(jc + 1) * 128], lhsT=HZ[:, jc, :], rhs=RhT[:, jc, :], start=False, stop=True),
                         reads=[HZ, RhT], writes=[bY])
                for jc in range(4):
                    S.op('pe', lambda e, jc=jc: e.matmul(bH[:, jc * 128:(jc + 1) * 128], lhsT=BH[:, jc, :], rhs=UVp[:, jc, :], start=True, stop=False),
                         reads=[BH, UVp], writes=[bH])
                    S.op('pe', lambda e, jc=jc: e.matmul(bH[:, jc * 128:(jc + 1) * 128], lhsT=KH[:, jc, :], rhs=Vp[:, jc, :], start=False, stop=False),
                         reads=[KH, Vp], writes=[bH])
                    S.op('pe', lambda e, jc=jc: e.matmul(bH[:, jc * 128:(jc + 1) * 128], lhsT=GZ[:, jc, :], rhs=HZ[:, jc, :], start=False, stop=True),
                         reads=[GZ, HZ], writes=[bH])
                for jc in range(4):
                    S.op('dve', lambda e, jc=jc: e.scalar_tensor_tensor(out=Hf[:, jc, :], in0=Hf[:, jc, :], scalar=Epos[:, jc, 127:128],
                                                                        in1=bH[:, jc * 128:(jc + 1) * 128], op0=ALU.mult, op1=ALU.add),
                         reads=[Hf, Epos, bH], writes=[Hf])
                S.op('pool', lambda e: e.tensor_tensor(out=Hf[:, :, :], in0=Hf[:, :, :], in1=bmf, op=ALU.mult), reads=[Hf, tabf], writes=[Hf])
                S.op('act', lambda e: e.copy(out=HZ[:, :, :], in_=Hf[:, :, :]), reads=[Hf], writes=[HZ])
                if _stop(10):
                    return
                S.op('act', lambda e: e.copy(out=yb[:, :, :], in_=v3(bY, 4)), reads=[bY], writes=[yb])
                S.op('act', lambda e: e.activation(out=ysq[:, :, :], in_=v3(bY, 4), func=AF.Square), reads=[bY], writes=[ysq])
                S.op('pool', lambda e: e.tensor_tensor(out=t0[:, :, :], in0=pl[:, 0:4, :], in1=kp[:, :, :], op=ALU.mult), reads=[plt[0], kp], writes=[t0])
                S.op('pool', lambda e: e.tensor_tensor(out=rkb[:, :, :], in0=t0[:, :, :], in1=cb(RK, 4, [128, 4, 128]), op=ALU.mult),
                     reads=[t0, cst], writes=[rkb])
                bM = nb(); bQ = nb(); bO = nb()
                for jc in range(4):
                    S.op('pe', lambda e, jc=jc: e.matmul(bM[:, jc * 128:(jc + 1) * 128], lhsT=bones, rhs=yb[:, jc, :], start=True, stop=True),
                         reads=[tabb, yb], writes=[bM])
                    S.op('pe', lambda e, jc=jc: e.matmul(bQ[:, jc * 128:(jc + 1) * 128], lhsT=bones, rhs=ysq[:, jc, :], start=True, stop=True),
                         reads=[tabb, ysq], writes=[bQ])
                    S.op('pe', lambda e, jc=jc: e.matmul(bO[:, jc * 128:(jc + 1) * 128], lhsT=bones, rhs=rkb[:, jc, :], start=True, stop=True),
                         reads=[tabb, rkb], writes=[bO])
                S.op('act', lambda e: e.activation(out=t1[:, :, :], in_=v3(bM, 4), func=AF.Identity, scale=1.0 / 64), reads=[bM], writes=[t1])
                S.op('pool', lambda e: e.tensor_tensor(out=t2[:, :, :], in0=t1[:, :, :], in1=t1[:, :, :], op=ALU.mult), reads=[t1], writes=[t2])
                S.op('dve', lambda e: e.scalar_tensor_tensor(out=t2[:, :, :], in0=v3(bQ, 4), scalar=1.0 / 64, in1=t2[:, :, :],
                                                             op0=ALU.mult, op1=ALU.subtract), reads=[bQ, t2], writes=[t2])
                S.op('act', lambda e: e.activation(out=t2[:, :, :], in_=t2[:, :, :], func=AF.Ln, bias=64e-5), reads=[t2], writes=[t2])
                S.op('act', lambda e: e.activation(out=t2[:, :, :], in_=t2[:, :, :], func=AF.Exp, scale=-0.5), reads=[t2], writes=[t2])
                S.op('dve', lambda e: e.tensor_tensor(out=t0[:, :, :], in0=v3(bY, 4), in1=t1[:, :, :], op=ALU.subtract), reads=[bY, t1], writes=[t0])
                S.op('pool', lambda e: e.tensor_tensor(out=t0[:, :, :], in0=t0[:, :, :], in1=t2[:, :, :], op=ALU.mult), reads=[t0, t2], writes=[t0])
                S.op('pool', lambda e: e.tensor_tensor(out=t0[:, :, :], in0=t0[:, :, :], in1=cb(LW, 4, [128, 4, 128]), op=ALU.mult),
                     reads=[t0, cst], writes=[t0])
                S.op('pool', lambda e: e.tensor_tensor(out=t0[:, :, :], in0=t0[:, :, :], in1=cb(LB, 4, [128, 4, 128]), op=ALU.add),
                     reads=[t0, cst], writes=[t0])
                S.op('dve', lambda e: e.tensor_tensor(out=t1[:, :, :], in0=v3(bO, 4), in1=pl[:, 8:12, :], op=ALU.mult), reads=[bO, plt[2]], writes=[t1])
                S.op('pool', lambda e: e.tensor_tensor(out=t0[:, :, :], in0=t0[:, :, :], in1=t1[:, :, :], op=ALU.add), reads=[t0, t1], writes=[t0])
                S.op('pool', lambda e: e.tensor_tensor(out=yrwT[:, :, :], in0=t0[:, :, :], in1=gT[:, :, :], op=ALU.mult), reads=[t0, gT], writes=[yrwT])
                S.dma('pool', lambda e, ci=ci: e.dma_start(out=yrw_s[ci].rearrange("p (a b) -> p a b", a=4), in_=yrwT[:, :, :]), 'yst', reads=[yrwT])
            for ci in range(NCHUNK):
                _chunk(ci)
            S.barrier()

        if "B" in phases:
          with ExitStack() as st:
            NBC = INC - RWC
            w_b = S.sb(st, [128, 8, NBC], BF16)
            wbrw = S.sb(st, [128, 4, D], BF16); wbret = S.sb(st, [128, 8, D], BF16); wout = S.sb(st, [128, 8, D], BF16)
            for kc in range(8):
                S.dma('pool', lambda e, kc=kc: e.dma_start(out=w_b[:, kc, :], in_=win_d[kc * 128:(kc + 1) * 128, RWC:INC]), 'wB', writes=[w_b])
                S.dma('pool', lambda e, kc=kc: e.dma_start(out=wbret[:, kc, :], in_=wbret_d[kc * 128:(kc + 1) * 128, :]), 'wB', writes=[wbret])
                S.dma('pool', lambda e, kc=kc: e.dma_start(out=wout[:, kc, :], in_=wout_d[kc * 128:(kc + 1) * 128, :]), 'wB', writes=[wout])
            for kc in range(4):
                S.dma('pool', lambda e, kc=kc: e.dma_start(out=wbrw[:, kc, :], in_=wbrw_d[kc * 128:(kc + 1) * 128, :]), 'wB', writes=[wbrw])
            _xb = S.sb(st, [128, D], F32); xs2 = [_xb, _xb]
            hnT2 = [S.sb(st, [128, 8, 128], BF16) for _ in range(2)]
            yrw2 = [S.sb(st, [128, 4, 128], BF16) for _ in range(2)]
            posi2 = [S.sb(st, [128, 128], I32) for _ in range(2)]
            posf = S.sb(st, [128, 128], F32); u0 = S.sb(st, [128, 128], F32); u1 = S.sb(st, [128, 128], F32)
            ti = S.sb(st, [128, 128], I32); tf = S.sb(st, [128, 128], F32)
            cosT = S.sb(st, [128, 128], F32); sinT = S.sb(st, [128, 128], F32)
            qk = S.sb(st, [128, 8, 128], F32); qkb = S.sb(st, [128, 8, 128], BF16)
            r1 = S.sb(st, [128, 8, 128], F32); r2 = S.sb(st, [128, 8, 128], F32)
            rot = S.sb(st, [128, 8, 128], BF16); qd = S.sb(st, [128, 4, 128], BF16)
            v_bf = S.sb(st, [128, D], BF16); sgb = S.sb(st, [128, D], BF16); sA = S.sb(st, [128, D], BF16); sB = S.sb(st, [128, D], BF16)
            kdZ = S.sb(st, [128, 4, 2, 128], BF16)
            sT = S.sb(st, [128, 8, 128], BF16)
            Rf = S.sb(st, [128, 4, 128], F32); Rb = S.sb(st, [128, 4, 128], BF16)
            of = qk; osq = r1
            stt = S.sb(st, [128, 16], F32); mean = S.sb(st, [128, 8], F32); var = S.sb(st, [128, 8], F32)
            yret = S.sb(st, [128, D], BF16); yretT = S.sb(st, [128, 8, 128], BF16)
            m1 = S.sb(st, [128, D], F32); m2 = S.sb(st, [128, D], F32); mg = S.sb(st, [128, D], BF16); mT = S.sb(st, [128, 8, 128], BF16)
            mo = m2; junk = mg; ss = S.sb(st, [128, 1], F32); rs = S.sb(st, [128, 1], F32)
            hh = m1
            S.op('pool', lambda e: e.memset(kdZ[:, :, :, :], 0.0), writes=[kdZ])
            DBG.update({k_: v_.name for k_, v_ in list(locals().items()) if isinstance(v_, Buf)})

            def _chunk(ci):
                b_i, c_i = divmod(ci, NCH)
                tok0 = ci * 128
                xs = xs2[ci % 2]; hnT = hnT2[ci % 2]; yrw = yrw2[ci % 2]; posi = posi2[ci % 2]
                S.dma('sp', lambda e, hnT=hnT, ci=ci: e.dma_start(out=hnT[:, :, :], in_=hnT_s[ci].rearrange("p (a b) -> p a b", a=8)),
                      'hB%d' % (ci % 2), writes=[hnT])
                S.dma('sp', lambda e, yrw=yrw, ci=ci: e.dma_start(out=yrw[:, :, :], in_=yrw_s[ci].rearrange("p (a b) -> p a b", a=4)),
                      'yB%d' % (ci % 2), writes=[yrw])
                S.dma('sp', lambda e, posi=posi, tok0=tok0: e.dma_start(out=posi[:, :], in_=pos_d[0:1, tok0:tok0 + 128].partition_broadcast(128)),
                      'pB%d' % (ci % 2), writes=[posi])
                S.dma('sp', lambda e, xs=xs, tok0=tok0: e.dma_start(out=xs[:, :], in_=x_d[tok0:tok0 + 128, :]), 'xB%d' % (ci % 2), writes=[xs])
                if c_i == 0:
                    S.op('pool', lambda e: e.memset(Rf[:, :, :], 0.0), writes=[Rf])
                    S.op('pool', lambda e: e.memset(Rb[:, :, :], 0.0), writes=[Rb])
                S.op('dve', lambda e, posi=posi: e.tensor_copy(out=posf[:, :], in_=posi[:, :]), reads=[posi], writes=[posf])
                S.op('dve', lambda e: e.tensor_scalar(out=u0[:, :], in0=posf[:, :], scalar1=cst[:, INV:INV + 1], scalar2=None, op0=ALU.mult),
                     reads=[posf, cst], writes=[u0])
                S.op('dve', lambda e: e.tensor_copy(out=ti[:, :], in_=u0[:, :]), reads=[u0], writes=[ti])
                S.op('dve', lambda e: e.tensor_copy(out=tf[:, :], in_=ti[:, :]), reads=[ti], writes=[tf])
                S.op('dve', lambda e: e.tensor_tensor(out=tf[:, :], in0=u0[:, :], in1=tf[:, :], op=ALU.subtract), reads=[u0, tf], writes=[tf])
                S.op('act', lambda e: e.activation(out=sinT[:, :], in_=tf[:, :], func=AF.Sin, scale=cst[:, SSC:SSC + 1]), reads=[tf, cst], writes=[sinT])
                S.op('pool', lambda e: e.tensor_scalar(out=u1[:, :], in0=u0[:, :], scalar1=0.25, scalar2=None, op0=ALU.add), reads=[u0], writes=[u1])
                S.op('dve', lambda e: e.tensor_copy(out=ti[:, :], in_=u1[:, :]), reads=[u1], writes=[ti])
                S.op('dve', lambda e: e.tensor_copy(out=tf[:, :], in_=ti[:, :]), reads=[ti], writes=[tf])
                S.op('dve', lambda e: e.tensor_tensor(out=tf[:, :], in0=u1[:, :], in1=tf[:, :], op=ALU.subtract), reads=[u1, tf], writes=[tf])
                S.op('act', lambda e: e.activation(out=cosT[:, :], in_=tf[:, :], func=AF.Sin, scale=2.0 * math.pi), reads=[tf], writes=[cosT])
                for g in range(2):
                    bk = nb()
                    for jj in range(4):
                        j = g * 4 + jj
                        for kc in range(8):
                            S.op('pe', lambda e, bk=bk, jj=jj, j=j, kc=kc, hnT=hnT: e.matmul(
                                bk[:, jj * 128:(jj + 1) * 128], lhsT=w_b[:, kc, j * 128:(j + 1) * 128], rhs=hnT[:, kc, :],
                                start=(kc == 0), stop=(kc == 7)), reads=[w_b, hnT], writes=[bk])
                    S.op('act', lambda e, bk=bk, g=g: e.copy(out=qk[:, g * 4:g * 4 + 4, :], in_=v3(bk, 4)), reads=[bk], writes=[qk])
                S.op('dve', lambda e: e.tensor_copy(out=qkb[:, :, :], in_=qk[:, :, :]), reads=[qk], writes=[qkb])
                for grp in range(4):
                    for half in range(2):
                        bk = nb(); c0 = 1024 + grp * 1024 + half * 512
                        for kc in range(8):
                            S.op('pe', lambda e, bk=bk, kc=kc, c0=c0, hnT=hnT: e.matmul(bk[:, :], lhsT=hnT[:, kc, :], rhs=w_b[:, kc, c0:c0 + 512],
                                                                                        start=(kc == 0), stop=(kc == 7)), reads=[w_b, hnT], writes=[bk])
                        dst = (v_bf, sgb, sA, sB)[grp]
                        fn = (AF.Copy, AF.Silu, AF.Sigmoid, AF.Sigmoid)[grp]
                        S.op('act', lambda e, bk=bk, dst=dst, fn=fn, half=half: e.activation(out=dst[:, half * 512:(half + 1) * 512], in_=bk[:, :], func=fn),
                             reads=[bk], writes=[dst])
                bsw = [nb(), nb()]
                for j in range(8):
                    S.op('pe', lambda e, j=j: e.matmul(bsw[j // 4][:, (j % 4) * 128:(j % 4 + 1) * 128], lhsT=pswap, rhs=qkb[:, j, :], start=True, stop=True),
                         reads=[tabb, qkb], writes=[bsw[j // 4]])
                S.op('pool', lambda e: e.tensor_tensor(out=r1[:, :, :], in0=qk[:, :, :], in1=cosT[:, :].unsqueeze(1).to_broadcast([128, 8, 128]), op=ALU.mult),
                     reads=[qk, cosT], writes=[r1])
                for g in range(2):
                    S.op('dve', lambda e, g=g: e.tensor_tensor(out=r2[:, g * 4:g * 4 + 4, :], in0=v3(bsw[g], 4),
                                                               in1=sinT[:, :].unsqueeze(1).to_broadcast([128, 4, 128]), op=ALU.mult),
                         reads=[bsw[g], sinT], writes=[r2])
                S.op('dve', lambda e: e.tensor_tensor(out=rot[:, :, :], in0=r1[:, :, :], in1=r2[:, :, :], op=ALU.add), reads=[r1, r2], writes=[rot])
                S.op('pool', lambda e: e.tensor_tensor(out=qd[:, :, :], in0=rot[:, 0:4, :], in1=cst[:, QD:QD + 512].rearrange("p (a b) -> p a b", a=4), op=ALU.mult),
                     reads=[rot, cst], writes=[qd])
                bk = nb()
                for jc in range(4):
                    S.op('pe', lambda e, bk=bk, jc=jc: e.transpose(out=vbf(bk)[:, jc * 128:(jc + 1) * 128], in_=rot[:, 4 + jc, :], identity=ident),
                         reads=[rot, tabb], writes=[bk])
                bkv = vbf(bk)[:, 0:512].rearrange("p (a h c) -> p a h c", a=4, h=2)
                kdv = cst[:, KDEC:KDEC + 8].rearrange("p (a h) -> p a h", a=4)
                for hp in range(2):
                    S.op('dve', lambda e, hp=hp, bkv=bkv: e.tensor_tensor(out=kdZ[:, :, hp, hp * 64:(hp + 1) * 64], in0=bkv[:, :, hp, :],
                                                                          in1=kdv[:, :, hp:hp + 1].to_broadcast([128, 4, 64]), op=ALU.mult),
                         reads=[bk, cst], writes=[kdZ])
                for hp in range(2):
                    bk = nb(); pb = 64 * hp
                    for jc in range(4):
                        S.op('pe', lambda e, bk=bk, jc=jc, pb=pb: e.matmul(bk[:, jc * 128:(jc + 1) * 128], lhsT=rot[pb:pb + 64, 4 + jc, :],
                                                                           rhs=rot[pb:pb + 64, jc, :], start=True, stop=True), reads=[rot], writes=[bk])
                    S.op('dve', lambda e, bk=bk, hp=hp: e.tensor_tensor(
                        out=sT[:, :, :].rearrange("p (a h) c -> p a h c", h=2)[:, :, hp, :], in0=v3(bk, 4),
                        in1=tabf[:, T_MT:T_MT + 1024].rearrange("p (a h c) -> p a h c", a=4, h=2)[:, :, hp, :], op=ALU.mult),
                         reads=[bk, tabf], writes=[sT])
                bo = [nb(), nb()]
                for h in range(8):
                    jc = h // 2; pb = 64 * (h % 2); bk = bo[h // 4]; cc = (h % 4) * 128
                    S.op('pe', lambda e, bk=bk, cc=cc, h=h: e.matmul(bk[:, cc:cc + 128], lhsT=sT[:, h, :], rhs=v_bf[:, h * 128:(h + 1) * 128],
                                                                     start=True, stop=False), reads=[sT, v_bf], writes=[bk])
                    S.op('pe', lambda e, bk=bk, cc=cc, jc=jc, pb=pb: e.matmul(bk[:, cc:cc + 128], lhsT=qd[pb:pb + 64, jc, :], rhs=Rb[pb:pb + 64, jc, :],
                                                                              start=False, stop=True), reads=[qd, Rb], writes=[bk])
                for g in range(2):
                    S.op('act', lambda e, g=g: e.copy(out=of[:, g * 4:g * 4 + 4, :], in_=v3(bo[g], 4)), reads=[bo[g]], writes=[of])
                bR = nb()
                for jc in range(4):
                    for hp in range(2):
                        h = 2 * jc + hp
                        S.op('pe', lambda e, jc=jc, hp=hp, h=h: e.matmul(bR[:, jc * 128:(jc + 1) * 128], lhsT=kdZ[:, jc, hp, :], rhs=v_bf[:, h * 128:(h + 1) * 128],
                                                                         start=(hp == 0), stop=(hp == 1)), reads=[kdZ, v_bf], writes=[bR])
                S.op('pool', lambda e: e.tensor_tensor(out=Rf[:, :, :], in0=Rf[:, :, :], in1=cb(CD, 4, [128, 4, 128]), op=ALU.mult), reads=[Rf, cst], writes=[Rf])
                S.op('dve', lambda e: e.tensor_tensor(out=Rf[:, :, :], in0=Rf[:, :, :], in1=v3(bR, 4), op=ALU.add), reads=[Rf, bR], writes=[Rf])
                S.op('act', lambda e: e.copy(out=Rb[:, :, :], in_=Rf[:, :, :]), reads=[Rf], writes=[Rb])
                S.op('dve', lambda e: e.tensor_reduce(out=stt[:, 0:8], in_=of[:, :, :], axis=AX.X, op=ALU.add), reads=[of], writes=[stt])
                S.op('act', lambda e: e.activation(out=osq[:, :, :], in_=of[:, :, :], func=AF.Square), reads=[of], writes=[osq])
                S.op('dve', lambda e: e.tensor_reduce(out=stt[:, 8:16], in_=osq[:, :, :], axis=AX.X, op=ALU.add), reads=[osq], writes=[stt])
                S.op('dve', lambda e: e.tensor_scalar(out=mean[:, :], in0=stt[:, 0:8], scalar1=1.0 / 128, scalar2=None, op0=ALU.mult), reads=[stt], writes=[mean])
                S.op('dve', lambda e: e.tensor_tensor(out=var[:, :], in0=mean[:, :], in1=mean[:, :], op=ALU.mult), reads=[mean], writes=[var])
                S.op('dve', lambda e: e.scalar_tensor_tensor(out=var[:, :], in0=stt[:, 8:16], scalar=1.0 / 128, in1=var[:, :], op0=ALU.mult, op1=ALU.subtract),
                     reads=[stt, var], writes=[var])
                S.op('act', lambda e: e.activation(out=var[:, :], in_=var[:, :], func=AF.Sqrt, bias=1e-5), reads=[var], writes=[var])
                S.op('dve', lambda e: e.reciprocal(out=var[:, :], in_=var[:, :]), reads=[var], writes=[var])
                S.op('dve', lambda e: e.tensor_tensor(out=of[:, :, :], in0=of[:, :, :], in1=mean[:, :].unsqueeze(2).to_broadcast([128, 8, 128]), op=ALU.subtract),
                     reads=[of, mean], writes=[of])
                S.op('dve', lambda e: e.tensor_tensor(out=of[:, :, :], in0=of[:, :, :], in1=var[:, :].unsqueeze(2).to_broadcast([128, 8, 128]), op=ALU.mult),
                     reads=[of, var], writes=[of])
                S.op('dve', lambda e: e.tensor_tensor(out=yret[:, :], in0=of[:, :, :].rearrange("p a b -> p (a b)"), in1=sgb[:, :], op=ALU.mult),
                     reads=[of, sgb], writes=[yret])
                bk = nb()
                for kc in range(8):
                    S.op('pe', lambda e, bk=bk, kc=kc: e.transpose(out=vbf(bk)[:, kc * 128:(kc + 1) * 128], in_=yret[:, kc * 128:(kc + 1) * 128], identity=ident),
                         reads=[yret, tabb], writes=[bk])
                S.op('act', lambda e, bk=bk: e.copy(out=yretT[:, :, :], in_=vbf(bk).rearrange("p (a b) -> p a b", a=8)), reads=[bk], writes=[yretT])
                for half in range(2):
                    b1 = nb(); b2 = nb(); hs = slice(half * 512, (half + 1) * 512)
                    for jc in range(4):
                        S.op('pe', lambda e, b1=b1, jc=jc, hs=hs, yrw=yrw: e.matmul(b1[:, :], lhsT=yrw[:, jc, :], rhs=wbrw[:, jc, hs], start=(jc == 0), stop=(jc == 3)),
                             reads=[yrw, wbrw], writes=[b1])
                    for kc in range(8):
                        S.op('pe', lambda e, b2=b2, kc=kc, hs=hs: e.matmul(b2[:, :], lhsT=yretT[:, kc, :], rhs=wbret[:, kc, hs], start=(kc == 0), stop=(kc == 7)),
                             reads=[yretT, wbret], writes=[b2])
                    S.op('dve', lambda e, b1=b1, hs=hs: e.tensor_tensor(out=m1[:, hs], in0=b1[:, :], in1=sA[:, hs], op=ALU.mult), reads=[b1, sA], writes=[m1])
                    S.op('dve', lambda e, b2=b2, hs=hs: e.tensor_tensor(out=m2[:, hs], in0=b2[:, :], in1=sB[:, hs], op=ALU.mult), reads=[b2, sB], writes=[m2])
                S.op('dve', lambda e: e.tensor_tensor(out=mg[:, :], in0=m1[:, :], in1=m2[:, :], op=ALU.add), reads=[m1, m2], writes=[mg])
                bk = nb()
                for kc in range(8):
                    S.op('pe', lambda e, bk=bk, kc=kc: e.transpose(out=vbf(bk)[:, kc * 128:(kc + 1) * 128], in_=mg[:, kc * 128:(kc + 1) * 128], identity=ident),
                         reads=[mg, tabb], writes=[bk])
                S.op('act', lambda e, bk=bk: e.copy(out=mT[:, :, :], in_=vbf(bk).rearrange("p (a b) -> p a b", a=8)), reads=[bk], writes=[mT])
                for half in range(2):
                    bk = nb(); hs = slice(half * 512, (half + 1) * 512)
                    for kc in range(8):
                        S.op('pe', lambda e, bk=bk, kc=kc, hs=hs: e.matmul(bk[:, :], lhsT=mT[:, kc, :], rhs=wout[:, kc, hs], start=(kc == 0), stop=(kc == 7)),
                             reads=[mT, wout], writes=[bk])
                    S.op('act', lambda e, bk=bk, hs=hs: e.copy(out=mo[:, hs], in_=bk[:, :]), reads=[bk], writes=[mo])
                S.op('act', lambda e: e.activation(out=junk[:, :], in_=mo[:, :], func=AF.Square, accum_out=ss[:, 0:1]), reads=[mo], writes=[junk, ss])
                S.op('act', lambda e: e.activation(out=rs[:, 0:1], in_=ss[:, 0:1], func=AF.Sqrt, scale=1.0 / D, bias=1e-6), reads=[ss], writes=[rs])
                S.op('dve', lambda e: e.reciprocal(out=rs[:, 0:1], in_=rs[:, 0:1]), reads=[rs], writes=[rs])
                S.op('dve', lambda e: e.scalar_tensor_tensor(out=hh[:, :], in0=mo[:, :], scalar=rs[:, 0:1], in1=rows[:, 0, :], op0=ALU.mult, op1=ALU.mult),
                     reads=[mo, rs, rows], writes=[hh])
                S.op('dve', lambda e, xs=xs: e.tensor_tensor(out=hh[:, :], in0=hh[:, :], in1=xs[:, :], op=ALU.add), reads=[hh, xs], writes=[hh])
                S.dma('pool', lambda e, tok0=tok0: e.dma_start(out=h_s[tok0:tok0 + 128, :], in_=hh[:, :]), 'hstore', reads=[hh])
            for ci in range(NCHUNK):
                _chunk(ci)
            S.barrier()

        if "C" in phases:
          with ExitStack() as st:
            wup = S.sb(st, [128, 8, 2 * DFF], BF16); wdn = S.sb(st, [128, 22, D], BF16)
            for kc in range(8):
                S.dma('pool', lambda e, kc=kc: e.dma_start(out=wup[:, kc, :], in_=wup_d[kc * 128:(kc + 1) * 128, :]), 'wC', writes=[wup])
            for j in range(22):
                S.dma('pool', lambda e, j=j: e.dma_start(out=wdn[:, j, :], in_=wdn_d[j * 128:(j + 1) * 128, :]), 'wC', writes=[wdn])
            hb = [S.sb(st, [128, D], F32) for _ in range(NSUB)]
            tmp_bf = S.sb(st, [128, D], BF16); xn = S.sb(st, [128, D], BF16)
            ss = S.sb(st, [128, 1], F32); rs = S.sb(st, [128, 1], F32)
            hn2T = S.sb(st, [128, 8, FT], BF16)
            actT = S.sb(st, [128, 22, FT], BF16)
            ub2 = [S.sb(st, [128, FT + 2], F32) for _ in range(4)]
            acc2 = [S.sb(st, [128, FT], F32) for _ in range(4)]
            gg2 = [S.sb(st, [128, FT], F32) for _ in range(2)]
            carry = S.sb(st, [128, 44, 2], F32)
            fo = S.sb(st, [128, D], F32); oo = S.sb(st, [128, D], F32)
            ucl = [0]

            def _tile(ti_):
                b_i, t_i = divmod(ti_, NTILE)
                tokb = ti_ * FT
                if t_i == 0:
                    S.op('pool', lambda e: e.memset(carry[:, :, :], 0.0), writes=[carry])
                for sc in range(NSUB):
                    S.dma('sp', lambda e, sc=sc, tokb=tokb: e.dma_start(out=hb[sc][:, :], in_=h_s[tokb + sc * 128:tokb + (sc + 1) * 128, :]),
                          'hC%d' % sc, writes=[hb[sc]])
                    rmsnorm_T(st, hb[sc], hn2T, sc * 128, G3, tmp_bf, ss, rs, xn)
                def _finish(jp):
                    ag = acc2[(2 * jp) % 4]; av = acc2[(2 * jp + 1) % 4]; gg = gg2[jp % 2]
                    S.op('act', lambda e: e.activation(out=gg[:, :], in_=ag[:, :], func=AF.Gelu_apprx_tanh), reads=[ag], writes=[gg])
                    S.op('pool', lambda e: e.tensor_tensor(out=actT[:, jp, :], in0=gg[:, :], in1=av[:, :], op=ALU.mult),
                         reads=[gg, av], writes=[actT])

                for jp in range(22):
                    if jp >= 1:
                        pass
                    for which in range(2):
                        j = jp + 22 * which
                        bk = nb(); ub = ub2[(2 * jp + which) % 4]; acc = acc2[(2 * jp + which) % 4]
                        for kc in range(8):
                            S.op('pe', lambda e, bk=bk, kc=kc, j=j: e.matmul(bk[:, 0:FT], lhsT=wup[:, kc, j * 128:(j + 1) * 128], rhs=hn2T[:, kc, :],
                                                                             start=(kc == 0), stop=(kc == 7)), reads=[wup, hn2T], writes=[bk])
                        S.op('act', lambda e, bk=bk, ub=ub: e.copy(out=ub[:, 2:FT + 2], in_=bk[:, 0:FT]), reads=[bk], writes=[ub])
                        S.op('pool', lambda e, ub=ub, j=j: e.tensor_copy(out=ub[:, 0:2], in_=carry[:, j, :]), reads=[carry], writes=[ub])
                        S.op('act', lambda e, bk=bk, acc=acc, j=j: e.activation(out=acc[:, :], in_=bk[:, 0:FT], func=AF.Identity,
                                                                                 scale=cst[:, CW + 2 * 44 + j:CW + 2 * 44 + j + 1], bias=cst[:, CB + j:CB + j + 1]),
                             reads=[bk, cst], writes=[acc])
                        S.op('dve', lambda e, ub=ub, acc=acc, j=j: e.scalar_tensor_tensor(out=acc[:, :], in0=ub[:, 1:FT + 1], scalar=cst[:, CW + 44 + j:CW + 44 + j + 1],
                                                                                         in1=acc[:, :], op0=ALU.mult, op1=ALU.add), reads=[ub, acc, cst], writes=[acc])
                        S.op('dve', lambda e, ub=ub, acc=acc, j=j: e.scalar_tensor_tensor(out=acc[:, :], in0=ub[:, 0:FT], scalar=cst[:, CW + j:CW + j + 1],
                                                                                         in1=acc[:, :], op0=ALU.mult, op1=ALU.add), reads=[ub, acc, cst], writes=[acc])
                        S.op('pool', lambda e, ub=ub, j=j: e.tensor_copy(out=carry[:, j, :], in_=ub[:, FT:FT + 2]), reads=[ub], writes=[carry])
                    if jp >= 1:
                        _finish(jp - 1)
                _finish(21)
                for sc in range(NSUB):
                    for half in range(2):
                        bk = nb(); hs = slice(half * 512, (half + 1) * 512)
                        for j in range(22):
                            S.op('pe', lambda e, bk=bk, j=j, sc=sc, hs=hs: e.matmul(bk[:, :], lhsT=actT[:, j, sc * 128:(sc + 1) * 128], rhs=wdn[:, j, hs],
                                                                                    start=(j == 0), stop=(j == 21)), reads=[actT, wdn], writes=[bk])
                        S.op('act', lambda e, bk=bk, hs=hs: e.copy(out=fo[:, hs], in_=bk[:, :]), reads=[bk], writes=[fo])
                    S.op('act', lambda e: e.activation(out=tmp_bf[:, :], in_=fo[:, :], func=AF.Square, accum_out=ss[:, 0:1]), reads=[fo], writes=[tmp_bf, ss])
                    S.op('act', lambda e: e.activation(out=rs[:, 0:1], in_=ss[:, 0:1], func=AF.Sqrt, scale=1.0 / D, bias=1e-6), reads=[ss], writes=[rs])
                    S.op('dve', lambda e: e.reciprocal(out=rs[:, 0:1], in_=rs[:, 0:1]), reads=[rs], writes=[rs])
                    S.op('dve', lambda e: e.scalar_tensor_tensor(out=oo[:, :], in0=fo[:, :], scalar=rs[:, 0:1], in1=rows[:, 1, :], op0=ALU.mult, op1=ALU.mult),
                         reads=[fo, rs, rows], writes=[oo])
                    S.op('pool', lambda e, sc=sc: e.tensor_tensor(out=oo[:, :], in0=oo[:, :], in1=hb[sc][:, :], op=ALU.add), reads=[oo, hb[sc]], writes=[oo])
                    S.dma('pool', lambda e, sc=sc, tokb=tokb: e.dma_start(out=out_d[tokb + sc * 128:tokb + (sc + 1) * 128, :], in_=oo[:, :]), 'ostore', reads=[oo])
            for ti_ in range(NSEQ * NTILE):
                _tile(ti_)
            S.barrier()
        S.barrier()
        S.emit()
    return nc


def host_consts(inp):
    f = np.float32
    c = np.zeros((128, NCONST), f)

    def pk(v, n):
        return np.asarray(v, f).reshape(n, 128).T
    c[:, G1:G1 + 8] = pk(inp["norm_mix_pre"][0], 8)
    c[:, MU:MU + 14] = pk(inp["rw_mu"][0], 14)
    c[:, W0:W0 + 4] = pk(inp["rw_w0"][0], 4)
    c[:, A0:A0 + 4] = pk(inp["rw_a0"][0], 4)
    c[:, KK:KK + 4] = pk(inp["rw_k_k"][0], 4)
    c[:, KA:KA + 4] = pk(inp["rw_k_a"][0], 4)
    c[:, RK:RK + 4] = pk(inp["rw_r_k"][0], 4)
    c[:, LW:LW + 4] = pk(inp["rw_lnx_w"][0], 4)
    c[:, LB:LB + 4] = pk(inp["rw_lnx_b"][0], 4)
    c[:, G3:G3 + 8] = pk(inp["norm_ffn_pre"][0], 8)
    c[:, CB:CB + 44] = pk(inp["ffn_conv_b"][0], 44)
    for tap in range(3):
        c[:, CW + tap * 44:CW + (tap + 1) * 44] = pk(inp["ffn_conv_w"][0, tap], 44)
    p = np.arange(128)
    inv = (10000.0 ** (-(np.arange(32, dtype=np.float32)) / np.float32(32))).astype(f)
    c[:, INV] = inv[p % 32] / f(2 * math.pi)
    c[:, SSC] = np.where((p % 64) < 32, -2 * math.pi, 2 * math.pi).astype(f)
    lg = np.log1p(-np.exp2(-5.0 - np.arange(8, dtype=np.float64)))
    for jc in range(4):
        hsel = 2 * jc + p // 64
        c[:, CD + jc] = np.exp(128 * lg[hsel])
        c[:, QD + jc * 128:QD + (jc + 1) * 128] = np.exp((np.arange(128)[None, :] + 1.0) * lg[hsel][:, None])
    for h in range(8):
        c[:, KDEC + h] = 0.125 * np.exp((127.0 - p) * lg[h])
    tab = np.zeros((128, NTAB), f)
    s = np.arange(128)[:, None]; t = np.arange(128)[None, :]
    strict = (t > s).astype(f); incl = (t >= s).astype(f)
    tab[:, T_MP:T_MP + 512] = np.concatenate([strict, incl, strict, incl], axis=1)
    tab[:, T_M0:T_M0 + 128] = (t < s).astype(f)
    for h in range(8):
        tab[:, T_MT + h * 128:T_MT + (h + 1) * 128] = np.where(t >= s, 0.125 * np.exp(np.maximum(t - s, 0) * lg[h]), 0.0)
    tab[:, T_BM:T_BM + 128] = ((s // 64) == (t // 64)).astype(f)
    tab[:, T_ID:T_ID + 128] = np.eye(128, dtype=f)
    tab[:, T_SW:T_SW + 128] = (t == (s ^ 32)).astype(f)
    lr = np.concatenate([np.concatenate([inp["rw_w2"][0], inp["rw_a2"][0]], axis=0), inp["rw_g2"][0]], axis=1).astype(f)
    rows = np.stack([inp["norm_mix_post"][0], inp["norm_ffn_post"][0]], axis=0).astype(f)
    return c, tab, lr, rows


_NC_CACHE = {}
STOP = [None]


def _stop(n):
    return STOP[0] is not None and n >= STOP[0]

DBG = {}


def run(inp, NSEQ, NCH, ncores, dbg=False, phases="ABC"):
    key = (NSEQ, NCH, dbg, phases, STOP[0])
    if key not in _NC_CACHE:
        _NC_CACHE[key] = build(NSEQ, NCH, dbg, phases)
    nc = _NC_CACHE[key]
    c, tab, lr, rows = host_consts(inp)
    T = NCH * 128
    shared = {
        "w_in": np.ascontiguousarray(inp["w_in"][0]), "wbrw": np.ascontiguousarray(inp["w_branch_rw"][0]),
        "wbret": np.ascontiguousarray(inp["w_branch_ret"][0]), "wout": np.ascontiguousarray(inp["w_out"][0]),
        "wup": np.ascontiguousarray(inp["ffn_w_up"][0]), "wdn": np.ascontiguousarray(inp["ffn_w_down"][0]),
        "lr": lr, "c128": c, "rows": rows, "tab": tab,
    }
    in_maps = []
    for i in range(ncores):
        xs = np.ascontiguousarray(inp["x"][i * NSEQ:(i + 1) * NSEQ, :T, :]).reshape(NSEQ * T, D)
        ps = np.ascontiguousarray(inp["positions"][i * NSEQ:(i + 1) * NSEQ, :T]).reshape(1, NSEQ * T).astype(np.int32)
        m = dict(shared); m["x"] = xs; m["pos"] = ps
        in_maps.append(m)
    res = run_bass_kernel_spmd(nc, in_maps, core_ids=list(range(ncores)))
    return res


def kernel(**inputs):
    inp = {k: np.asarray(v) for k, v in inputs.items()}
    B, T, _ = inp["x"].shape
    ncores = 8
    NSEQ = B // ncores
    res = run(inp, NSEQ, T // 128, ncores)
    out = np.concatenate([r["out"].reshape(NSEQ, T, D) for r in res.results], axis=0)
    return out.astype(np.float32)
```

```python
import math
import numpy as np
from contextlib import ExitStack
import concourse.bass as bass
import concourse.mybir as mybir
from concourse.bass_utils import run_bass_kernel_spmd

F32 = mybir.dt.float32; BF16 = mybir.dt.bfloat16; I32 = mybir.dt.int32
AF = mybir.ActivationFunctionType; ALU = mybir.AluOpType; AX = mybir.AxisListType

D = 1024; RWC = 1792; INC = 6912; DFF = 2816
C0 = math.exp(-0.5)
G1, MU, W0, A0, KK, KA, RK, LW, LB, INV, SSC, CD, G3, CB, CW, QD, KDEC, NCONST = (
    0, 8, 22, 26, 30, 34, 38, 42, 46, 50, 51, 52, 56, 64, 108, 240, 752, 760)
T_MP, T_M0, T_MT, T_BM, T_ID, T_SW, NTAB = 0, 512, 640, 1664, 1792, 1920, 2048


class Buf:
    def __init__(self, t, name):
        self.t = t; self.name = name; self.writers = {}; self.readers = {}

    def __getitem__(self, k):
        return self.t[k]


class Sched:
    ENG = ['pe', 'act', 'dve', 'pool', 'sp']

    def __init__(self, nc, stack):
        self.nc = nc; self.stack = stack; self.semh = {}
        for e in self.ENG:
            self.semh[e] = stack.enter_context(nc.semaphore("s_" + e))
        self.cnt = {e: 0 for e in self.ENG}
        self.known = {e: {} for e in self.ENG}
        self.prog = {e: [] for e in self.ENG}
        self.dcnt = {}
        self.nbuf = 0

    def sb(self, st, shape, dt):
        self.nbuf += 1
        name = f"b{self.nbuf}"
        return Buf(st.enter_context(self.nc.sbuf_tensor(name, list(shape), dt)), name)

    def ps(self, st, shape, dt):
        self.nbuf += 1
        name = f"p{self.nbuf}"
        b = Buf(st.enter_context(self.nc.psum_tensor(name, list(shape), dt)), name)
        b.psum = True
        return b

    def _waits(self, eng, reads, writes):
        need = {}

        def add(k, v, raw):
            if k == eng and not raw and eng == 'pe':
                return
            if need.get(k, 0) < v:
                need[k] = v
        for b in reads:
            for k, v in b.writers.items():
                add(k, v, True)
        for b in writes:
            for k, v in b.writers.items():
                add(k, v, False)
            for k, v in b.readers.items():
                add(k, v, False)
        out = []
        for k, v in need.items():
            if k.startswith('d_'):
                v = self.dcnt[k]
            if self.known[eng].get(k, 0) >= v:
                continue
            self.known[eng][k] = v
            out.append((self.semh[k], v))
        return out

    def op(self, eng, fn, reads=(), writes=()):
        pr = [b for b in reads if getattr(b, 'psum', False)]
        if pr:
            writes = list(writes) + [b for b in pr if b not in writes]
            reads = [b for b in reads if not getattr(b, 'psum', False)]
        waits = self._waits(eng, reads, writes)
        self.cnt[eng] += 1
        n = self.cnt[eng]
        sem = self.semh[eng]

        def run(e, waits=waits, fn=fn, sem=sem):
            for s, v in waits:
                e.wait_ge(s, v)
            fn(e).then_inc(sem, 1)
        self.prog[eng].append(run)
        for b in reads:
            if b.readers.get(eng, 0) < n:
                b.readers[eng] = n
        for b in writes:
            b.writers = {eng: n}; b.readers = {}

    def dma(self, q, fn, semname, reads=(), writes=()):
        key = 'd_' + semname
        if key not in self.semh:
            self.semh[key] = self.stack.enter_context(self.nc.semaphore(key))
            self.dcnt[key] = 0
        waits = self._waits(q, reads, writes)
        self.dcnt[key] += 16
        n = self.dcnt[key]
        sem = self.semh[key]

        def run(e, waits=waits, fn=fn, sem=sem):
            for s, v in waits:
                e.wait_ge(s, v)
            fn(e).then_inc(sem, 16)
        self.prog[q].append(run)
        for b in reads:
            if b.readers.get(key, 0) < n:
                b.readers[key] = n
        for b in writes:
            b.writers = dict(b.writers); b.writers[key] = n
            b.readers = {}

    def barrier(self):
        allc = {}
        for e in self.ENG:
            if self.cnt[e] > 0:
                allc[e] = self.cnt[e]
        for k, v in self.dcnt.items():
            if v > 0:
                allc[k] = v
        for e in self.ENG:
            waits = []
            for k, v in allc.items():
                if self.known[e].get(k, 0) >= v:
                    continue
                self.known[e][k] = v
                waits.append((self.semh[k], v))

            def run(en, waits=waits):
                for s, v in waits:
                    en.wait_ge(s, v)
            self.prog[e].append(run)

    def emit(self):
        nc = self.nc
        with nc.Block() as block:
            @block.tensor
            def _(e):
                for f in self.prog['pe']:
                    f(e)

            @block.scalar
            def _(e):
                for f in self.prog['act']:
                    f(e)

            @block.vector
            def _(e):
                for f in self.prog['dve']:
                    f(e)

            @block.gpsimd
            def _(e):
                for f in self.prog['pool']:
                    f(e)

            @block.sync
            def _(e):
                for f in self.prog['sp']:
                    f(e)


def build(NSEQ, NCH, dbg=False, phases="ABC"):
    T = NCH * 128; NTOK = NSEQ * T; NCHUNK = NSEQ * NCH
    FT = min(512, T); NSUB = FT // 128; NTILE = T // FT
    nc = bass.Bass("TRN2", target_bir_lowering=False)

    def dr(name, shape, dt, kind="ExternalInput"):
        return nc.dram_tensor(name, list(shape), dt, kind=kind).ap()
    x_d = dr("x", [NTOK, D], F32)
    pos_d = dr("pos", [1, NTOK], I32)
    win_d = dr("w_in", [D, INC], F32)
    wbrw_d = dr("wbrw", [512, D], F32)
    wbret_d = dr("wbret", [D, D], F32)
    wout_d = dr("wout", [D, D], F32)
    wup_d = dr("wup", [D, 2 * DFF], F32)
    wdn_d = dr("wdn", [DFF, D], F32)
    lr_d = dr("lr", [128, 1024], F32)
    c_d = dr("c128", [128, NCONST], F32)
    rows_d = dr("rows", [2, D], F32)
    tab_d = dr("tab", [128, NTAB], F32)
    out_d = dr("out", [NTOK, D], F32, kind="ExternalOutput")
    sk = "ExternalOutput" if dbg else "Internal"
    hnT_s = dr("hnT_s", [NCHUNK, 128, 1024], BF16, kind=sk)
    yrw_s = dr("yrw_s", [NCHUNK, 128, 512], BF16, kind=sk)
    h_s = dr("h_s", [NTOK, D], F32, kind=sk)

    with ExitStack() as st0:
        S = Sched(nc, st0)
        banks = [S.ps(st0, [128, 512], F32) for _ in range(8)]
        bstate = [0]

        def nb():
            b = banks[bstate[0] % 8]; bstate[0] += 1
            return b

        def v3(b, a):
            return b.t[:, :].rearrange("p (a b) -> p a b", a=a)

        def vbf(b):
            return b.t[:, :].bitcast(BF16)

        cst = S.sb(st0, [128, NCONST], F32)
        ones = S.sb(st0, [128, 128], F32)
        omka = S.sb(st0, [128, 4], F32)
        stAB = ExitStack()
        tabf = S.sb(stAB, [128, NTAB], F32)
        tabb = S.sb(stAB, [128, NTAB], BF16)
        rowsB = S.sb(stAB, [128, D], F32)
        S.dma('sp', lambda e: e.dma_start(out=cst[:, :], in_=c_d), 'cst', writes=[cst])
        S.dma('sp', lambda e: e.dma_start(out=tabf[:, :], in_=tab_d), 'tabf', writes=[tabf])
        S.dma('pool', lambda e: e.dma_start(out=tabb[:, :], in_=tab_d), 'tabb', writes=[tabb])
        S.dma('sp', lambda e: e.dma_start(out=rowsB[:, :], in_=rows_d[0:1, :].partition_broadcast(128)), 'rows', writes=[rowsB])
        S.op('pool', lambda e: e.memset(ones[:, :], 1.0), writes=[ones])
        S.op('dve', lambda e: e.tensor_scalar(out=omka[:, :], in0=cst[:, KA:KA + 4], scalar1=-1.0, scalar2=1.0,
                                              op0=ALU.mult, op1=ALU.add), reads=[cst], writes=[omka])
        ident = tabb[:, T_ID:T_ID + 128]
        bones = tabb[:, T_BM:T_BM + 128]
        pswap = tabb[:, T_SW:T_SW + 128]

        def cb(off, n, shape):
            return cst[:, off:off + n].unsqueeze(2).to_broadcast(shape)

        def rmsnorm_T(stp, xs, dstT, col0, gcol, tmp_bf, ss, rs, xn, use_ln=False, idn=None):
            idap, idbuf = idn if idn is not None else (ident, tabb)
            S.op('act', lambda e: e.activation(out=tmp_bf[:, :], in_=xs[:, :], func=AF.Square, accum_out=ss[:, 0:1]),
                 reads=[xs], writes=[tmp_bf, ss])
            if use_ln:
                S.op('act', lambda e: e.activation(out=rs[:, 0:1], in_=ss[:, 0:1], func=AF.Ln, scale=1.0 / D, bias=1e-6),
                     reads=[ss], writes=[rs])
                S.op('act', lambda e: e.activation(out=rs[:, 0:1], in_=rs[:, 0:1], func=AF.Exp, scale=-0.5), reads=[rs], writes=[rs])
            else:
                S.op('act', lambda e: e.activation(out=rs[:, 0:1], in_=ss[:, 0:1], func=AF.Sqrt, scale=1.0 / D, bias=1e-6),
                     reads=[ss], writes=[rs])
                S.op('dve', lambda e: e.reciprocal(out=rs[:, 0:1], in_=rs[:, 0:1]), reads=[rs], writes=[rs])
            S.op('dve', lambda e: e.tensor_scalar(out=xn[:, :], in0=xs[:, :], scalar1=rs[:, 0:1], scalar2=None,
                                                  op0=ALU.mult), reads=[xs, rs], writes=[xn])
            bk = nb()
            for kc in range(8):
                S.op('pe', lambda e, kc=kc: e.transpose(out=vbf(bk)[:, kc * 128:(kc + 1) * 128],
                                                        in_=xn[:, kc * 128:(kc + 1) * 128], identity=idap),
                     reads=[xn, idbuf], writes=[bk])
            S.op('dve', lambda e: e.tensor_tensor(
                out=dstT[:, :, col0:col0 + 128],
                in0=vbf(bk).rearrange("p (a b) -> p a b", a=8),
                in1=cb(gcol, 8, [128, 8, 128]), op=ALU.mult), reads=[bk, cst], writes=[dstT])

        if "A" in phases:
          with ExitStack() as st:
            w_rw = S.sb(st, [128, 8, RWC], BF16)
            lrb = S.sb(st, [128, 1024], BF16)
            for kc in range(8):
                S.dma('pool', lambda e, kc=kc: e.dma_start(out=w_rw[:, kc, :], in_=win_d[kc * 128:(kc + 1) * 128, 0:RWC]),
                      'wA', writes=[w_rw])
            S.dma('pool', lambda e: e.dma_start(out=lrb[:, :], in_=lr_d), 'wA', writes=[lrb])
            xs2 = [S.sb(st, [128, D], F32) for _ in range(2)]
            tmp_bf = S.sb(st, [128, D], BF16); xn = S.sb(st, [128, D], BF16)
            ss = S.sb(st, [128, 1], F32); rs = S.sb(st, [128, 1], F32)
            hnT = S.sb(st, [128, 8, 128], BF16)
            praw = S.sb(st, [128, 14, 129], F32)
            pl = S.sb(st, [128, 14, 128], F32)
            lr12 = S.sb(st, [128, 128], BF16); sg = S.sb(st, [128, 128], BF16)
            f = [S.sb(st, [128, 4, 128], F32) for _ in range(16)]
            (gT, sig, cs, csx, Epos, Eneg, Em, Ehat, al, kq, kkb, kp, bv, t0, t1, t2) = f
            sqb = S.sb(st, [128, 4, 128], BF16)
            SCin = S.sb(st, [128, 4, 2, 128], BF16)
            BtT = S.sb(st, [128, 4, 128], BF16); KtT = S.sb(st, [128, 4, 128], BF16)
            BhT = S.sb(st, [128, 4, 128], BF16); KhT = S.sb(st, [128, 4, 128], BF16)
            vTb = S.sb(st, [128, 4, 128], BF16)
            Vp = S.sb(st, [128, 4, 128], BF16); BH = S.sb(st, [128, 4, 128], BF16); KH = S.sb(st, [128, 4, 128], BF16)
            VZ = S.sb(st, [128, 4, 2, 128], BF16); AZ = S.sb(st, [128, 4, 2, 128], BF16); UVZ = S.sb(st, [128, 4, 2, 128], BF16)
            Ap = S.sb(st, [128, 4, 128], BF16); UVp = S.sb(st, [128, 4, 128], BF16)
            SCT = S.sb(st, [128, 4, 2, 512], BF16)
            Xs = S.sb(st, [128, 4, 2, 128], BF16)
            MN = S.sb(st, [128, 4, 2, 256], BF16)
            Xt = [Buf(Xs.t, 'Xt%d' % q_) for q_ in range(4)]
            MNt = [Buf(MN.t, 'MNt%d' % q_) for q_ in range(4)]
            RhT = S.sb(st, [128, 4, 128], BF16); GZ = S.sb(st, [128, 4, 128], BF16)
            HZ = S.sb(st, [128, 4, 128], BF16); Hf = S.sb(st, [128, 4, 128], F32)
            yb = S.sb(st, [128, 4, 128], BF16); ysq = S.sb(st, [128, 4, 128], BF16); rkb = S.sb(st, [128, 4, 128], BF16)
            yrwT = S.sb(st, [128, 4, 128], BF16)
            for z in (VZ, AZ, UVZ):
                S.op('pool', lambda e, z=z: e.memset(z[:, :, :, :], 0.0), writes=[z])
            bmf = tabf[:, T_BM:T_BM + 128].unsqueeze(1).to_broadcast([128, 4, 128])
            prt = [Buf(praw.t, 'prt%d' % q_) for q_ in range(4)]
            plt = [Buf(pl.t, 'plt%d' % q_) for q_ in range(4)]
            tmpS = [S.sb(st, [128, 512], BF16) for _ in range(2)]

            DBG.update({k_: v_.name for k_, v_ in list(locals().items()) if isinstance(v_, Buf)})

            def _chunk(ci):
                b_i, c_i = divmod(ci, NCH)
                tok0 = ci * 128
                xs = xs2[ci % 2]
                S.dma('sp', lambda e, xs=xs, tok0=tok0: e.dma_start(out=xs[:, :], in_=x_d[tok0:tok0 + 128, :]),
                      'xA%d' % (ci % 2), writes=[xs])
                if c_i == 0:
                    S.op('pool', lambda e: e.memset(praw[:, :, 0:1], 0.0), writes=prt)
                    S.op('pool', lambda e: e.memset(Hf[:, :, :], 0.0), writes=[Hf])
                    S.op('pool', lambda e: e.memset(HZ[:, :, :], 0.0), writes=[HZ])
                rmsnorm_T(st, xs, hnT, 0, G1, tmp_bf, ss, rs, xn, use_ln=True)
                S.dma('sp', lambda e, ci=ci: e.dma_start(out=hnT_s[ci].rearrange("p (a b) -> p a b", a=8), in_=hnT[:, :, :]),
                      'hst', reads=[hnT])
                if _stop(1):
                    return
                def proj_group(g):
                    bk = nb(); n = 4 if g < 3 else 2
                    j0 = g * 4
                    for jj in range(n):
                        j = j0 + jj
                        for kc in range(8):
                            S.op('pe', lambda e, jj=jj, j=j, kc=kc: e.matmul(
                                bk[:, jj * 128:(jj + 1) * 128], lhsT=w_rw[:, kc, j * 128:(j + 1) * 128], rhs=hnT[:, kc, :],
                                start=(kc == 0), stop=(kc == 7)), reads=[w_rw, hnT], writes=[bk])
                    S.op('act', lambda e: e.copy(out=praw[:, j0:j0 + n, 1:129], in_=v3(bk, 4)[:, 0:n, :]), reads=[bk], writes=[prt[g]])
                    S.op('dve', lambda e: e.tensor_tensor(out=pl[:, j0:j0 + n, :], in0=praw[:, j0:j0 + n, 0:128], in1=praw[:, j0:j0 + n, 1:129],
                                                          op=ALU.subtract), reads=[prt[g]], writes=[plt[g]])
                    S.op('pool', lambda e: e.tensor_tensor(out=pl[:, j0:j0 + n, :], in0=pl[:, j0:j0 + n, :], in1=cb(MU + j0, n, [128, n, 128]),
                                                           op=ALU.mult), reads=[plt[g], cst], writes=[plt[g]])
                    S.op('dve', lambda e: e.tensor_tensor(out=pl[:, j0:j0 + n, :], in0=pl[:, j0:j0 + n, :], in1=praw[:, j0:j0 + n, 1:129],
                                                          op=ALU.add), reads=[plt[g], prt[g]], writes=[plt[g]])
                    S.op('act', lambda e: e.copy(out=praw[:, j0:j0 + n, 0:1], in_=praw[:, j0:j0 + n, 128:129]), reads=[prt[g]], writes=[prt[g]])

                proj_group(3)
                proj_group(1)
                if _stop(2):
                    return
                S.op('act', lambda e: e.activation(out=lr12[0:64, :], in_=pl[0:64, 12, :], func=AF.Tanh), reads=[plt[3]], writes=[lr12])
                S.op('act', lambda e: e.copy(out=lr12[64:128, :], in_=pl[64:128, 12, :]), reads=[plt[3]], writes=[lr12])
                S.op('act', lambda e: e.activation(out=sg[:, :], in_=pl[:, 13, :], func=AF.Sigmoid), reads=[plt[3]], writes=[sg])
                proj_group(0)
                bw = nb(); ba = nb(); bg = nb()
                for jc in range(4):
                    S.op('pe', lambda e, jc=jc: e.matmul(bw[:, jc * 128:(jc + 1) * 128], lhsT=lrb[0:64, jc * 128:(jc + 1) * 128],
                                                         rhs=lr12[0:64, :], start=True, stop=True), reads=[lrb, lr12], writes=[bw])
                for jc in range(4):
                    S.op('pe', lambda e, jc=jc: e.matmul(ba[:, jc * 128:(jc + 1) * 128], lhsT=lrb[64:128, jc * 128:(jc + 1) * 128],
                                                         rhs=lr12[64:128, :], start=True, stop=True), reads=[lrb, lr12], writes=[ba])
                for jc in range(4):
                    S.op('pe', lambda e, jc=jc: e.matmul(bg[:, jc * 128:(jc + 1) * 128], lhsT=lrb[:, 512 + jc * 128:512 + (jc + 1) * 128],
                                                         rhs=sg[:, :], start=True, stop=True), reads=[lrb, sg], writes=[bg])
                proj_group(2)
                if _stop(3):
                    return
                S.op('dve', lambda e: e.tensor_tensor(out=t0[:, :, :], in0=v3(bw, 4), in1=cb(W0, 4, [128, 4, 128]), op=ALU.add),
                     reads=[bw, cst], writes=[t0])
                S.op('act', lambda e: e.activation(out=sig[:, :, :], in_=t0[:, :, :], func=AF.Sigmoid), reads=[t0], writes=[sig])
                S.op('dve', lambda e: e.tensor_tensor(out=t2[:, :, :], in0=v3(ba, 4), in1=cb(A0, 4, [128, 4, 128]), op=ALU.add),
                     reads=[ba, cst], writes=[t2])
                S.op('act', lambda e: e.activation(out=al[:, :, :], in_=t2[:, :, :], func=AF.Sigmoid), reads=[t2], writes=[al])
                S.op('act', lambda e: e.copy(out=gT[:, :, :], in_=v3(bg, 4)), reads=[bg], writes=[gT])
                for jc in range(4):
                    S.op('dve', lambda e, jc=jc: e.tensor_tensor_scan(out=cs[:, jc, :], data0=ones[:, :], data1=sig[:, jc, :],
                                                                      initial=0.0, op0=ALU.mult, op1=ALU.add),
                         reads=[ones, sig], writes=[cs])
                S.op('pool', lambda e: e.tensor_tensor(out=csx[:, :, :], in0=cs[:, :, :], in1=sig[:, :, :], op=ALU.subtract),
                     reads=[cs, sig], writes=[csx])
                S.op('act', lambda e: e.activation(out=Epos[:, :, :], in_=cs[:, :, :], func=AF.Exp, scale=-C0), reads=[cs], writes=[Epos])
                S.op('act', lambda e: e.activation(out=Eneg[:, :, :], in_=cs[:, :, :], func=AF.Exp, scale=C0), reads=[cs], writes=[Eneg])
                S.op('act', lambda e: e.activation(out=Em[:, :, :], in_=csx[:, :, :], func=AF.Exp, scale=-C0), reads=[csx], writes=[Em])
                S.op('dve', lambda e: e.tensor_tensor(out=t1[:, :, :], in0=cs[:, :, 127:128].to_broadcast([128, 4, 128]),
                                                      in1=cs[:, :, :], op=ALU.subtract), reads=[cs], writes=[t1])
                S.op('act', lambda e: e.activation(out=Ehat[:, :, :], in_=t1[:, :, :], func=AF.Exp, scale=-C0), reads=[t1], writes=[Ehat])
                S.op('pool', lambda e: e.tensor_tensor(out=kq[:, :, :], in0=pl[:, 4:8, :], in1=cb(KK, 4, [128, 4, 128]), op=ALU.mult),
                     reads=[plt[1], cst], writes=[kq])
                S.op('pool', lambda e: e.tensor_tensor(out=sqb[:, :, :], in0=kq[:, :, :], in1=kq[:, :, :], op=ALU.mult),
                     reads=[kq], writes=[sqb])
                bs = nb()
                for jc in range(4):
                    S.op('pe', lambda e, jc=jc: e.matmul(bs[:, jc * 128:(jc + 1) * 128], lhsT=bones, rhs=sqb[:, jc, :],
                                                         start=True, stop=True), reads=[tabb, sqb], writes=[bs])
                S.op('dve', lambda e: e.tensor_scalar(out=t0[:, :, :], in0=v3(bs, 4), scalar1=1e-24, scalar2=None, op0=ALU.max),
                     reads=[bs], writes=[t0])
                S.op('act', lambda e: e.activation(out=t0[:, :, :], in_=t0[:, :, :], func=AF.Ln, scale=float(2.0 ** 40)), reads=[t0], writes=[t0])
                S.op('act', lambda e: e.activation(out=t0[:, :, :], in_=t0[:, :, :], func=AF.Exp, scale=-0.5, bias=20.0 * math.log(2.0)),
                     reads=[t0], writes=[t0])
                S.op('dve', lambda e: e.tensor_tensor(out=kkb[:, :, :], in0=kq[:, :, :], in1=t0[:, :, :], op=ALU.mult),
                     reads=[kq, t0], writes=[kkb])
                S.op('pool', lambda e: e.tensor_tensor(out=t2[:, :, :], in0=al[:, :, :], in1=cb(KA, 4, [128, 4, 128]), op=ALU.mult),
                     reads=[al, cst], writes=[t2])
                S.op('pool', lambda e: e.tensor_tensor(out=t2[:, :, :], in0=t2[:, :, :],
                                                       in1=omka[:, :].unsqueeze(2).to_broadcast([128, 4, 128]), op=ALU.add),
                     reads=[t2, omka], writes=[t2])
                S.op('dve', lambda e: e.tensor_tensor(out=kp[:, :, :], in0=pl[:, 4:8, :], in1=t2[:, :, :], op=ALU.mult),
                     reads=[plt[1], t2], writes=[kp])
                if _stop(4):
                    return
                S.op('pool', lambda e: e.tensor_tensor(out=t1[:, :, :], in0=kkb[:, :, :], in1=Em[:, :, :], op=ALU.mult),
                     reads=[kkb, Em], writes=[t1])
                S.op('act', lambda e: e.activation(out=SCin[:, :, 0, :], in_=t1[:, :, :], func=AF.Identity, scale=-1.0),
                     reads=[t1], writes=[SCin])
                S.op('pool', lambda e: e.tensor_tensor(out=bv[:, :, :], in0=kkb[:, :, :], in1=al[:, :, :], op=ALU.mult),
                     reads=[kkb, al], writes=[bv])
                S.op('dve', lambda e: e.tensor_tensor(out=BtT[:, :, :], in0=bv[:, :, :], in1=Eneg[:, :, :], op=ALU.mult),
                     reads=[bv, Eneg], writes=[BtT])
                S.op('pool', lambda e: e.tensor_tensor(out=KtT[:, :, :], in0=kp[:, :, :], in1=Eneg[:, :, :], op=ALU.mult),
                     reads=[kp, Eneg], writes=[KtT])
                S.op('dve', lambda e: e.tensor_tensor(out=SCin[:, :, 1, :], in0=pl[:, 0:4, :], in1=Epos[:, :, :], op=ALU.mult),
                     reads=[plt[0], Epos], writes=[SCin])
                S.op('pool', lambda e: e.tensor_tensor(out=BhT[:, :, :], in0=bv[:, :, :], in1=Ehat[:, :, :], op=ALU.mult),
                     reads=[bv, Ehat], writes=[BhT])
                S.op('dve', lambda e: e.tensor_tensor(out=KhT[:, :, :], in0=kp[:, :, :], in1=Ehat[:, :, :], op=ALU.mult),
                     reads=[kp, Ehat], writes=[KhT])
                S.op('act', lambda e: e.copy(out=vTb[:, :, :], in_=pl[:, 8:12, :]), reads=[plt[2]], writes=[vTb])
                if _stop(4.3):
                    return
                bt1 = nb(); bt2 = nb()
                for jc in range(4):
                    S.op('pe', lambda e, jc=jc: e.transpose(out=vbf(bt1)[:, jc * 128:(jc + 1) * 128], in_=SCin[:, jc, 0, :], identity=ident),
                         reads=[SCin, tabb], writes=[bt1])
                    S.op('pe', lambda e, jc=jc: e.transpose(out=vbf(bt1)[:, 512 + jc * 128:512 + (jc + 1) * 128], in_=vTb[:, jc, :], identity=ident),
                         reads=[vTb, tabb], writes=[bt1])
                    S.op('pe', lambda e, jc=jc: e.transpose(out=vbf(bt2)[:, jc * 128:(jc + 1) * 128], in_=BhT[:, jc, :], identity=ident),
                         reads=[BhT, tabb], writes=[bt2])
                    S.op('pe', lambda e, jc=jc: e.transpose(out=vbf(bt2)[:, 512 + jc * 128:512 + (jc + 1) * 128], in_=KhT[:, jc, :], identity=ident),
                         reads=[KhT, tabb], writes=[bt2])
                if _stop(4.6):
                    return
                b1v = vbf(bt1).rearrange("p (k a h c) -> p k a h c", k=2, a=4, h=2)
                S.op('act', lambda e: e.copy(out=Xs[:, :, :, 0:64], in_=b1v[:, 0, :, :, :]), reads=[bt1], writes=Xt)
                if _stop(4.7):
                    return
                S.op('dve', lambda e: e.tensor_copy(out=Vp[:, :, :], in_=vbf(bt1)[:, 512:1024].rearrange("p (a b) -> p a b", a=4)),
                     reads=[bt1], writes=[Vp])
                if _stop(4.8):
                    return
                for hp in range(2):
                    S.op('act', lambda e, hp=hp: e.copy(out=VZ[:, :, hp, hp * 64:(hp + 1) * 64], in_=b1v[:, 1, :, hp, :]),
                         reads=[bt1], writes=[VZ])
                if _stop(4.85):
                    return
                S.op('dve', lambda e: e.tensor_copy(out=BH[:, :, :], in_=vbf(bt2)[:, 0:512].rearrange("p (a b) -> p a b", a=4)),
                     reads=[bt2], writes=[BH])
                if _stop(4.9):
                    return
                S.op('act', lambda e: e.copy(out=KH[:, :, :], in_=vbf(bt2)[:, 512:1024].rearrange("p (a b) -> p a b", a=4)),
                     reads=[bt2], writes=[KH])
                if _stop(5):
                    return
                b3 = [None, None]
                for jc in range(4):
                    for hp in range(2):
                        pb = 64 * hp; h = 2 * jc + hp
                        if h % 4 == 0:
                            b3 = [nb(), nb()]
                        bk = nb()
                        rhs2 = SCin[pb:pb + 64, jc, :, :].rearrange("p a b -> p (a b)")
                        S.op('pe', lambda e, bk=bk, jc=jc, pb=pb, rhs2=rhs2: e.matmul(bk[:, 0:256], lhsT=BtT[pb:pb + 64, jc, :], rhs=rhs2,
                                                                                      start=True, stop=True), reads=[BtT, SCin], writes=[bk])
                        S.op('pe', lambda e, bk=bk, jc=jc, pb=pb, rhs2=rhs2: e.matmul(bk[:, 256:512], lhsT=KtT[pb:pb + 64, jc, :], rhs=rhs2,
                                                                                      start=True, stop=True), reads=[KtT, SCin], writes=[bk])
                        if hp == 0:
                            S.op('dve', lambda e, bk=bk, jc=jc, hp=hp: e.tensor_tensor(out=SCT[:, jc, hp, :], in0=bk[:, :], in1=tabf[:, T_MP:T_MP + 512],
                                                                                       op=ALU.mult), reads=[bk, tabf], writes=[SCT])
                        else:
                            tS = tmpS[jc % 2]
                            S.op('act', lambda e, bk=bk, tS=tS: e.copy(out=tS[:, :], in_=bk[:, :]), reads=[bk], writes=[tS])
                            S.op('pool', lambda e, tS=tS, jc=jc, hp=hp: e.tensor_tensor(out=SCT[:, jc, hp, :], in0=tS[:, :], in1=tabb[:, T_MP:T_MP + 512],
                                                                                        op=ALU.mult), reads=[tS, tabb], writes=[SCT])
                        b3k = b3[hp]
                        S.op('pe', lambda e, b3k=b3k, jc=jc, pb=pb: e.matmul(b3k[:, (jc % 2) * 128:(jc % 2 + 1) * 128], lhsT=SCin[pb:pb + 64, jc, 0, :],
                                                                             rhs=BtT[pb:pb + 64, jc, :], start=True, stop=True),
                             reads=[SCin, BtT], writes=[b3k])
                        if h % 4 == 3:
                            for hq in range(2):
                                S.op('dve', lambda e, g=h // 4, hq=hq, b3g=b3[hq]: e.tensor_tensor(
                                    out=MN[:, 2 * g:2 * g + 2, hq, 0:128],
                                    in0=b3g.t[:, 0:256].rearrange("p (a c) -> p a c", a=2),
                                    in1=tabf[:, T_M0:T_M0 + 128].unsqueeze(1).to_broadcast([128, 2, 128]), op=ALU.mult),
                                    reads=[b3[hq], tabf], writes=[MNt[2 * (h // 4)], MNt[2 * (h // 4) + 1]])
                if _stop(6):
                    return
                for g in range(2):
                    bk = nb()
                    for hh in range(4):
                        jc = 2 * g + hh // 2; hp = hh % 2
                        S.op('pe', lambda e, bk=bk, hh=hh, jc=jc, hp=hp: e.matmul(bk[:, hh * 128:(hh + 1) * 128], lhsT=SCT[:, jc, hp, 256:384],
                                                                                  rhs=Vp[:, jc, :], start=True, stop=True), reads=[SCT, Vp], writes=[bk])
                    bvw = bk.t[:, :].rearrange("p (a h g c) -> p a h g c", a=2, h=2, g=2)
                    for hp in range(2):
                        S.op('act', lambda e, bvw=bvw, g=g, hp=hp: e.copy(out=Xs[:, 2 * g:2 * g + 2, hp, 64:128], in_=bvw[:, :, hp, hp, :]),
                             reads=[bk], writes=[Xt[2 * g], Xt[2 * g + 1]])
                if _stop(7):
                    return
                for j in range(7):
                    for jc in range(4):
                        bP = nb(); bQ = nb() if j < 6 else None
                        for hp in range(2):
                            Nj = SCT[:, jc, hp, 0:128] if j == 0 else MN[:, jc, hp, 128:256]
                            nsrc = [SCT] if j == 0 else []
                            S.op('pe', lambda e, bP=bP, hp=hp, jc=jc, Nj=Nj: e.matmul(bP[:, hp * 128:(hp + 1) * 128], lhsT=Nj, rhs=Xs[:, jc, hp, :],
                                                                                     start=True, stop=True), reads=nsrc + [Xt[jc], MNt[jc]], writes=[bP])
                            if j < 6:
                                S.op('pe', lambda e, bQ=bQ, hp=hp, jc=jc, Nj=Nj: e.matmul(bQ[:, hp * 256:hp * 256 + 128], lhsT=Nj, rhs=MN[:, jc, hp, 0:128],
                                                                                         start=True, stop=True), reads=nsrc + [MNt[jc]], writes=[bQ])
                                S.op('pe', lambda e, bQ=bQ, hp=hp, jc=jc, Nj=Nj: e.matmul(bQ[:, hp * 256 + 128:hp * 256 + 256], lhsT=MN[:, jc, hp, 0:128], rhs=Nj,
                                                                                         start=True, stop=True), reads=nsrc + [MNt[jc]], writes=[bQ])
                        S.op('dve', lambda e, jc=jc, bP=bP: e.tensor_tensor(out=Xs[:, jc, :, :], in0=Xs[:, jc, :, :],
                                                                            in1=bP.t[:, 0:256].rearrange("p (h c) -> p h c", h=2), op=ALU.add),
                             reads=[Xt[jc], bP], writes=[Xt[jc]])
                        if j < 6:
                            S.op('act', lambda e, jc=jc, bQ=bQ: e.copy(out=MN[:, jc, :, :], in_=bQ.t[:, :].rearrange("p (h c) -> p h c", h=2)),
                                 reads=[bQ], writes=[MNt[jc]])
                if _stop(8):
                    return
                for hp in range(2):
                    S.op('act', lambda e, hp=hp: e.copy(out=AZ[:, :, hp, hp * 64:(hp + 1) * 64], in_=Xs[:, :, hp, 0:64]), reads=Xt, writes=[AZ])
                    S.op('pool', lambda e, hp=hp: e.tensor_copy(out=UVZ[:, :, hp, hp * 64:(hp + 1) * 64], in_=Xs[:, :, hp, 64:128]), reads=Xt, writes=[UVZ])
                S.op('pool', lambda e: e.tensor_copy(out=Ap[:, :, :].rearrange("p a (h c) -> p a h c", h=2), in_=Xs[:, :, :, 0:64]), reads=Xt, writes=[Ap])
                S.op('dve', lambda e: e.tensor_copy(out=UVp[:, :, :].rearrange("p a (h c) -> p a h c", h=2), in_=Xs[:, :, :, 64:128]), reads=Xt, writes=[UVp])
                bR = nb(); bG = nb()
                for jc in range(4):
                    for hp in range(2):
                        S.op('pe', lambda e, jc=jc, hp=hp: e.matmul(bR[:, jc * 128:(jc + 1) * 128], lhsT=AZ[:, jc, hp, :], rhs=SCT[:, jc, hp, 128:256],
                                                                    start=(hp == 0), stop=(hp == 1)), reads=[AZ, SCT], writes=[bR])
                for jc in range(4):
                    S.op('pe', lambda e, jc=jc: e.matmul(bG[:, jc * 128:(jc + 1) * 128], lhsT=Ap[:, jc, :], rhs=BH[:, jc, :], start=True, stop=True),
                         reads=[Ap, BH], writes=[bG])
                S.op('dve', lambda e: e.tensor_tensor(out=RhT[:, :, :], in0=v3(bR, 4), in1=SCin[:, :, 1, :], op=ALU.add), reads=[bR, SCin], writes=[RhT])
                S.op('dve', lambda e: e.tensor_tensor(out=GZ[:, :, :], in0=v3(bG, 4), in1=bmf, op=ALU.mult), reads=[bG, tabf], writes=[GZ])
                if _stop(9):
                    return
                bY = nb(); bH = nb()
                for jc in range(4):
                    for hp in range(2):
                        S.op('pe', lambda e, jc=jc, hp=hp: e.matmul(bY[:, jc * 128:(jc + 1) * 128], lhsT=UVZ[:, jc, hp, :], rhs=SCT[:, jc, hp, 128:256],
                                                                    start=(hp == 0), stop=False), reads=[UVZ, SCT], writes=[bY])
                        S.op('pe', lambda e, jc=jc, hp=hp: e.matmul(bY[:, jc * 128:(jc + 1) * 128], lhsT=VZ[:, jc, hp, :], rhs=SCT[:, jc, hp, 384:512],
                                                                    start=False, stop=False), reads=[VZ, SCT], writes=[bY])
                    S.op('pe', lambda e, jc=jc: e.matmul(bY[:, jc * 128:(jc + 1) * 128], lhsT=HZ[:, jc, :], rhs=RhT[:, jc, :], start=False, stop=True),
                         reads=[HZ, RhT], writes=[bY])
                for jc in range(4):
                    S.op('pe', lambda e, jc=jc: e.matmul(bH[:, jc * 128:(jc + 1) * 128], lhsT=BH[:, jc, :], rhs=UVp[:, jc, :], start=True, stop=False),
                         reads=[BH, UVp], writes=[bH])
                    S.op('pe', lambda e, jc=jc: e.matmul(bH[:, jc * 128:(jc + 1) * 128], lhsT=KH[:, jc, :], rhs=Vp[:, jc, :], start=False, stop=False),
                         reads=[KH, Vp], writes=[bH])
                    S.op('pe', lambda e, jc=jc: e.matmul(bH[:, jc * 128:(jc + 1) * 128], lhsT=GZ[:, jc, :], rhs=HZ[:, jc, :], start=False, stop=True),
                         reads=[GZ, HZ], writes=[bH])
                for jc in range(4):
                    S.op('dve', lambda e, jc=jc: e.scalar_tensor_tensor(out=Hf[:, jc, :], in0=Hf[:, jc, :], scalar=Epos[:, jc, 127:128],
                                                                        in1=bH[:, jc * 128:(jc + 1) * 128], op0=ALU.mult, op1=ALU.add),
                         reads=[Hf, Epos, bH], writes=[Hf])
                S.op('pool', lambda e: e.tensor_tensor(out=Hf[:, :, :], in0=Hf[:, :, :], in1=bmf, op=ALU.mult), reads=[Hf, tabf], writes=[Hf])
                S.op('act', lambda e: e.copy(out=HZ[:, :, :], in_=Hf[:, :, :]), reads=[Hf], writes=[HZ])
                if _stop(10):
                    return
                S.op('act', lambda e: e.copy(out=yb[:, :, :], in_=v3(bY, 4)), reads=[bY], writes=[yb])
                S.op('act', lambda e: e.activation(out=ysq[:, :, :], in_=v3(bY, 4), func=AF.Square), reads=[bY], writes=[ysq])
                S.op('pool', lambda e: e.tensor_tensor(out=t0[:, :, :], in0=pl[:, 0:4, :], in1=kp[:, :, :], op=ALU.mult), reads=[plt[0], kp], writes=[t0])
                S.op('pool', lambda e: e.tensor_tensor(out=rkb[:, :, :], in0=t0[:, :, :], in1=cb(RK, 4, [128, 4, 128]), op=ALU.mult),
                     reads=[t0, cst], writes=[rkb])
                bM = nb(); bQ = nb(); bO = nb()
                for jc in range(4):
                    S.op('pe', lambda e, jc=jc: e.matmul(bM[:, jc * 128:(jc + 1) * 128], lhsT=bones, rhs=yb[:, jc, :], start=True, stop=True),
                         reads=[tabb, yb], writes=[bM])
                    S.op('pe', lambda e, jc=jc: e.matmul(bQ[:, jc * 128:(jc + 1) * 128], lhsT=bones, rhs=ysq[:, jc, :], start=True, stop=True),
                         reads=[tabb, ysq], writes=[bQ])
                    S.op('pe', lambda e, jc=jc: e.matmul(bO[:, jc * 128:(jc + 1) * 128], lhsT=bones, rhs=rkb[:, jc, :], start=True, stop=True),
                         reads=[tabb, rkb], writes=[bO])
                S.op('act', lambda e: e.activation(out=t1[:, :, :], in_=v3(bM, 4), func=AF.Identity, scale=1.0 / 64), reads=[bM], writes=[t1])
                S.op('pool', lambda e: e.tensor_tensor(out=t2[:, :, :], in0=t1[:, :, :], in1=t1[:, :, :], op=ALU.mult), reads=[t1], writes=[t2])
                S.op('dve', lambda e: e.scalar_tensor_tensor(out=t2[:, :, :], in0=v3(bQ, 4), scalar=1.0 / 64, in1=t2[:, :, :],
                                                             op0=ALU.mult, op1=ALU.subtract), reads=[bQ, t2], writes=[t2])
                S.op('act', lambda e: e.activation(out=t2[:, :, :], in_=t2[:, :, :], func=AF.Ln, bias=64e-5), reads=[t2], writes=[t2])
                S.op('act', lambda e: e.activation(out=t2[:, :, :], in_=t2[:, :, :], func=AF.Exp, scale=-0.5), reads=[t2], writes=[t2])
                S.op('dve', lambda e: e.tensor_tensor(out=t0[:, :, :], in0=v3(bY, 4), in1=t1[:, :, :], op=ALU.subtract), reads=[bY, t1], writes=[t0])
                S.op('pool', lambda e: e.tensor_tensor(out=t0[:, :, :], in0=t0[:, :, :], in1=t2[:, :, :], op=ALU.mult), reads=[t0, t2], writes=[t0])
                S.op('pool', lambda e: e.tensor_tensor(out=t0[:, :, :], in0=t0[:, :, :], in1=cb(LW, 4, [128, 4, 128]), op=ALU.mult),
                     reads=[t0, cst], writes=[t0])
                S.op('pool', lambda e: e.tensor_tensor(out=t0[:, :, :], in0=t0[:, :, :], in1=cb(LB, 4, [128, 4, 128]), op=ALU.add),
                     reads=[t0, cst], writes=[t0])
                S.op('dve', lambda e: e.tensor_tensor(out=t1[:, :, :], in0=v3(bO, 4), in1=pl[:, 8:12, :], op=ALU.mult), reads=[bO, plt[2]], writes=[t1])
                S.op('pool', lambda e: e.tensor_tensor(out=t0[:, :, :], in0=t0[:, :, :], in1=t1[:, :, :], op=ALU.add), reads=[t0, t1], writes=[t0])
                S.op('pool', lambda e: e.tensor_tensor(out=yrwT[:, :, :], in0=t0[:, :, :], in1=gT[:, :, :], op=ALU.mult), reads=[t0, gT], writes=[yrwT])
                S.dma('pool', lambda e, ci=ci: e.dma_start(out=yrw_s[ci].rearrange("p (a b) -> p a b", a=4), in_=yrwT[:, :, :]), 'yst', reads=[yrwT])
            for ci in range(NCHUNK):
                _chunk(ci)
            S.barrier()

        if "B" in phases:
          with ExitStack() as st:
            NBC = INC - RWC
            w_b = S.sb(st, [128, 8, NBC], BF16)
            wbrw = S.sb(st, [128, 4, D], BF16); wbret = S.sb(st, [128, 8, D], BF16); wout = S.sb(st, [128, 8, D], BF16)
            for kc in range(8):
                S.dma('pool', lambda e, kc=kc: e.dma_start(out=w_b[:, kc, :], in_=win_d[kc * 128:(kc + 1) * 128, RWC:INC]), 'wB', writes=[w_b])
                S.dma('pool', lambda e, kc=kc: e.dma_start(out=wbret[:, kc, :], in_=wbret_d[kc * 128:(kc + 1) * 128, :]), 'wB', writes=[wbret])
                S.dma('pool', lambda e, kc=kc: e.dma_start(out=wout[:, kc, :], in_=wout_d[kc * 128:(kc + 1) * 128, :]), 'wB', writes=[wout])
            for kc in range(4):
                S.dma('pool', lambda e, kc=kc: e.dma_start(out=wbrw[:, kc, :], in_=wbrw_d[kc * 128:(kc + 1) * 128, :]), 'wB', writes=[wbrw])
            _xb = S.sb(st, [128, D], F32); xs2 = [_xb, _xb]
            hnT2 = [S.sb(st, [128, 8, 128], BF16) for _ in range(2)]
            yrw2 = [S.sb(st, [128, 4, 128], BF16) for _ in range(2)]
            posi2 = [S.sb(st, [128, 128], I32) for _ in range(2)]
            posf = S.sb(st, [128, 128], F32); u0 = S.sb(st, [128, 128], F32); u1 = S.sb(st, [128, 128], F32)
            ti = S.sb(st, [128, 128], I32); tf = S.sb(st, [128, 128], F32)
            cosT = S.sb(st, [128, 128], F32); sinT = S.sb(st, [128, 128], F32)
            qk = S.sb(st, [128, 8, 128], F32); qkb = S.sb(st, [128, 8, 128], BF16)
            r1 = S.sb(st, [128, 8, 128], F32); r2 = S.sb(st, [128, 8, 128], F32)
            rot = S.sb(st, [128, 8, 128], BF16); qd = S.sb(st, [128, 4, 128], BF16)
            v_bf = S.sb(st, [128, D], BF16); sgb = S.sb(st, [128, D], BF16); sA = S.sb(st, [128, D], BF16); sB = S.sb(st, [128, D], BF16)
            kdZ = S.sb(st, [128, 4, 2, 128], BF16)
            sT = S.sb(st, [128, 8, 128], BF16)
            Rf = S.sb(st, [128, 4, 128], F32); Rb = S.sb(st, [128, 4, 128], BF16)
            of = qk; osq = r1
            stt = S.sb(st, [128, 16], F32); mean = S.sb(st, [128, 8], F32); var = S.sb(st, [128, 8], F32)
            yret = S.sb(st, [128, D], BF16); yretT = S.sb(st, [128, 8, 128], BF16)
            m1 = S.sb(st, [128, D], F32); m2 = S.sb(st, [128, D], F32); mg = S.sb(st, [128, D], BF16); mT = S.sb(st, [128, 8, 128], BF16)
            mo = m2; junk = mg; ss = S.sb(st, [128, 1], F32); rs = S.sb(st, [128, 1], F32)
            hh = m1
            S.op('pool', lambda e: e.memset(kdZ[:, :, :, :], 0.0), writes=[kdZ])
            DBG.update({k_: v_.name for k_, v_ in list(locals().items()) if isinstance(v_, Buf)})

            def _chunk(ci):
                b_i, c_i = divmod(ci, NCH)
                tok0 = ci * 128
                xs = xs2[ci % 2]; hnT = hnT2[ci % 2]; yrw = yrw2[ci % 2]; posi = posi2[ci % 2]
                S.dma('sp', lambda e, hnT=hnT, ci=ci: e.dma_start(out=hnT[:, :, :], in_=hnT_s[ci].rearrange("p (a b) -> p a b", a=8)),
                      'hB%d' % (ci % 2), writes=[hnT])
                S.dma('sp', lambda e, yrw=yrw, ci=ci: e.dma_start(out=yrw[:, :, :], in_=yrw_s[ci].rearrange("p (a b) -> p a b", a=4)),
                      'yB%d' % (ci % 2), writes=[yrw])
                S.dma('sp', lambda e, posi=posi, tok0=tok0: e.dma_start(out=posi[:, :], in_=pos_d[0:1, tok0:tok0 + 128].partition_broadcast(128)),
                      'pB%d' % (ci % 2), writes=[posi])
                S.dma('sp', lambda e, xs=xs, tok0=tok0: e.dma_start(out=xs[:, :], in_=x_d[tok0:tok0 + 128, :]), 'xB%d' % (ci % 2), writes=[xs])
                if c_i == 0:
                    S.op('pool', lambda e: e.memset(Rf[:, :, :], 0.0), writes=[Rf])
                    S.op('pool', lambda e: e.memset(Rb[:, :, :], 0.0), writes=[Rb])
                S.op('dve', lambda e, posi=posi: e.tensor_copy(out=posf[:, :], in_=posi[:, :]), reads=[posi], writes=[posf])
                S.op('dve', lambda e: e.tensor_scalar(out=u0[:, :], in0=posf[:, :], scalar1=cst[:, INV:INV + 1], scalar2=None, op0=ALU.mult),
                     reads=[posf, cst], writes=[u0])
                S.op('dve', lambda e: e.tensor_copy(out=ti[:, :], in_=u0[:, :]), reads=[u0], writes=[ti])
                S.op('dve', lambda e: e.tensor_copy(out=tf[:, :], in_=ti[:, :]), reads=[ti], writes=[tf])
                S.op('dve', lambda e: e.tensor_tensor(out=tf[:, :], in0=u0[:, :], in1=tf[:, :], op=ALU.subtract), reads=[u0, tf], writes=[tf])
                S.op('act', lambda e: e.activation(out=sinT[:, :], in_=tf[:, :], func=AF.Sin, scale=cst[:, SSC:SSC + 1]), reads=[tf, cst], writes=[sinT])
                S.op('pool', lambda e: e.tensor_scalar(out=u1[:, :], in0=u0[:, :], scalar1=0.25, scalar2=None, op0=ALU.add), reads=[u0], writes=[u1])
                S.op('dve', lambda e: e.tensor_copy(out=ti[:, :], in_=u1[:, :]), reads=[u1], writes=[ti])
                S.op('dve', lambda e: e.tensor_copy(out=tf[:, :], in_=ti[:, :]), reads=[ti], writes=[tf])
                S.op('dve', lambda e: e.tensor_tensor(out=tf[:, :], in0=u1[:, :], in1=tf[:, :], op=ALU.subtract), reads=[u1, tf], writes=[tf])
                S.op('act', lambda e: e.activation(out=cosT[:, :], in_=tf[:, :], func=AF.Sin, scale=2.0 * math.pi), reads=[tf], writes=[cosT])
                for g in range(2):
                    bk = nb()
                    for jj in range(4):
                        j = g * 4 + jj
                        for kc in range(8):
                            S.op('pe', lambda e, bk=bk, jj=jj, j=j, kc=kc, hnT=hnT: e.matmul(
                                bk[:, jj * 128:(jj + 1) * 128], lhsT=w_b[:, kc, j * 128:(j + 1) * 128], rhs=hnT[:, kc, :],
                                start=(kc == 0), stop=(kc == 7)), reads=[w_b, hnT], writes=[bk])
                    S.op('act', lambda e, bk=bk, g=g: e.copy(out=qk[:, g * 4:g * 4 + 4, :], in_=v3(bk, 4)), reads=[bk], writes=[qk])
                S.op('pool', lambda e: e.tensor_copy(out=qkb[:, :, :], in_=qk[:, :, :]), reads=[qk], writes=[qkb])
                for grp in range(4):
                    for half in range(2):
                        bk = nb(); c0 = 1024 + grp * 1024 + half * 512
                        for kc in range(8):
                            S.op('pe', lambda e, bk=bk, kc=kc, c0=c0, hnT=hnT: e.matmul(bk[:, :], lhsT=hnT[:, kc, :], rhs=w_b[:, kc, c0:c0 + 512],
                                                                                        start=(kc == 0), stop=(kc == 7)), reads=[w_b, hnT], writes=[bk])
                        dst = (v_bf, sgb, sA, sB)[grp]
                        fn = (AF.Copy, AF.Silu, AF.Sigmoid, AF.Sigmoid)[grp]
                        S.op('act', lambda e, bk=bk, dst=dst, fn=fn, half=half: e.activation(out=dst[:, half * 512:(half + 1) * 512], in_=bk[:, :], func=fn),
                             reads=[bk], writes=[dst])
                bsw = [nb(), nb()]
                for j in range(8):
                    S.op('pe', lambda e, j=j: e.matmul(bsw[j // 4][:, (j % 4) * 128:(j % 4 + 1) * 128], lhsT=pswap, rhs=qkb[:, j, :], start=True, stop=True),
                         reads=[tabb, qkb], writes=[bsw[j // 4]])
                S.op('pool', lambda e: e.tensor_tensor(out=r1[:, :, :], in0=qk[:, :, :], in1=cosT[:, :].unsqueeze(1).to_broadcast([128, 8, 128]), op=ALU.mult),
                     reads=[qk, cosT], writes=[r1])
                for g in range(2):
                    S.op('dve', lambda e, g=g: e.tensor_tensor(out=r2[:, g * 4:g * 4 + 4, :], in0=v3(bsw[g], 4),
                                                               in1=sinT[:, :].unsqueeze(1).to_broadcast([128, 4, 128]), op=ALU.mult),
                         reads=[bsw[g], sinT], writes=[r2])
                S.op('dve', lambda e: e.tensor_tensor(out=rot[:, :, :], in0=r1[:, :, :], in1=r2[:, :, :], op=ALU.add), reads=[r1, r2], writes=[rot])
                S.op('pool', lambda e: e.tensor_tensor(out=qd[:, :, :], in0=rot[:, 0:4, :], in1=cst[:, QD:QD + 512].rearrange("p (a b) -> p a b", a=4), op=ALU.mult),
                     reads=[rot, cst], writes=[qd])
                bk = nb()
                for jc in range(4):
                    S.op('pe', lambda e, bk=bk, jc=jc: e.transpose(out=vbf(bk)[:, jc * 128:(jc + 1) * 128], in_=rot[:, 4 + jc, :], identity=ident),
                         reads=[rot, tabb], writes=[bk])
                bkv = vbf(bk)[:, 0:512].rearrange("p (a h c) -> p a h c", a=4, h=2)
                kdv = cst[:, KDEC:KDEC + 8].rearrange("p (a h) -> p a h", a=4)
                for hp in range(2):
                    S.op('dve', lambda e, hp=hp, bkv=bkv: e.tensor_tensor(out=kdZ[:, :, hp, hp * 64:(hp + 1) * 64], in0=bkv[:, :, hp, :],
                                                                          in1=kdv[:, :, hp:hp + 1].to_broadcast([128, 4, 64]), op=ALU.mult),
                         reads=[bk, cst], writes=[kdZ])
                for hp in range(2):
                    bk = nb(); pb = 64 * hp
                    for jc in range(4):
                        S.op('pe', lambda e, bk=bk, jc=jc, pb=pb: e.matmul(bk[:, jc * 128:(jc + 1) * 128], lhsT=rot[pb:pb + 64, 4 + jc, :],
                                                                           rhs=rot[pb:pb + 64, jc, :], start=True, stop=True), reads=[rot], writes=[bk])
                    S.op('dve', lambda e, bk=bk, hp=hp: e.tensor_tensor(
                        out=sT[:, :, :].rearrange("p (a h) c -> p a h c", h=2)[:, :, hp, :], in0=v3(bk, 4),
                        in1=tabf[:, T_MT:T_MT + 1024].rearrange("p (a h c) -> p a h c", a=4, h=2)[:, :, hp, :], op=ALU.mult),
                         reads=[bk, tabf], writes=[sT])
                bo = [nb(), nb()]
                for h in range(8):
                    jc = h // 2; pb = 64 * (h % 2); bk = bo[h // 4]; cc = (h % 4) * 128
                    S.op('pe', lambda e, bk=bk, cc=cc, h=h: e.matmul(bk[:, cc:cc + 128], lhsT=sT[:, h, :], rhs=v_bf[:, h * 128:(h + 1) * 128],
                                                                     start=True, stop=False), reads=[sT, v_bf], writes=[bk])
                    S.op('pe', lambda e, bk=bk, cc=cc, jc=jc, pb=pb: e.matmul(bk[:, cc:cc + 128], lhsT=qd[pb:pb + 64, jc, :], rhs=Rb[pb:pb + 64, jc, :],
                                                                              start=False, stop=True), reads=[qd, Rb], writes=[bk])
                for g in range(2):
                    S.op('act', lambda e, g=g: e.copy(out=of[:, g * 4:g * 4 + 4, :], in_=v3(bo[g], 4)), reads=[bo[g]], writes=[of])
                bR = nb()
                for jc in range(4):
                    for hp in range(2):
                        h = 2 * jc + hp
                        S.op('pe', lambda e, jc=jc, hp=hp, h=h: e.matmul(bR[:, jc * 128:(jc + 1) * 128], lhsT=kdZ[:, jc, hp, :], rhs=v_bf[:, h * 128:(h + 1) * 128],
                                                                         start=(hp == 0), stop=(hp == 1)), reads=[kdZ, v_bf], writes=[bR])
                S.op('pool', lambda e: e.tensor_tensor(out=Rf[:, :, :], in0=Rf[:, :, :], in1=cb(CD, 4, [128, 4, 128]), op=ALU.mult), reads=[Rf, cst], writes=[Rf])
                S.op('dve', lambda e: e.tensor_tensor(out=Rf[:, :, :], in0=Rf[:, :, :], in1=v3(bR, 4), op=ALU.add), reads=[Rf, bR], writes=[Rf])
                S.op('act', lambda e: e.copy(out=Rb[:, :, :], in_=Rf[:, :, :]), reads=[Rf], writes=[Rb])
                S.op('dve', lambda e: e.tensor_reduce(out=stt[:, 0:8], in_=of[:, :, :], axis=AX.X, op=ALU.add), reads=[of], writes=[stt])
                S.op('pool', lambda e: e.tensor_tensor(out=osq[:, :, :], in0=of[:, :, :], in1=of[:, :, :], op=ALU.mult), reads=[of], writes=[osq])
                S.op('dve', lambda e: e.tensor_reduce(out=stt[:, 8:16], in_=osq[:, :, :], axis=AX.X, op=ALU.add), reads=[osq], writes=[stt])
                S.op('dve', lambda e: e.tensor_scalar(out=mean[:, :], in0=stt[:, 0:8], scalar1=1.0 / 128, scalar2=None, op0=ALU.mult), reads=[stt], writes=[mean])
                S.op('dve', lambda e: e.tensor_tensor(out=var[:, :], in0=mean[:, :], in1=mean[:, :], op=ALU.mult), reads=[mean], writes=[var])
                S.op('dve', lambda e: e.scalar_tensor_tensor(out=var[:, :], in0=stt[:, 8:16], scalar=1.0 / 128, in1=var[:, :], op0=ALU.mult, op1=ALU.subtract),
                     reads=[stt, var], writes=[var])
                S.op('act', lambda e: e.activation(out=var[:, :], in_=var[:, :], func=AF.Sqrt, bias=1e-5), reads=[var], writes=[var])
                S.op('dve', lambda e: e.reciprocal(out=var[:, :], in_=var[:, :]), reads=[var], writes=[var])
                S.op('dve', lambda e: e.tensor_tensor(out=of[:, :, :], in0=of[:, :, :], in1=mean[:, :].unsqueeze(2).to_broadcast([128, 8, 128]), op=ALU.subtract),
                     reads=[of, mean], writes=[of])
                S.op('pool', lambda e: e.tensor_tensor(out=of[:, :, :], in0=of[:, :, :], in1=var[:, :].unsqueeze(2).to_broadcast([128, 8, 128]), op=ALU.mult),
                     reads=[of, var], writes=[of])
                S.op('dve', lambda e: e.tensor_tensor(out=yret[:, :], in0=of[:, :, :].rearrange("p a b -> p (a b)"), in1=sgb[:, :], op=ALU.mult),
                     reads=[of, sgb], writes=[yret])
                bk = nb()
                for kc in range(8):
                    S.op('pe', lambda e, bk=bk, kc=kc: e.transpose(out=vbf(bk)[:, kc * 128:(kc + 1) * 128], in_=yret[:, kc * 128:(kc + 1) * 128], identity=ident),
                         reads=[yret, tabb], writes=[bk])
                S.op('act', lambda e, bk=bk: e.copy(out=yretT[:, :, :], in_=vbf(bk).rearrange("p (a b) -> p a b", a=8)), reads=[bk], writes=[yretT])
                for half in range(2):
                    b1 = nb(); b2 = nb(); hs = slice(half * 512, (half + 1) * 512)
                    for jc in range(4):
                        S.op('pe', lambda e, b1=b1, jc=jc, hs=hs, yrw=yrw: e.matmul(b1[:, :], lhsT=yrw[:, jc, :], rhs=wbrw[:, jc, hs], start=(jc == 0), stop=(jc == 3)),
                             reads=[yrw, wbrw], writes=[b1])
                    for kc in range(8):
                        S.op('pe', lambda e, b2=b2, kc=kc, hs=hs: e.matmul(b2[:, :], lhsT=yretT[:, kc, :], rhs=wbret[:, kc, hs], start=(kc == 0), stop=(kc == 7)),
                             reads=[yretT, wbret], writes=[b2])
                    S.op('dve', lambda e, b1=b1, hs=hs: e.tensor_tensor(out=m1[:, hs], in0=b1[:, :], in1=sA[:, hs], op=ALU.mult), reads=[b1, sA], writes=[m1])
                    S.op('dve', lambda e, b2=b2, hs=hs: e.tensor_tensor(out=m2[:, hs], in0=b2[:, :], in1=sB[:, hs], op=ALU.mult), reads=[b2, sB], writes=[m2])
                S.op('pool', lambda e: e.tensor_tensor(out=mg[:, :], in0=m1[:, :], in1=m2[:, :], op=ALU.add), reads=[m1, m2], writes=[mg])
                bk = nb()
                for kc in range(8):
                    S.op('pe', lambda e, bk=bk, kc=kc: e.transpose(out=vbf(bk)[:, kc * 128:(kc + 1) * 128], in_=mg[:, kc * 128:(kc + 1) * 128], identity=ident),
                         reads=[mg, tabb], writes=[bk])
                S.op('act', lambda e, bk=bk: e.copy(out=mT[:, :, :], in_=vbf(bk).rearrange("p (a b) -> p a b", a=8)), reads=[bk], writes=[mT])
                for half in range(2):
                    bk = nb(); hs = slice(half * 512, (half + 1) * 512)
                    for kc in range(8):
                        S.op('pe', lambda e, bk=bk, kc=kc, hs=hs: e.matmul(bk[:, :], lhsT=mT[:, kc, :], rhs=wout[:, kc, hs], start=(kc == 0), stop=(kc == 7)),
                             reads=[mT, wout], writes=[bk])
                    S.op('act', lambda e, bk=bk, hs=hs: e.copy(out=mo[:, hs], in_=bk[:, :]), reads=[bk], writes=[mo])
                S.op('act', lambda e: e.activation(out=junk[:, :], in_=mo[:, :], func=AF.Square, accum_out=ss[:, 0:1]), reads=[mo], writes=[junk, ss])
                S.op('act', lambda e: e.activation(out=rs[:, 0:1], in_=ss[:, 0:1], func=AF.Sqrt, scale=1.0 / D, bias=1e-6), reads=[ss], writes=[rs])
                S.op('dve', lambda e: e.reciprocal(out=rs[:, 0:1], in_=rs[:, 0:1]), reads=[rs], writes=[rs])
                S.op('dve', lambda e: e.scalar_tensor_tensor(out=hh[:, :], in0=mo[:, :], scalar=rs[:, 0:1], in1=rowsB[:, :], op0=ALU.mult, op1=ALU.mult),
                     reads=[mo, rs, rowsB], writes=[hh])
                S.op('pool', lambda e, xs=xs: e.tensor_tensor(out=hh[:, :], in0=hh[:, :], in1=xs[:, :], op=ALU.add), reads=[hh, xs], writes=[hh])
                S.dma('pool', lambda e, tok0=tok0: e.dma_start(out=h_s[tok0:tok0 + 128, :], in_=hh[:, :]), 'hstore', reads=[hh])
            for ci in range(NCHUNK):
                _chunk(ci)
            S.barrier()

        stAB.close()
        if "C" in phases:
          with ExitStack() as st:
            wup = S.sb(st, [128, 8, 2 * DFF], BF16); wdn = S.sb(st, [128, 22, D], BF16)
            identC = S.sb(st, [128, 128], BF16); rowsC = S.sb(st, [128, D], F32)
            S.dma('pool', lambda e: e.dma_start(out=identC[:, :], in_=tab_d[:, T_ID:T_ID + 128]), 'wC', writes=[identC])
            S.dma('sp', lambda e: e.dma_start(out=rowsC[:, :], in_=rows_d[1:2, :].partition_broadcast(128)), 'rowsC', writes=[rowsC])
            for kc in range(8):
                S.dma('pool', lambda e, kc=kc: e.dma_start(out=wup[:, kc, :], in_=wup_d[kc * 128:(kc + 1) * 128, :]), 'wC', writes=[wup])
            for j in range(22):
                S.dma('pool', lambda e, j=j: e.dma_start(out=wdn[:, j, :], in_=wdn_d[j * 128:(j + 1) * 128, :]), 'wC', writes=[wdn])
            hb = [S.sb(st, [128, D], F32) for _ in range(2)]
            xnC = S.sb(st, [128, D], BF16)
            ssC = S.sb(st, [128, 1], F32); rsC = S.sb(st, [128, 1], F32)
            hn2T = S.sb(st, [128, 8, FT], BF16)
            actT = S.sb(st, [128, 22, FT], BF16)
            ub2 = [S.sb(st, [128, FT + 2], F32) for _ in range(4)]
            acc2 = [S.sb(st, [128, FT], F32) for _ in range(4)]
            gg2 = [S.sb(st, [128, FT], BF16) for _ in range(2)]
            carry = S.sb(st, [128, 44, 2], F32)
            fo = S.sb(st, [128, D], F32)

            def _tile(ti_):
                b_i, t_i = divmod(ti_, NTILE)
                tokb = ti_ * FT
                if t_i == 0:
                    S.op('pool', lambda e: e.memset(carry[:, :, :], 0.0), writes=[carry])
                for sc in range(NSUB):
                    hbs = hb[sc % 2]
                    S.dma('sp', lambda e, sc=sc, hbs=hbs: e.dma_start(out=hbs[:, :], in_=h_s[tokb + sc * 128:tokb + (sc + 1) * 128, :]),
                          'hC%d' % (sc % 2), writes=[hbs])
                    rmsnorm_T(st, hbs, hn2T, sc * 128, G3, xnC, ssC, rsC, xnC, idn=(identC[:, :], identC))

                def _finish(jp):
                    ag = acc2[(2 * jp) % 4]; av = acc2[(2 * jp + 1) % 4]; gg = gg2[jp % 2]
                    S.op('act', lambda e: e.activation(out=gg[:, :], in_=ag[:, :], func=AF.Gelu_apprx_tanh), reads=[ag], writes=[gg])
                    S.op('pool', lambda e: e.tensor_tensor(out=actT[:, jp, :], in0=gg[:, :], in1=av[:, :], op=ALU.mult),
                         reads=[gg, av], writes=[actT])

                for jp in range(22):
                    for which in range(2):
                        j = jp + 22 * which
                        bk = nb(); ub = ub2[(2 * jp + which) % 4]; acc = acc2[(2 * jp + which) % 4]
                        for kc in range(8):
                            S.op('pe', lambda e, bk=bk, kc=kc, j=j: e.matmul(bk[:, 0:FT], lhsT=wup[:, kc, j * 128:(j + 1) * 128], rhs=hn2T[:, kc, :],
                                                                             start=(kc == 0), stop=(kc == 7)), reads=[wup, hn2T], writes=[bk])
                        S.op('act', lambda e, bk=bk, ub=ub: e.copy(out=ub[:, 2:FT + 2], in_=bk[:, 0:FT]), reads=[bk], writes=[ub])
                        S.op('pool', lambda e, ub=ub, j=j: e.tensor_copy(out=ub[:, 0:2], in_=carry[:, j, :]), reads=[carry], writes=[ub])
                        S.op('act', lambda e, bk=bk, acc=acc, j=j: e.activation(out=acc[:, :], in_=bk[:, 0:FT], func=AF.Identity,
                                                                                 scale=cst[:, CW + 2 * 44 + j:CW + 2 * 44 + j + 1], bias=cst[:, CB + j:CB + j + 1]),
                             reads=[bk, cst], writes=[acc])
                        S.op('dve', lambda e, ub=ub, acc=acc, j=j: e.scalar_tensor_tensor(out=acc[:, :], in0=ub[:, 1:FT + 1], scalar=cst[:, CW + 44 + j:CW + 44 + j + 1],
                                                                                         in1=acc[:, :], op0=ALU.mult, op1=ALU.add), reads=[ub, acc, cst], writes=[acc])
                        S.op('dve', lambda e, ub=ub, acc=acc, j=j: e.scalar_tensor_tensor(out=acc[:, :], in0=ub[:, 0:FT], scalar=cst[:, CW + j:CW + j + 1],
                                                                                         in1=acc[:, :], op0=ALU.mult, op1=ALU.add), reads=[ub, acc, cst], writes=[acc])
                        S.op('pool', lambda e, ub=ub, j=j: e.tensor_copy(out=carry[:, j, :], in_=ub[:, FT:FT + 2]), reads=[ub], writes=[carry])
                    if jp >= 1:
                        _finish(jp - 1)
                _finish(21)
                for sc in range(NSUB):
                    hbs = hb[sc % 2]
                    S.dma('sp', lambda e, sc=sc, hbs=hbs: e.dma_start(out=hbs[:, :], in_=h_s[tokb + sc * 128:tokb + (sc + 1) * 128, :]),
                          'hC%d' % (sc % 2), writes=[hbs])
                    for half in range(2):
                        bk = nb(); hs = slice(half * 512, (half + 1) * 512)
                        for j in range(22):
                            S.op('pe', lambda e, bk=bk, j=j, sc=sc, hs=hs: e.matmul(bk[:, :], lhsT=actT[:, j, sc * 128:(sc + 1) * 128], rhs=wdn[:, j, hs],
                                                                                    start=(j == 0), stop=(j == 21)), reads=[actT, wdn], writes=[bk])
                        S.op('act', lambda e, bk=bk, hs=hs: e.copy(out=fo[:, hs], in_=bk[:, :]), reads=[bk], writes=[fo])
                    S.op('act', lambda e: e.activation(out=xnC[:, :], in_=fo[:, :], func=AF.Square, accum_out=ssC[:, 0:1]), reads=[fo], writes=[xnC, ssC])
                    S.op('act', lambda e: e.activation(out=rsC[:, 0:1], in_=ssC[:, 0:1], func=AF.Sqrt, scale=1.0 / D, bias=1e-6), reads=[ssC], writes=[rsC])
                    S.op('dve', lambda e: e.reciprocal(out=rsC[:, 0:1], in_=rsC[:, 0:1]), reads=[rsC], writes=[rsC])
                    S.op('dve', lambda e: e.scalar_tensor_tensor(out=fo[:, :], in0=fo[:, :], scalar=rsC[:, 0:1], in1=rowsC[:, :], op0=ALU.mult, op1=ALU.mult),
                         reads=[fo, rsC, rowsC], writes=[fo])
                    S.op('pool', lambda e, hbs=hbs: e.tensor_tensor(out=fo[:, :], in0=fo[:, :], in1=hbs[:, :], op=ALU.add), reads=[fo, hbs], writes=[fo])
                    S.dma('pool', lambda e, sc=sc: e.dma_start(out=out_d[tokb + sc * 128:tokb + (sc + 1) * 128, :], in_=fo[:, :]), 'ostore', reads=[fo])
            for ti_ in range(NSEQ * NTILE):
                _tile(ti_)
            S.barrier()
        S.barrier()
        S.emit()
    return nc


def host_consts(inp):
    f = np.float32
    c = np.zeros((128, NCONST), f)

    def pk(v, n):
        return np.asarray(v, f).reshape(n, 128).T
    c[:, G1:G1 + 8] = pk(inp["norm_mix_pre"][0], 8)
    c[:, MU:MU + 14] = pk(inp["rw_mu"][0], 14)
    c[:, W0:W0 + 4] = pk(inp["rw_w0"][0], 4)
    c[:, A0:A0 + 4] = pk(inp["rw_a0"][0], 4)
    c[:, KK:KK + 4] = pk(inp["rw_k_k"][0], 4)
    c[:, KA:KA + 4] = pk(inp["rw_k_a"][0], 4)
    c[:, RK:RK + 4] = pk(inp["rw_r_k"][0], 4)
    c[:, LW:LW + 4] = pk(inp["rw_lnx_w"][0], 4)
    c[:, LB:LB + 4] = pk(inp["rw_lnx_b"][0], 4)
    c[:, G3:G3 + 8] = pk(inp["norm_ffn_pre"][0], 8)
    c[:, CB:CB + 44] = pk(inp["ffn_conv_b"][0], 44)
    for tap in range(3):
        c[:, CW + tap * 44:CW + (tap + 1) * 44] = pk(inp["ffn_conv_w"][0, tap], 44)
    p = np.arange(128)
    inv = (10000.0 ** (-(np.arange(32, dtype=np.float32)) / np.float32(32))).astype(f)
    c[:, INV] = inv[p % 32] / f(2 * math.pi)
    c[:, SSC] = np.where((p % 64) < 32, -2 * math.pi, 2 * math.pi).astype(f)
    lg = np.log1p(-np.exp2(-5.0 - np.arange(8, dtype=np.float64)))
    for jc in range(4):
        hsel = 2 * jc + p // 64
        c[:, CD + jc] = np.exp(128 * lg[hsel])
        c[:, QD + jc * 128:QD + (jc + 1) * 128] = np.exp((np.arange(128)[None, :] + 1.0) * lg[hsel][:, None])
    for h in range(8):
        c[:, KDEC + h] = 0.125 * np.exp((127.0 - p) * lg[h])
    tab = np.zeros((128, NTAB), f)
    s = np.arange(128)[:, None]; t = np.arange(128)[None, :]
    strict = (t > s).astype(f); incl = (t >= s).astype(f)
    tab[:, T_MP:T_MP + 512] = np.concatenate([strict, incl, strict, incl], axis=1)
    tab[:, T_M0:T_M0 + 128] = (t < s).astype(f)
    for h in range(8):
        tab[:, T_MT + h * 128:T_MT + (h + 1) * 128] = np.where(t >= s, 0.125 * np.exp(np.maximum(t - s, 0) * lg[h]), 0.0)
    tab[:, T_BM:T_BM + 128] = ((s // 64) == (t // 64)).astype(f)
    tab[:, T_ID:T_ID + 128] = np.eye(128, dtype=f)
    tab[:, T_SW:T_SW + 128] = (t == (s ^ 32)).astype(f)
    lr = np.concatenate([np.concatenate([inp["rw_w2"][0], inp["rw_a2"][0]], axis=0), inp["rw_g2"][0]], axis=1).astype(f)
    rows = np.stack([inp["norm_mix_post"][0], inp["norm_ffn_post"][0]], axis=0).astype(f)
    return c, tab, lr, rows


_NC_CACHE = {}
STOP = [None]


def _stop(n):
    return STOP[0] is not None and n >= STOP[0]

DBG = {}


def run(inp, NSEQ, NCH, ncores, dbg=False, phases="ABC"):
    key = (NSEQ, NCH, dbg, phases, STOP[0])
    if key not in _NC_CACHE:
        _NC_CACHE[key] = build(NSEQ, NCH, dbg, phases)
    nc = _NC_CACHE[key]
    c, tab, lr, rows = host_consts(inp)
    T = NCH * 128
    shared = {
        "w_in": np.ascontiguousarray(inp["w_in"][0]), "wbrw": np.ascontiguousarray(inp["w_branch_rw"][0]),
        "wbret": np.ascontiguousarray(inp["w_branch_ret"][0]), "wout": np.ascontiguousarray(inp["w_out"][0]),
        "wup": np.ascontiguousarray(inp["ffn_w_up"][0]), "wdn": np.ascontiguousarray(inp["ffn_w_down"][0]),
        "lr": lr, "c128": c, "rows": rows, "tab": tab,
    }
    in_maps = []
    for i in range(ncores):
        xs = np.ascontiguousarray(inp["x"][i * NSEQ:(i + 1) * NSEQ, :T, :]).reshape(NSEQ * T, D)
        ps = np.ascontiguousarray(inp["positions"][i * NSEQ:(i + 1) * NSEQ, :T]).reshape(1, NSEQ * T).astype(np.int32)
        m = dict(shared); m["x"] = xs; m["pos"] = ps
        in_maps.append(m)
    res = run_bass_kernel_spmd(nc, in_maps, core_ids=list(range(ncores)))
    return res


def kernel(**inputs):
    inp = {k: np.asarray(v) for k, v in inputs.items()}
    B, T, _ = inp["x"].shape
    ncores = 8
    NSEQ = B // ncores
    res = run(inp, NSEQ, T // 128, ncores)
    out = np.concatenate([r["out"].reshape(NSEQ, T, D) for r in res.results], axis=0)
    return out.astype(np.float32)
```

```python
import math
import numpy as np
from contextlib import ExitStack
import concourse.bass as bass
import concourse.mybir as mybir
from concourse.bass_utils import run_bass_kernel_spmd

F32 = mybir.dt.float32; BF16 = mybir.dt.bfloat16; I32 = mybir.dt.int32
AF = mybir.ActivationFunctionType; ALU = mybir.AluOpType; AX = mybir.AxisListType

D = 1024; RWC = 1792; INC = 6912; DFF = 2816
C0 = math.exp(-0.5)
G1, MU, W0, A0, KK, KA, RK, LW, LB, INV, SSC, CD, G3, CB, CW, QD, KDEC, NCONST = (
    0, 8, 22, 26, 30, 34, 38, 42, 46, 50, 51, 52, 56, 64, 108, 240, 752, 760)
T_MP, T_M0, T_MT, T_BM, T_ID, T_SW, NTAB = 0, 512, 640, 1664, 1792, 1920, 2048


class Buf:
    def __init__(self, t, name):
        self.t = t; self.name = name; self.writers = {}; self.readers = {}

    def __getitem__(self, k):
        return self.t[k]


class Sched:
    ENG = ['pe', 'act', 'dve', 'pool', 'sp']

    def __init__(self, nc, stack):
        self.nc = nc; self.stack = stack; self.semh = {}
        for e in self.ENG:
            self.semh[e] = stack.enter_context(nc.semaphore("s_" + e))
        self.cnt = {e: 0 for e in self.ENG}
        self.known = {e: {} for e in self.ENG}
        self.prog = {e: [] for e in self.ENG}
        self.dcnt = {}
        self.nbuf = 0

    def sb(self, st, shape, dt):
        self.nbuf += 1
        name = f"b{self.nbuf}"
        return Buf(st.enter_context(self.nc.sbuf_tensor(name, list(shape), dt)), name)

    def ps(self, st, shape, dt):
        self.nbuf += 1
        name = f"p{self.nbuf}"
        b = Buf(st.enter_context(self.nc.psum_tensor(name, list(shape), dt)), name)
        b.psum = True
        return b

    def _waits(self, eng, reads, writes):
        need = {}

        def add(k, v, raw):
            if k == eng and not raw and eng == 'pe':
                return
            if need.get(k, 0) < v:
                need[k] = v
        for b in reads:
            for k, v in b.writers.items():
                add(k, v, True)
        for b in writes:
            for k, v in b.writers.items():
                add(k, v, False)
            for k, v in b.readers.items():
                add(k, v, False)
        out = []
        for k, v in need.items():
            if k.startswith('d_'):
                v = self.dcnt[k]
            if self.known[eng].get(k, 0) >= v:
                continue
            self.known[eng][k] = v
            out.append((self.semh[k], v))
        return out

    def op(self, eng, fn, reads=(), writes=()):
        pr = [b for b in reads if getattr(b, 'psum', False)]
        if pr:
            writes = list(writes) + [b for b in pr if b not in writes]
            reads = [b for b in reads if not getattr(b, 'psum', False)]
        waits = self._waits(eng, reads, writes)
        self.cnt[eng] += 1
        n = self.cnt[eng]
        sem = self.semh[eng]

        def run(e, waits=waits, fn=fn, sem=sem):
            for s, v in waits:
                e.wait_ge(s, v)
            fn(e).then_inc(sem, 1)
        self.prog[eng].append(run)
        for b in reads:
            if b.readers.get(eng, 0) < n:
                b.readers[eng] = n
        for b in writes:
            b.writers = {eng: n}; b.readers = {}

    def dma(self, q, fn, semname, reads=(), writes=()):
        key = 'd_' + semname
        if key not in self.semh:
            self.semh[key] = self.stack.enter_context(self.nc.semaphore(key))
            self.dcnt[key] = 0
        waits = self._waits(q, reads, writes)
        self.dcnt[key] += 16
        n = self.dcnt[key]
        sem = self.semh[key]

        def run(e, waits=waits, fn=fn, sem=sem):
            for s, v in waits:
                e.wait_ge(s, v)
            fn(e).then_inc(sem, 16)
        self.prog[q].append(run)
        for b in reads:
            if b.readers.get(key, 0) < n:
                b.readers[key] = n
        for b in writes:
            b.writers = dict(b.writers); b.writers[key] = n
            b.readers = {}

    def barrier(self):
        allc = {}
        for e in self.ENG:
            if self.cnt[e] > 0:
                allc[e] = self.cnt[e]
        for k, v in self.dcnt.items():
            if v > 0:
                allc[k] = v
        for e in self.ENG:
            waits = []
            for k, v in allc.items():
                if self.known[e].get(k, 0) >= v:
                    continue
                self.known[e][k] = v
                waits.append((self.semh[k], v))

            def run(en, waits=waits):
                for s, v in waits:
                    en.wait_ge(s, v)
            self.prog[e].append(run)

    def emit(self):
        nc = self.nc
        with nc.Block() as block:
            @block.tensor
            def _(e):
                for f in self.prog['pe']:
                    f(e)

            @block.scalar
            def _(e):
                for f in self.prog['act']:
                    f(e)

            @block.vector
            def _(e):
                for f in self.prog['dve']:
                    f(e)

            @block.gpsimd
            def _(e):
                for f in self.prog['pool']:
                    f(e)

            @block.sync
            def _(e):
                for f in self.prog['sp']:
                    f(e)


def build(NSEQ, NCH, dbg=False, phases="ABC"):
    T = NCH * 128; NTOK = NSEQ * T; NCHUNK = NSEQ * NCH
    FT = min(512, T); NSUB = FT // 128; NTILE = T // FT
    nc = bass.Bass("TRN2", target_bir_lowering=False)

    def dr(name, shape, dt, kind="ExternalInput"):
        return nc.dram_tensor(name, list(shape), dt, kind=kind).ap()
    x_d = dr("x", [NTOK, D], F32)
    pos_d = dr("pos", [1, NTOK], I32)
    win_d = dr("w_in", [D, INC], F32)
    wbrw_d = dr("wbrw", [512, D], F32)
    wbret_d = dr("wbret", [D, D], F32)
    wout_d = dr("wout", [D, D], F32)
    wup_d = dr("wup", [D, 2 * DFF], F32)
    wdn_d = dr("wdn", [DFF, D], F32)
    lr_d = dr("lr", [128, 1024], F32)
    c_d = dr("c128", [128, NCONST], F32)
    rows_d = dr("rows", [2, D], F32)
    tab_d = dr("tab", [128, NTAB], F32)
    out_d = dr("out", [NTOK, D], F32, kind="ExternalOutput")
    sk = "ExternalOutput" if dbg else "Internal"
    hnT_s = dr("hnT_s", [NCHUNK, 128, 1024], BF16, kind=sk)
    yrw_s = dr("yrw_s", [NCHUNK, 128, 512], BF16, kind=sk)
    h_s = dr("h_s", [NTOK, D], F32, kind=sk)

    with ExitStack() as st0:
        S = Sched(nc, st0)
        banks = [S.ps(st0, [128, 512], F32) for _ in range(8)]
        bstate = [0]

        def nb():
            b = banks[bstate[0] % 8]; bstate[0] += 1
            return b

        def v3(b, a):
            return b.t[:, :].rearrange("p (a b) -> p a b", a=a)

        def vbf(b):
            return b.t[:, :].bitcast(BF16)

        cst = S.sb(st0, [128, NCONST], F32)
        ones = S.sb(st0, [128, 128], F32)
        omka = S.sb(st0, [128, 4], F32)
        stAB = ExitStack()
        tabf = S.sb(stAB, [128, NTAB], F32)
        tabb = S.sb(stAB, [128, NTAB], BF16)
        rowsB = S.sb(stAB, [128, D], F32)
        S.dma('sp', lambda e: e.dma_start(out=cst[:, :], in_=c_d), 'cst', writes=[cst])
        S.dma('sp', lambda e: e.dma_start(out=tabf[:, :], in_=tab_d), 'tabf', writes=[tabf])
        S.dma('pool', lambda e: e.dma_start(out=tabb[:, :], in_=tab_d), 'tabb', writes=[tabb])
        S.dma('sp', lambda e: e.dma_start(out=rowsB[:, :], in_=rows_d[0:1, :].partition_broadcast(128)), 'rows', writes=[rowsB])
        S.op('pool', lambda e: e.memset(ones[:, :], 1.0), writes=[ones])
        S.op('dve', lambda e: e.tensor_scalar(out=omka[:, :], in0=cst[:, KA:KA + 4], scalar1=-1.0, scalar2=1.0,
                                              op0=ALU.mult, op1=ALU.add), reads=[cst], writes=[omka])
        ident = tabb[:, T_ID:T_ID + 128]
        bones = tabb[:, T_BM:T_BM + 128]
        pswap = tabb[:, T_SW:T_SW + 128]

        def cb(off, n, shape):
            return cst[:, off:off + n].unsqueeze(2).to_broadcast(shape)

        def rmsnorm_T(stp, xs, dstT, col0, gcol, tmp_bf, ss, rs, xn, use_ln=False, idn=None):
            idap, idbuf = idn if idn is not None else (ident, tabb)
            S.op('act', lambda e: e.activation(out=tmp_bf[:, :], in_=xs[:, :], func=AF.Square, accum_out=ss[:, 0:1]),
                 reads=[xs], writes=[tmp_bf, ss])
            if use_ln:
                S.op('act', lambda e: e.activation(out=rs[:, 0:1], in_=ss[:, 0:1], func=AF.Ln, scale=1.0 / D, bias=1e-6),
                     reads=[ss], writes=[rs])
                S.op('act', lambda e: e.activation(out=rs[:, 0:1], in_=rs[:, 0:1], func=AF.Exp, scale=-0.5), reads=[rs], writes=[rs])
            else:
                S.op('act', lambda e: e.activation(out=rs[:, 0:1], in_=ss[:, 0:1], func=AF.Sqrt, scale=1.0 / D, bias=1e-6),
                     reads=[ss], writes=[rs])
                S.op('dve', lambda e: e.reciprocal(out=rs[:, 0:1], in_=rs[:, 0:1]), reads=[rs], writes=[rs])
            S.op('dve', lambda e: e.tensor_scalar(out=xn[:, :], in0=xs[:, :], scalar1=rs[:, 0:1], scalar2=None,
                                                  op0=ALU.mult), reads=[xs, rs], writes=[xn])
            bk = nb()
            for kc in range(8):
                S.op('pe', lambda e, kc=kc: e.transpose(out=vbf(bk)[:, kc * 128:(kc + 1) * 128],
                                                        in_=xn[:, kc * 128:(kc + 1) * 128], identity=idap),
                     reads=[xn, idbuf], writes=[bk])
            S.op('dve', lambda e: e.tensor_tensor(
                out=dstT[:, :, col0:col0 + 128],
                in0=vbf(bk).rearrange("p (a b) -> p a b", a=8),
                in1=cb(gcol, 8, [128, 8, 128]), op=ALU.mult), reads=[bk, cst], writes=[dstT])

        if "A" in phases:
          with ExitStack() as st:
            w_rw = S.sb(st, [128, 8, RWC], BF16)
            lrb = S.sb(st, [128, 1024], BF16)
            for kc in range(8):
                S.dma('pool', lambda e, kc=kc: e.dma_start(out=w_rw[:, kc, :], in_=win_d[kc * 128:(kc + 1) * 128, 0:RWC]),
                      'wA', writes=[w_rw])
            S.dma('pool', lambda e: e.dma_start(out=lrb[:, :], in_=lr_d), 'wA', writes=[lrb])
            xs2 = [S.sb(st, [128, D], F32) for _ in range(2)]
            tmp_bf = S.sb(st, [128, D], BF16); xn = S.sb(st, [128, D], BF16)
            ss = S.sb(st, [128, 1], F32); rs = S.sb(st, [128, 1], F32)
            hnT = S.sb(st, [128, 8, 128], BF16)
            praw = S.sb(st, [128, 14, 129], F32)
            pl = S.sb(st, [128, 14, 128], F32)
            lr12 = S.sb(st, [128, 128], BF16); sg = S.sb(st, [128, 128], BF16)
            f = [S.sb(st, [128, 4, 128], F32) for _ in range(16)]
            (gT, sig, cs, csx, Epos, Eneg, Em, Ehat, al, kq, kkb, kp, bv, t0, t1, t2) = f
            sqb = S.sb(st, [128, 4, 128], BF16)
            SCin = S.sb(st, [128, 4, 2, 128], BF16)
            BtT = S.sb(st, [128, 4, 128], BF16); KtT = S.sb(st, [128, 4, 128], BF16)
            BhT = S.sb(st, [128, 4, 128], BF16); KhT = S.sb(st, [128, 4, 128], BF16)
            vTb = S.sb(st, [128, 4, 128], BF16)
            Vp = S.sb(st, [128, 4, 128], BF16); BH = S.sb(st, [128, 4, 128], BF16); KH = S.sb(st, [128, 4, 128], BF16)
            VZ = S.sb(st, [128, 4, 2, 128], BF16); AZ = S.sb(st, [128, 4, 2, 128], BF16); UVZ = S.sb(st, [128, 4, 2, 128], BF16)
            Ap = S.sb(st, [128, 4, 128], BF16); UVp = S.sb(st, [128, 4, 128], BF16)
            SCT = S.sb(st, [128, 4, 2, 512], BF16)
            Xs = S.sb(st, [128, 4, 2, 128], BF16)
            MN = S.sb(st, [128, 4, 2, 256], BF16)
            Xt = [Buf(Xs.t, 'Xt%d' % q_) for q_ in range(4)]
            MNt = [Buf(MN.t, 'MNt%d' % q_) for q_ in range(4)]
            RhT = S.sb(st, [128, 4, 128], BF16); GZ = S.sb(st, [128, 4, 128], BF16)
            HZ = S.sb(st, [128, 4, 128], BF16); Hf = S.sb(st, [128, 4, 128], F32)
            yb = S.sb(st, [128, 4, 128], BF16); ysq = S.sb(st, [128, 4, 128], BF16); rkb = S.sb(st, [128, 4, 128], BF16)
            yrwT = S.sb(st, [128, 4, 128], BF16)
            for z in (VZ, AZ, UVZ):
                S.op('pool', lambda e, z=z: e.memset(z[:, :, :, :], 0.0), writes=[z])
            bmf = tabf[:, T_BM:T_BM + 128].unsqueeze(1).to_broadcast([128, 4, 128])
            prt = [Buf(praw.t, 'prt%d' % q_) for q_ in range(4)]
            plt = [Buf(pl.t, 'plt%d' % q_) for q_ in range(4)]
            tmpS = [S.sb(st, [128, 512], BF16) for _ in range(2)]

            DBG.update({k_: v_.name for k_, v_ in list(locals().items()) if isinstance(v_, Buf)})

            def _chunk(ci):
                b_i, c_i = divmod(ci, NCH)
                tok0 = ci * 128
                xs = xs2[ci % 2]
                S.dma('sp', lambda e, xs=xs, tok0=tok0: e.dma_start(out=xs[:, :], in_=x_d[tok0:tok0 + 128, :]),
                      'xA%d' % (ci % 2), writes=[xs])
                if c_i == 0:
                    S.op('pool', lambda e: e.memset(praw[:, :, 0:1], 0.0), writes=prt)
                    S.op('pool', lambda e: e.memset(Hf[:, :, :], 0.0), writes=[Hf])
                    S.op('pool', lambda e: e.memset(HZ[:, :, :], 0.0), writes=[HZ])
                rmsnorm_T(st, xs, hnT, 0, G1, tmp_bf, ss, rs, xn, use_ln=True)
                S.dma('sp', lambda e, ci=ci: e.dma_start(out=hnT_s[ci].rearrange("p (a b) -> p a b", a=8), in_=hnT[:, :, :]),
                      'hst', reads=[hnT])
                if _stop(1):
                    return
                def proj_group(g):
                    bk = nb(); n = 4 if g < 3 else 2
                    j0 = g * 4
                    for jj in range(n):
                        j = j0 + jj
                        for kc in range(8):
                            S.op('pe', lambda e, jj=jj, j=j, kc=kc: e.matmul(
                                bk[:, jj * 128:(jj + 1) * 128], lhsT=w_rw[:, kc, j * 128:(j + 1) * 128], rhs=hnT[:, kc, :],
                                start=(kc == 0), stop=(kc == 7)), reads=[w_rw, hnT], writes=[bk])
                    S.op('act', lambda e: e.copy(out=praw[:, j0:j0 + n, 1:129], in_=v3(bk, 4)[:, 0:n, :]), reads=[bk], writes=[prt[g]])
                    S.op('dve', lambda e: e.tensor_tensor(out=pl[:, j0:j0 + n, :], in0=praw[:, j0:j0 + n, 0:128], in1=praw[:, j0:j0 + n, 1:129],
                                                          op=ALU.subtract), reads=[prt[g]], writes=[plt[g]])
                    S.op('pool', lambda e: e.tensor_tensor(out=pl[:, j0:j0 + n, :], in0=pl[:, j0:j0 + n, :], in1=cb(MU + j0, n, [128, n, 128]),
                                                           op=ALU.mult), reads=[plt[g], cst], writes=[plt[g]])
                    S.op('dve', lambda e: e.tensor_tensor(out=pl[:, j0:j0 + n, :], in0=pl[:, j0:j0 + n, :], in1=praw[:, j0:j0 + n, 1:129],
                                                          op=ALU.add), reads=[plt[g], prt[g]], writes=[plt[g]])
                    S.op('act', lambda e: e.copy(out=praw[:, j0:j0 + n, 0:1], in_=praw[:, j0:j0 + n, 128:129]), reads=[prt[g]], writes=[prt[g]])

                proj_group(3)
                proj_group(1)
                if _stop(2):
                    return
                S.op('act', lambda e: e.activation(out=lr12[0:64, :], in_=pl[0:64, 12, :], func=AF.Tanh), reads=[plt[3]], writes=[lr12])
                S.op('act', lambda e: e.copy(out=lr12[64:128, :], in_=pl[64:128, 12, :]), reads=[plt[3]], writes=[lr12])
                S.op('act', lambda e: e.activation(out=sg[:, :], in_=pl[:, 13, :], func=AF.Sigmoid), reads=[plt[3]], writes=[sg])
                proj_group(0)
                bw = nb(); ba = nb(); bg = nb()
                for jc in range(4):
                    S.op('pe', lambda e, jc=jc: e.matmul(bw[:, jc * 128:(jc + 1) * 128], lhsT=lrb[0:64, jc * 128:(jc + 1) * 128],
                                                         rhs=lr12[0:64, :], start=True, stop=True), reads=[lrb, lr12], writes=[bw])
                for jc in range(4):
                    S.op('pe', lambda e, jc=jc: e.matmul(ba[:, jc * 128:(jc + 1) * 128], lhsT=lrb[64:128, jc * 128:(jc + 1) * 128],
                                                         rhs=lr12[64:128, :], start=True, stop=True), reads=[lrb, lr12], writes=[ba])
                for jc in range(4):
                    S.op('pe', lambda e, jc=jc: e.matmul(bg[:, jc * 128:(jc + 1) * 128], lhsT=lrb[:, 512 + jc * 128:512 + (jc + 1) * 128],
                                                         rhs=sg[:, :], start=True, stop=True), reads=[lrb, sg], writes=[bg])
                proj_group(2)
                if _stop(3):
                    return
                S.op('dve', lambda e: e.tensor_tensor(out=t0[:, :, :], in0=v3(bw, 4), in1=cb(W0, 4, [128, 4, 128]), op=ALU.add),
                     reads=[bw, cst], writes=[t0])
                S.op('act', lambda e: e.activation(out=sig[:, :, :], in_=t0[:, :, :], func=AF.Sigmoid), reads=[t0], writes=[sig])
                S.op('dve', lambda e: e.tensor_tensor(out=t2[:, :, :], in0=v3(ba, 4), in1=cb(A0, 4, [128, 4, 128]), op=ALU.add),
                     reads=[ba, cst], writes=[t2])
                S.op('act', lambda e: e.activation(out=al[:, :, :], in_=t2[:, :, :], func=AF.Sigmoid), reads=[t2], writes=[al])
                S.op('act', lambda e: e.copy(out=gT[:, :, :], in_=v3(bg, 4)), reads=[bg], writes=[gT])
                for jc in range(4):
                    S.op('dve', lambda e, jc=jc: e.tensor_tensor_scan(out=cs[:, jc, :], data0=ones[:, :], data1=sig[:, jc, :],
                                                                      initial=0.0, op0=ALU.mult, op1=ALU.add),
                         reads=[ones, sig], writes=[cs])
                S.op('pool', lambda e: e.tensor_tensor(out=csx[:, :, :], in0=cs[:, :, :], in1=sig[:, :, :], op=ALU.subtract),
                     reads=[cs, sig], writes=[csx])
                S.op('act', lambda e: e.activation(out=Epos[:, :, :], in_=cs[:, :, :], func=AF.Exp, scale=-C0), reads=[cs], writes=[Epos])
                S.op('act', lambda e: e.activation(out=Eneg[:, :, :], in_=cs[:, :, :], func=AF.Exp, scale=C0), reads=[cs], writes=[Eneg])
                S.op('act', lambda e: e.activation(out=Em[:, :, :], in_=csx[:, :, :], func=AF.Exp, scale=-C0), reads=[csx], writes=[Em])
                S.op('dve', lambda e: e.tensor_tensor(out=t1[:, :, :], in0=cs[:, :, 127:128].to_broadcast([128, 4, 128]),
                                                      in1=cs[:, :, :], op=ALU.subtract), reads=[cs], writes=[t1])
                S.op('act', lambda e: e.activation(out=Ehat[:, :, :], in_=t1[:, :, :], func=AF.Exp, scale=-C0), reads=[t1], writes=[Ehat])
                S.op('pool', lambda e: e.tensor_tensor(out=kq[:, :, :], in0=pl[:, 4:8, :], in1=cb(KK, 4, [128, 4, 128]), op=ALU.mult),
                     reads=[plt[1], cst], writes=[kq])
                S.op('pool', lambda e: e.tensor_tensor(out=sqb[:, :, :], in0=kq[:, :, :], in1=kq[:, :, :], op=ALU.mult),
                     reads=[kq], writes=[sqb])
                bs = nb()
                for jc in range(4):
                    S.op('pe', lambda e, jc=jc: e.matmul(bs[:, jc * 128:(jc + 1) * 128], lhsT=bones, rhs=sqb[:, jc, :],
                                                         start=True, stop=True), reads=[tabb, sqb], writes=[bs])
                S.op('dve', lambda e: e.tensor_scalar(out=t0[:, :, :], in0=v3(bs, 4), scalar1=1e-24, scalar2=None, op0=ALU.max),
                     reads=[bs], writes=[t0])
                S.op('act', lambda e: e.activation(out=t0[:, :, :], in_=t0[:, :, :], func=AF.Ln, scale=float(2.0 ** 40)), reads=[t0], writes=[t0])
                S.op('act', lambda e: e.activation(out=t0[:, :, :], in_=t0[:, :, :], func=AF.Exp, scale=-0.5, bias=20.0 * math.log(2.0)),
                     reads=[t0], writes=[t0])
                S.op('dve', lambda e: e.tensor_tensor(out=kkb[:, :, :], in0=kq[:, :, :], in1=t0[:, :, :], op=ALU.mult),
                     reads=[kq, t0], writes=[kkb])
                S.op('pool', lambda e: e.tensor_tensor(out=t2[:, :, :], in0=al[:, :, :], in1=cb(KA, 4, [128, 4, 128]), op=ALU.mult),
                     reads=[al, cst], writes=[t2])
                S.op('pool', lambda e: e.tensor_tensor(out=t2[:, :, :], in0=t2[:, :, :],
                                                       in1=omka[:, :].unsqueeze(2).to_broadcast([128, 4, 128]), op=ALU.add),
                     reads=[t2, omka], writes=[t2])
                S.op('dve', lambda e: e.tensor_tensor(out=kp[:, :, :], in0=pl[:, 4:8, :], in1=t2[:, :, :], op=ALU.mult),
                     reads=[plt[1], t2], writes=[kp])
                if _stop(4):
                    return
                S.op('pool', lambda e: e.tensor_tensor(out=t1[:, :, :], in0=kkb[:, :, :], in1=Em[:, :, :], op=ALU.mult),
                     reads=[kkb, Em], writes=[t1])
                S.op('act', lambda e: e.activation(out=SCin[:, :, 0, :], in_=t1[:, :, :], func=AF.Identity, scale=-1.0),
                     reads=[t1], writes=[SCin])
                S.op('pool', lambda e: e.tensor_tensor(out=bv[:, :, :], in0=kkb[:, :, :], in1=al[:, :, :], op=ALU.mult),
                     reads=[kkb, al], writes=[bv])
                S.op('dve', lambda e: e.tensor_tensor(out=BtT[:, :, :], in0=bv[:, :, :], in1=Eneg[:, :, :], op=ALU.mult),
                     reads=[bv, Eneg], writes=[BtT])
                S.op('pool', lambda e: e.tensor_tensor(out=KtT[:, :, :], in0=kp[:, :, :], in1=Eneg[:, :, :], op=ALU.mult),
                     reads=[kp, Eneg], writes=[KtT])
                S.op('dve', lambda e: e.tensor_tensor(out=SCin[:, :, 1, :], in0=pl[:, 0:4, :], in1=Epos[:, :, :], op=ALU.mult),
                     reads=[plt[0], Epos], writes=[SCin])
                S.op('pool', lambda e: e.tensor_tensor(out=BhT[:, :, :], in0=bv[:, :, :], in1=Ehat[:, :, :], op=ALU.mult),
                     reads=[bv, Ehat], writes=[BhT])
                S.op('dve', lambda e: e.tensor_tensor(out=KhT[:, :, :], in0=kp[:, :, :], in1=Ehat[:, :, :], op=ALU.mult),
                     reads=[kp, Ehat], writes=[KhT])
                S.op('act', lambda e: e.copy(out=vTb[:, :, :], in_=pl[:, 8:12, :]), reads=[plt[2]], writes=[vTb])
                if _stop(4.3):
                    return
                bt1 = nb(); bt2 = nb()
                for jc in range(4):
                    S.op('pe', lambda e, jc=jc: e.transpose(out=vbf(bt1)[:, jc * 128:(jc + 1) * 128], in_=SCin[:, jc, 0, :], identity=ident),
                         reads=[SCin, tabb], writes=[bt1])
                    S.op('pe', lambda e, jc=jc: e.transpose(out=vbf(bt1)[:, 512 + jc * 128:512 + (jc + 1) * 128], in_=vTb[:, jc, :], identity=ident),
                         reads=[vTb, tabb], writes=[bt1])
                    S.op('pe', lambda e, jc=jc: e.transpose(out=vbf(bt2)[:, jc * 128:(jc + 1) * 128], in_=BhT[:, jc, :], identity=ident),
                         reads=[BhT, tabb], writes=[bt2])
                    S.op('pe', lambda e, jc=jc: e.transpose(out=vbf(bt2)[:, 512 + jc * 128:512 + (jc + 1) * 128], in_=KhT[:, jc, :], identity=ident),
                         reads=[KhT, tabb], writes=[bt2])
                if _stop(4.6):
                    return
                b1v = vbf(bt1).rearrange("p (k a h c) -> p k a h c", k=2, a=4, h=2)
                S.op('act', lambda e: e.copy(out=Xs[:, :, :, 0:64], in_=b1v[:, 0, :, :, :]), reads=[bt1], writes=Xt)
                if _stop(4.7):
                    return
                S.op('dve', lambda e: e.tensor_copy(out=Vp[:, :, :], in_=vbf(bt1)[:, 512:1024].rearrange("p (a b) -> p a b", a=4)),
                     reads=[bt1], writes=[Vp])
                if _stop(4.8):
                    return
                for hp in range(2):
                    S.op('act', lambda e, hp=hp: e.copy(out=VZ[:, :, hp, hp * 64:(hp + 1) * 64], in_=b1v[:, 1, :, hp, :]),
                         reads=[bt1], writes=[VZ])
                if _stop(4.85):
                    return
                S.op('dve', lambda e: e.tensor_copy(out=BH[:, :, :], in_=vbf(bt2)[:, 0:512].rearrange("p (a b) -> p a b", a=4)),
                     reads=[bt2], writes=[BH])
                if _stop(4.9):
                    return
                S.op('act', lambda e: e.copy(out=KH[:, :, :], in_=vbf(bt2)[:, 512:1024].rearrange("p (a b) -> p a b", a=4)),
                     reads=[bt2], writes=[KH])
                if _stop(5):
                    return
                b3 = [None, None]
                for jc in range(4):
                    for hp in range(2):
                        pb = 64 * hp; h = 2 * jc + hp
                        if h % 4 == 0:
                            b3 = [nb(), nb()]
                        bk = nb()
                        rhs2 = SCin[pb:pb + 64, jc, :, :].rearrange("p a b -> p (a b)")
                        S.op('pe', lambda e, bk=bk, jc=jc, pb=pb, rhs2=rhs2: e.matmul(bk[:, 0:256], lhsT=BtT[pb:pb + 64, jc, :], rhs=rhs2,
                                                                                      start=True, stop=True), reads=[BtT, SCin], writes=[bk])
                        S.op('pe', lambda e, bk=bk, jc=jc, pb=pb, rhs2=rhs2: e.matmul(bk[:, 256:512], lhsT=KtT[pb:pb + 64, jc, :], rhs=rhs2,
                                                                                      start=True, stop=True), reads=[KtT, SCin], writes=[bk])
                        if hp == 0:
                            S.op('dve', lambda e, bk=bk, jc=jc, hp=hp: e.tensor_tensor(out=SCT[:, jc, hp, :], in0=bk[:, :], in1=tabf[:, T_MP:T_MP + 512],
                                                                                       op=ALU.mult), reads=[bk, tabf], writes=[SCT])
                        else:
                            tS = tmpS[jc % 2]
                            S.op('act', lambda e, bk=bk, tS=tS: e.copy(out=tS[:, :], in_=bk[:, :]), reads=[bk], writes=[tS])
                            S.op('pool', lambda e, tS=tS, jc=jc, hp=hp: e.tensor_tensor(out=SCT[:, jc, hp, :], in0=tS[:, :], in1=tabb[:, T_MP:T_MP + 512],
                                                                                        op=ALU.mult), reads=[tS, tabb], writes=[SCT])
                        b3k = b3[hp]
                        S.op('pe', lambda e, b3k=b3k, jc=jc, pb=pb: e.matmul(b3k[:, (jc % 2) * 128:(jc % 2 + 1) * 128], lhsT=SCin[pb:pb + 64, jc, 0, :],
                                                                             rhs=BtT[pb:pb + 64, jc, :], start=True, stop=True),
                             reads=[SCin, BtT], writes=[b3k])
                        if h % 4 == 3:
                            for hq in range(2):
                                S.op('dve', lambda e, g=h // 4, hq=hq, b3g=b3[hq]: e.tensor_tensor(
                                    out=MN[:, 2 * g:2 * g + 2, hq, 0:128],
                                    in0=b3g.t[:, 0:256].rearrange("p (a c) -> p a c", a=2),
                                    in1=tabf[:, T_M0:T_M0 + 128].unsqueeze(1).to_broadcast([128, 2, 128]), op=ALU.mult),
                                    reads=[b3[hq], tabf], writes=[MNt[2 * (h // 4)], MNt[2 * (h // 4) + 1]])
                if _stop(6):
                    return
                for g in range(2):
                    bk = nb()
                    for hh in range(4):
                        jc = 2 * g + hh // 2; hp = hh % 2
                        S.op('pe', lambda e, bk=bk, hh=hh, jc=jc, hp=hp: e.matmul(bk[:, hh * 128:(hh + 1) * 128], lhsT=SCT[:, jc, hp, 256:384],
                                                                                  rhs=Vp[:, jc, :], start=True, stop=True), reads=[SCT, Vp], writes=[bk])
                    bvw = bk.t[:, :].rearrange("p (a h g c) -> p a h g c", a=2, h=2, g=2)
                    for hp in range(2):
                        S.op('act', lambda e, bvw=bvw, g=g, hp=hp: e.copy(out=Xs[:, 2 * g:2 * g + 2, hp, 64:128], in_=bvw[:, :, hp, hp, :]),
                             reads=[bk], writes=[Xt[2 * g], Xt[2 * g + 1]])
                if _stop(7):
                    return
                for j in range(7):
                    for jc in range(4):
                        bP = nb(); bQ = nb() if j < 6 else None
                        for hp in range(2):
                            Nj = SCT[:, jc, hp, 0:128] if j == 0 else MN[:, jc, hp, 128:256]
                            nsrc = [SCT] if j == 0 else []
                            S.op('pe', lambda e, bP=bP, hp=hp, jc=jc, Nj=Nj: e.matmul(bP[:, hp * 128:(hp + 1) * 128], lhsT=Nj, rhs=Xs[:, jc, hp, :],
                                                                                     start=True, stop=True), reads=nsrc + [Xt[jc], MNt[jc]], writes=[bP])
                            if j < 6:
                                S.op('pe', lambda e, bQ=bQ, hp=hp, jc=jc, Nj=Nj: e.matmul(bQ[:, hp * 256:hp * 256 + 128], lhsT=Nj, rhs=MN[:, jc, hp, 0:128],
                                                                                         start=True, stop=True), reads=nsrc + [MNt[jc]], writes=[bQ])
                                S.op('pe', lambda e, bQ=bQ, hp=hp, jc=jc, Nj=Nj: e.matmul(bQ[:, hp * 256 + 128:hp * 256 + 256], lhsT=MN[:, jc, hp, 0:128], rhs=Nj,
                                                                                         start=True, stop=True), reads=nsrc + [MNt[jc]], writes=[bQ])
                        S.op('dve', lambda e, jc=jc, bP=bP: e.tensor_tensor(out=Xs[:, jc, :, :], in0=Xs[:, jc, :, :],
                                                                            in1=bP.t[:, 0:256].rearrange("p (h c) -> p h c", h=2), op=ALU.add),
                             reads=[Xt[jc], bP], writes=[Xt[jc]])
                        if j < 6:
                            S.op('act', lambda e, jc=jc, bQ=bQ: e.copy(out=MN[:, jc, :, :], in_=bQ.t[:, :].rearrange("p (h c) -> p h c", h=2)),
                                 reads=[bQ], writes=[MNt[jc]])
                if _stop(8):
                    return
                for hp in range(2):
                    S.op('act', lambda e, hp=hp: e.copy(out=AZ[:, :, hp, hp * 64:(hp + 1) * 64], in_=Xs[:, :, hp, 0:64]), reads=Xt, writes=[AZ])
                    S.op('pool', lambda e, hp=hp: e.tensor_copy(out=UVZ[:, :, hp, hp * 64:(hp + 1) * 64], in_=Xs[:, :, hp, 64:128]), reads=Xt, writes=[UVZ])
                S.op('pool', lambda e: e.tensor_copy(out=Ap[:, :, :].rearrange("p a (h c) -> p a h c", h=2), in_=Xs[:, :, :, 0:64]), reads=Xt, writes=[Ap])
                S.op('dve', lambda e: e.tensor_copy(out=UVp[:, :, :].rearrange("p a (h c) -> p a h c", h=2), in_=Xs[:, :, :, 64:128]), reads=Xt, writes=[UVp])
                bR = nb(); bG = nb()
                for jc in range(4):
                    for hp in range(2):
                        S.op('pe', lambda e, jc=jc, hp=hp: e.matmul(bR[:, jc * 128:(jc + 1) * 128], lhsT=AZ[:, jc, hp, :], rhs=SCT[:, jc, hp, 128:256],
                                                                    start=(hp == 0), stop=(hp == 1)), reads=[AZ, SCT], writes=[bR])
                for jc in range(4):
                    S.op('pe', lambda e, jc=jc: e.matmul(bG[:, jc * 128:(jc + 1) * 128], lhsT=Ap[:, jc, :], rhs=BH[:, jc, :], start=True, stop=True),
                         reads=[Ap, BH], writes=[bG])
                S.op('dve', lambda e: e.tensor_tensor(out=RhT[:, :, :], in0=v3(bR, 4), in1=SCin[:, :, 1, :], op=ALU.add), reads=[bR, SCin], writes=[RhT])
                S.op('dve', lambda e: e.tensor_tensor(out=GZ[:, :, :], in0=v3(bG, 4), in1=bmf, op=ALU.mult), reads=[bG, tabf], writes=[GZ])
                if _stop(9):
                    return
                bY = nb(); bH = nb()
                for jc in range(4):
                    for hp in range(2):
                        S.op('pe', lambda e, jc=jc, hp=hp: e.matmul(bY[:, jc * 128:(jc + 1) * 128], lhsT=UVZ[:, jc, hp, :], rhs=SCT[:, jc, hp, 128:256],
                                                                    start=(hp == 0), stop=False), reads=[UVZ, SCT], writes=[bY])
                        S.op('pe', lambda e, jc=jc, hp=hp: e.matmul(bY[:, jc * 128:(jc + 1) * 128], lhsT=VZ[:, jc, hp, :], rhs=SCT[:, jc, hp, 384:512],
                                                                    start=False, stop=False), reads=[VZ, SCT], writes=[bY])
                    S.op('pe', lambda e, jc=jc: e.matmul(bY[:, jc * 128:(jc + 1) * 128], lhsT=HZ[:, jc, :], rhs=RhT[:, jc, :], start=False, stop=True),
                         reads=[HZ, RhT], writes=[bY])
                for jc in range(4):
                    S.op('pe', lambda e, jc=jc: e.matmul(bH[:, jc * 128:(jc + 1) * 128], lhsT=BH[:, jc, :], rhs=UVp[:, jc, :], start=True, stop=False),
                         reads=[BH, UVp], writes=[bH])
                    S.op('pe', lambda e, jc=jc: e.matmul(bH[:, jc * 128:(jc + 1) * 128], lhsT=KH[:, jc, :], rhs=Vp[:, jc, :], start=False, stop=False),
                         reads=[KH, Vp], writes=[bH])
                    S.op('pe', lambda e, jc=jc: e.matmul(bH[:, jc * 128:(jc + 1) * 128], lhsT=GZ[:, jc, :], rhs=HZ[:, jc, :], start=False, stop=True),
                         reads=[GZ, HZ], writes=[bH])
                for jc in range(4):
                    S.op('dve', lambda e, jc=jc: e.scalar_tensor_tensor(out=Hf[:, jc, :], in0=Hf[:, jc, :], scalar=Epos[:, jc, 127:128],
                                                                        in1=bH[:, jc * 128:(jc + 1) * 128], op0=ALU.mult, op1=ALU.add),
                         reads=[Hf, Epos, bH], writes=[Hf])
                S.op('pool', lambda e: e.tensor_tensor(out=Hf[:, :, :], in0=Hf[:, :, :], in1=bmf, op=ALU.mult), reads=[Hf, tabf], writes=[Hf])
                S.op('act', lambda e: e.copy(out=HZ[:, :, :], in_=Hf[:, :, :]), reads=[Hf], writes=[HZ])
                if _stop(10):
                    return
                S.op('act', lambda e: e.copy(out=yb[:, :, :], in_=v3(bY, 4)), reads=[bY], writes=[yb])
                S.op('act', lambda e: e.activation(out=ysq[:, :, :], in_=v3(bY, 4), func=AF.Square), reads=[bY], writes=[ysq])
                S.op('pool', lambda e: e.tensor_tensor(out=t0[:, :, :], in0=pl[:, 0:4, :], in1=kp[:, :, :], op=ALU.mult), reads=[plt[0], kp], writes=[t0])
                S.op('pool', lambda e: e.tensor_tensor(out=rkb[:, :, :], in0=t0[:, :, :], in1=cb(RK, 4, [128, 4, 128]), op=ALU.mult),
                     reads=[t0, cst], writes=[rkb])
                bM = nb(); bQ = nb(); bO = nb()
                for jc in range(4):
                    S.op('pe', lambda e, jc=jc: e.matmul(bM[:, jc * 128:(jc + 1) * 128], lhsT=bones, rhs=yb[:, jc, :], start=True, stop=True),
                         reads=[tabb, yb], writes=[bM])
                    S.op('pe', lambda e, jc=jc: e.matmul(bQ[:, jc * 128:(jc + 1) * 128], lhsT=bones, rhs=ysq[:, jc, :], start=True, stop=True),
                         reads=[tabb, ysq], writes=[bQ])
                    S.op('pe', lambda e, jc=jc: e.matmul(bO[:, jc * 128:(jc + 1) * 128], lhsT=bones, rhs=rkb[:, jc, :], start=True, stop=True),
                         reads=[tabb, rkb], writes=[bO])
                S.op('act', lambda e: e.activation(out=t1[:, :, :], in_=v3(bM, 4), func=AF.Identity, scale=1.0 / 64), reads=[bM], writes=[t1])
                S.op('dve', lambda e: e.tensor_tensor(out=t2[:, :, :], in0=t1[:, :, :], in1=t1[:, :, :], op=ALU.mult), reads=[t1], writes=[t2])
                S.op('dve', lambda e: e.scalar_tensor_tensor(out=t2[:, :, :], in0=v3(bQ, 4), scalar=1.0 / 64, in1=t2[:, :, :],
                                                             op0=ALU.mult, op1=ALU.subtract), reads=[bQ, t2], writes=[t2])
                S.op('act', lambda e: e.activation(out=t2[:, :, :], in_=t2[:, :, :], func=AF.Ln, bias=64e-5), reads=[t2], writes=[t2])
                S.op('act', lambda e: e.activation(out=t2[:, :, :], in_=t2[:, :, :], func=AF.Exp, scale=-0.5), reads=[t2], writes=[t2])
                S.op('dve', lambda e: e.tensor_tensor(out=t0[:, :, :], in0=v3(bY, 4), in1=t1[:, :, :], op=ALU.subtract), reads=[bY, t1], writes=[t0])
                S.op('dve', lambda e: e.tensor_tensor(out=t0[:, :, :], in0=t0[:, :, :], in1=t2[:, :, :], op=ALU.mult), reads=[t0, t2], writes=[t0])
                S.op('dve', lambda e: e.tensor_tensor(out=t0[:, :, :], in0=t0[:, :, :], in1=cb(LW, 4, [128, 4, 128]), op=ALU.mult),
                     reads=[t0, cst], writes=[t0])
                S.op('dve', lambda e: e.tensor_tensor(out=t0[:, :, :], in0=t0[:, :, :], in1=cb(LB, 4, [128, 4, 128]), op=ALU.add),
                     reads=[t0, cst], writes=[t0])
                S.op('dve', lambda e: e.tensor_tensor(out=t1[:, :, :], in0=v3(bO, 4), in1=pl[:, 8:12, :], op=ALU.mult), reads=[bO, plt[2]], writes=[t1])
                S.op('dve', lambda e: e.tensor_tensor(out=t0[:, :, :], in0=t0[:, :, :], in1=t1[:, :, :], op=ALU.add), reads=[t0, t1], writes=[t0])
                S.op('pool', lambda e: e.tensor_tensor(out=yrwT[:, :, :], in0=t0[:, :, :], in1=gT[:, :, :], op=ALU.mult), reads=[t0, gT], writes=[yrwT])
                S.dma('pool', lambda e, ci=ci: e.dma_start(out=yrw_s[ci].rearrange("p (a b) -> p a b", a=4), in_=yrwT[:, :, :]), 'yst', reads=[yrwT])
            for ci in range(NCHUNK):
                _chunk(ci)
            S.barrier()

        if "B" in phases:
          with ExitStack() as st:
            NBC = INC - RWC
            w_b = S.sb(st, [128, 8, NBC], BF16)
            wbrw = S.sb(st, [128, 4, D], BF16); wbret = S.sb(st, [128, 8, D], BF16); wout = S.sb(st, [128, 8, D], BF16)
            for kc in range(8):
                S.dma('pool', lambda e, kc=kc: e.dma_start(out=w_b[:, kc, :], in_=win_d[kc * 128:(kc + 1) * 128, RWC:INC]), 'wB', writes=[w_b])
                S.dma('pool', lambda e, kc=kc: e.dma_start(out=wbret[:, kc, :], in_=wbret_d[kc * 128:(kc + 1) * 128, :]), 'wB', writes=[wbret])
                S.dma('pool', lambda e, kc=kc: e.dma_start(out=wout[:, kc, :], in_=wout_d[kc * 128:(kc + 1) * 128, :]), 'wB', writes=[wout])
            for kc in range(4):
                S.dma('pool', lambda e, kc=kc: e.dma_start(out=wbrw[:, kc, :], in_=wbrw_d[kc * 128:(kc + 1) * 128, :]), 'wB', writes=[wbrw])
            _xb = S.sb(st, [128, D], F32); xs2 = [_xb, _xb]
            hnT2 = [S.sb(st, [128, 8, 128], BF16) for _ in range(2)]
            yrw2 = [S.sb(st, [128, 4, 128], BF16) for _ in range(2)]
            posi2 = [S.sb(st, [128, 128], I32) for _ in range(2)]
            posf = S.sb(st, [128, 128], F32); u0 = S.sb(st, [128, 128], F32); u1 = S.sb(st, [128, 128], F32)
            ti = S.sb(st, [128, 128], I32); tf = S.sb(st, [128, 128], F32)
            cosT = S.sb(st, [128, 128], F32); sinT = S.sb(st, [128, 128], F32)
            qk = S.sb(st, [128, 8, 128], F32); qkb = S.sb(st, [128, 8, 128], BF16)
            r1 = S.sb(st, [128, 8, 128], F32); r2 = S.sb(st, [128, 8, 128], F32)
            rot = S.sb(st, [128, 8, 128], BF16); qd = S.sb(st, [128, 4, 128], BF16)
            v_bf = S.sb(st, [128, D], BF16); sgb = S.sb(st, [128, D], BF16); sA = S.sb(st, [128, D], BF16); sB = S.sb(st, [128, D], BF16)
            kdZ = S.sb(st, [128, 4, 2, 128], BF16)
            sT = S.sb(st, [128, 8, 128], BF16)
            Rf = S.sb(st, [128, 4, 128], F32); Rb = S.sb(st, [128, 4, 128], BF16)
            of = qk; osq = r1
            stt = S.sb(st, [128, 16], F32); mean = S.sb(st, [128, 8], F32); var = S.sb(st, [128, 8], F32)
            yret = S.sb(st, [128, D], BF16); yretT = S.sb(st, [128, 8, 128], BF16)
            m1 = S.sb(st, [128, D], F32); m2 = S.sb(st, [128, D], F32); mg = S.sb(st, [128, D], BF16); mT = S.sb(st, [128, 8, 128], BF16)
            mo = m2; junk = mg; ss = S.sb(st, [128, 1], F32); rs = S.sb(st, [128, 1], F32)
            hh = m1
            S.op('pool', lambda e: e.memset(kdZ[:, :, :, :], 0.0), writes=[kdZ])
            DBG.update({k_: v_.name for k_, v_ in list(locals().items()) if isinstance(v_, Buf)})

            def _chunk(ci):
                b_i, c_i = divmod(ci, NCH)
                tok0 = ci * 128
                xs = xs2[ci % 2]; hnT = hnT2[ci % 2]; yrw = yrw2[ci % 2]; posi = posi2[ci % 2]
                S.dma('sp', lambda e, hnT=hnT, ci=ci: e.dma_start(out=hnT[:, :, :], in_=hnT_s[ci].rearrange("p (a b) -> p a b", a=8)),
                      'hB%d' % (ci % 2), writes=[hnT])
                S.dma('sp', lambda e, yrw=yrw, ci=ci: e.dma_start(out=yrw[:, :, :], in_=yrw_s[ci].rearrange("p (a b) -> p a b", a=4)),
                      'yB%d' % (ci % 2), writes=[yrw])
                S.dma('sp', lambda e, posi=posi, tok0=tok0: e.dma_start(out=posi[:, :], in_=pos_d[0:1, tok0:tok0 + 128].partition_broadcast(128)),
                      'pB%d' % (ci % 2), writes=[posi])
                S.dma('sp', lambda e, xs=xs, tok0=tok0: e.dma_start(out=xs[:, :], in_=x_d[tok0:tok0 + 128, :]), 'xB%d' % (ci % 2), writes=[xs])
                if c_i == 0:
                    S.op('pool', lambda e: e.memset(Rf[:, :, :], 0.0), writes=[Rf])
                    S.op('pool', lambda e: e.memset(Rb[:, :, :], 0.0), writes=[Rb])
                S.op('dve', lambda e, posi=posi: e.tensor_copy(out=posf[:, :], in_=posi[:, :]), reads=[posi], writes=[posf])
                S.op('dve', lambda e: e.tensor_scalar(out=u0[:, :], in0=posf[:, :], scalar1=cst[:, INV:INV + 1], scalar2=None, op0=ALU.mult),
                     reads=[posf, cst], writes=[u0])
                S.op('dve', lambda e: e.tensor_copy(out=ti[:, :], in_=u0[:, :]), reads=[u0], writes=[ti])
                S.op('dve', lambda e: e.tensor_copy(out=tf[:, :], in_=ti[:, :]), reads=[ti], writes=[tf])
                S.op('dve', lambda e: e.tensor_tensor(out=tf[:, :], in0=u0[:, :], in1=tf[:, :], op=ALU.subtract), reads=[u0, tf], writes=[tf])
                S.op('act', lambda e: e.activation(out=sinT[:, :], in_=tf[:, :], func=AF.Sin, scale=cst[:, SSC:SSC + 1]), reads=[tf, cst], writes=[sinT])
                S.op('pool', lambda e: e.tensor_scalar(out=u1[:, :], in0=u0[:, :], scalar1=0.25, scalar2=None, op0=ALU.add), reads=[u0], writes=[u1])
                S.op('dve', lambda e: e.tensor_copy(out=ti[:, :], in_=u1[:, :]), reads=[u1], writes=[ti])
                S.op('dve', lambda e: e.tensor_copy(out=tf[:, :], in_=ti[:, :]), reads=[ti], writes=[tf])
                S.op('dve', lambda e: e.tensor_tensor(out=tf[:, :], in0=u1[:, :], in1=tf[:, :], op=ALU.subtract), reads=[u1, tf], writes=[tf])
                S.op('act', lambda e: e.activation(out=cosT[:, :], in_=tf[:, :], func=AF.Sin, scale=2.0 * math.pi), reads=[tf], writes=[cosT])
                for g in range(2):
                    bk = nb()
                    for jj in range(4):
                        j = g * 4 + jj
                        for kc in range(8):
                            S.op('pe', lambda e, bk=bk, jj=jj, j=j, kc=kc, hnT=hnT: e.matmul(
                                bk[:, jj * 128:(jj + 1) * 128], lhsT=w_b[:, kc, j * 128:(j + 1) * 128], rhs=hnT[:, kc, :],
                                start=(kc == 0), stop=(kc == 7)), reads=[w_b, hnT], writes=[bk])
                    S.op('act', lambda e, bk=bk, g=g: e.copy(out=qk[:, g * 4:g * 4 + 4, :], in_=v3(bk, 4)), reads=[bk], writes=[qk])
                S.op('pool', lambda e: e.tensor_copy(out=qkb[:, :, :], in_=qk[:, :, :]), reads=[qk], writes=[qkb])
                for grp in range(4):
                    for half in range(2):
                        bk = nb(); c0 = 1024 + grp * 1024 + half * 512
                        for kc in range(8):
                            S.op('pe', lambda e, bk=bk, kc=kc, c0=c0, hnT=hnT: e.matmul(bk[:, :], lhsT=hnT[:, kc, :], rhs=w_b[:, kc, c0:c0 + 512],
                                                                                        start=(kc == 0), stop=(kc == 7)), reads=[w_b, hnT], writes=[bk])
                        dst = (v_bf, sgb, sA, sB)[grp]
                        fn = (AF.Copy, AF.Silu, AF.Sigmoid, AF.Sigmoid)[grp]
                        S.op('act', lambda e, bk=bk, dst=dst, fn=fn, half=half: e.activation(out=dst[:, half * 512:(half + 1) * 512], in_=bk[:, :], func=fn),
                             reads=[bk], writes=[dst])
                bsw = [nb(), nb()]
                for j in range(8):
                    S.op('pe', lambda e, j=j: e.matmul(bsw[j // 4][:, (j % 4) * 128:(j % 4 + 1) * 128], lhsT=pswap, rhs=qkb[:, j, :], start=True, stop=True),
                         reads=[tabb, qkb], writes=[bsw[j // 4]])
                S.op('pool', lambda e: e.tensor_tensor(out=r1[:, :, :], in0=qk[:, :, :], in1=cosT[:, :].unsqueeze(1).to_broadcast([128, 8, 128]), op=ALU.mult),
                     reads=[qk, cosT], writes=[r1])
                for g in range(2):
                    S.op('dve', lambda e, g=g: e.tensor_tensor(out=r2[:, g * 4:g * 4 + 4, :], in0=v3(bsw[g], 4),
                                                               in1=sinT[:, :].unsqueeze(1).to_broadcast([128, 4, 128]), op=ALU.mult),
                         reads=[bsw[g], sinT], writes=[r2])
                S.op('dve', lambda e: e.tensor_tensor(out=rot[:, :, :], in0=r1[:, :, :], in1=r2[:, :, :], op=ALU.add), reads=[r1, r2], writes=[rot])
                S.op('pool', lambda e: e.tensor_tensor(out=qd[:, :, :], in0=rot[:, 0:4, :], in1=cst[:, QD:QD + 512].rearrange("p (a b) -> p a b", a=4), op=ALU.mult),
                     reads=[rot, cst], writes=[qd])
                bk = nb()
                for jc in range(4):
                    S.op('pe', lambda e, bk=bk, jc=jc: e.transpose(out=vbf(bk)[:, jc * 128:(jc + 1) * 128], in_=rot[:, 4 + jc, :], identity=ident),
                         reads=[rot, tabb], writes=[bk])
                bkv = vbf(bk)[:, 0:512].rearrange("p (a h c) -> p a h c", a=4, h=2)
                kdv = cst[:, KDEC:KDEC + 8].rearrange("p (a h) -> p a h", a=4)
                for hp in range(2):
                    S.op('dve', lambda e, hp=hp, bkv=bkv: e.tensor_tensor(out=kdZ[:, :, hp, hp * 64:(hp + 1) * 64], in0=bkv[:, :, hp, :],
                                                                          in1=kdv[:, :, hp:hp + 1].to_broadcast([128, 4, 64]), op=ALU.mult),
                         reads=[bk, cst], writes=[kdZ])
                for hp in range(2):
                    bk = nb(); pb = 64 * hp
                    for jc in range(4):
                        S.op('pe', lambda e, bk=bk, jc=jc, pb=pb: e.matmul(bk[:, jc * 128:(jc + 1) * 128], lhsT=rot[pb:pb + 64, 4 + jc, :],
                                                                           rhs=rot[pb:pb + 64, jc, :], start=True, stop=True), reads=[rot], writes=[bk])
                    S.op('dve', lambda e, bk=bk, hp=hp: e.tensor_tensor(
                        out=sT[:, :, :].rearrange("p (a h) c -> p a h c", h=2)[:, :, hp, :], in0=v3(bk, 4),
                        in1=tabf[:, T_MT:T_MT + 1024].rearrange("p (a h c) -> p a h c", a=4, h=2)[:, :, hp, :], op=ALU.mult),
                         reads=[bk, tabf], writes=[sT])
                bo = [nb(), nb()]
                for h in range(8):
                    jc = h // 2; pb = 64 * (h % 2); bk = bo[h // 4]; cc = (h % 4) * 128
                    S.op('pe', lambda e, bk=bk, cc=cc, h=h: e.matmul(bk[:, cc:cc + 128], lhsT=sT[:, h, :], rhs=v_bf[:, h * 128:(h + 1) * 128],
                                                                     start=True, stop=False), reads=[sT, v_bf], writes=[bk])
                    S.op('pe', lambda e, bk=bk, cc=cc, jc=jc, pb=pb: e.matmul(bk[:, cc:cc + 128], lhsT=qd[pb:pb + 64, jc, :], rhs=Rb[pb:pb + 64, jc, :],
                                                                              start=False, stop=True), reads=[qd, Rb], writes=[bk])
                for g in range(2):
                    S.op('act', lambda e, g=g: e.copy(out=of[:, g * 4:g * 4 + 4, :], in_=v3(bo[g], 4)), reads=[bo[g]], writes=[of])
                bR = nb()
                for jc in range(4):
                    for hp in range(2):
                        h = 2 * jc + hp
                        S.op('pe', lambda e, jc=jc, hp=hp, h=h: e.matmul(bR[:, jc * 128:(jc + 1) * 128], lhsT=kdZ[:, jc, hp, :], rhs=v_bf[:, h * 128:(h + 1) * 128],
                                                                         start=(hp == 0), stop=(hp == 1)), reads=[kdZ, v_bf], writes=[bR])
                S.op('pool', lambda e: e.tensor_tensor(out=Rf[:, :, :], in0=Rf[:, :, :], in1=cb(CD, 4, [128, 4, 128]), op=ALU.mult), reads=[Rf, cst], writes=[Rf])
                S.op('dve', lambda e: e.tensor_tensor(out=Rf[:, :, :], in0=Rf[:, :, :], in1=v3(bR, 4), op=ALU.add), reads=[Rf, bR], writes=[Rf])
                S.op('act', lambda e: e.copy(out=Rb[:, :, :], in_=Rf[:, :, :]), reads=[Rf], writes=[Rb])
                S.op('dve', lambda e: e.tensor_reduce(out=stt[:, 0:8], in_=of[:, :, :], axis=AX.X, op=ALU.add), reads=[of], writes=[stt])
                S.op('pool', lambda e: e.tensor_tensor(out=osq[:, :, :], in0=of[:, :, :], in1=of[:, :, :], op=ALU.mult), reads=[of], writes=[osq])
                S.op('dve', lambda e: e.tensor_reduce(out=stt[:, 8:16], in_=osq[:, :, :], axis=AX.X, op=ALU.add), reads=[osq], writes=[stt])
                S.op('dve', lambda e: e.tensor_scalar(out=mean[:, :], in0=stt[:, 0:8], scalar1=1.0 / 128, scalar2=None, op0=ALU.mult), reads=[stt], writes=[mean])
                S.op('dve', lambda e: e.tensor_tensor(out=var[:, :], in0=mean[:, :], in1=mean[:, :], op=ALU.mult), reads=[mean], writes=[var])
                S.op('dve', lambda e: e.scalar_tensor_tensor(out=var[:, :], in0=stt[:, 8:16], scalar=1.0 / 128, in1=var[:, :], op0=ALU.mult, op1=ALU.subtract),
                     reads=[stt, var], writes=[var])
                S.op('act', lambda e: e.activation(out=var[:, :], in_=var[:, :], func=AF.Sqrt, bias=1e-5), reads=[var], writes=[var])
                S.op('dve', lambda e: e.reciprocal(out=var[:, :], in_=var[:, :]), reads=[var], writes=[var])
                S.op('dve', lambda e: e.tensor_tensor(out=of[:, :, :], in0=of[:, :, :], in1=mean[:, :].unsqueeze(2).to_broadcast([128, 8, 128]), op=ALU.subtract),
                     reads=[of, mean], writes=[of])
                S.op('pool', lambda e: e.tensor_tensor(out=of[:, :, :], in0=of[:, :, :], in1=var[:, :].unsqueeze(2).to_broadcast([128, 8, 128]), op=ALU.mult),
                     reads=[of, var], writes=[of])
                S.op('dve', lambda e: e.tensor_tensor(out=yret[:, :], in0=of[:, :, :].rearrange("p a b -> p (a b)"), in1=sgb[:, :], op=ALU.mult),
                     reads=[of, sgb], writes=[yret])
                bk = nb()
                for kc in range(8):
                    S.op('pe', lambda e, bk=bk, kc=kc: e.transpose(out=vbf(bk)[:, kc * 128:(kc + 1) * 128], in_=yret[:, kc * 128:(kc + 1) * 128], identity=ident),
                         reads=[yret, tabb], writes=[bk])
                S.op('act', lambda e, bk=bk: e.copy(out=yretT[:, :, :], in_=vbf(bk).rearrange("p (a b) -> p a b", a=8)), reads=[bk], writes=[yretT])
                for half in range(2):
                    b1 = nb(); b2 = nb(); hs = slice(half * 512, (half + 1) * 512)
                    for jc in range(4):
                        S.op('pe', lambda e, b1=b1, jc=jc, hs=hs, yrw=yrw: e.matmul(b1[:, :], lhsT=yrw[:, jc, :], rhs=wbrw[:, jc, hs], start=(jc == 0), stop=(jc == 3)),
                             reads=[yrw, wbrw], writes=[b1])
                    for kc in range(8):
                        S.op('pe', lambda e, b2=b2, kc=kc, hs=hs: e.matmul(b2[:, :], lhsT=yretT[:, kc, :], rhs=wbret[:, kc, hs], start=(kc == 0), stop=(kc == 7)),
                             reads=[yretT, wbret], writes=[b2])
                    S.op('dve', lambda e, b1=b1, hs=hs: e.tensor_tensor(out=m1[:, hs], in0=b1[:, :], in1=sA[:, hs], op=ALU.mult), reads=[b1, sA], writes=[m1])
                    S.op('dve', lambda e, b2=b2, hs=hs: e.tensor_tensor(out=m2[:, hs], in0=b2[:, :], in1=sB[:, hs], op=ALU.mult), reads=[b2, sB], writes=[m2])
                S.op('pool', lambda e: e.tensor_tensor(out=mg[:, :], in0=m1[:, :], in1=m2[:, :], op=ALU.add), reads=[m1, m2], writes=[mg])
                bk = nb()
                for kc in range(8):
                    S.op('pe', lambda e, bk=bk, kc=kc: e.transpose(out=vbf(bk)[:, kc * 128:(kc + 1) * 128], in_=mg[:, kc * 128:(kc + 1) * 128], identity=ident),
                         reads=[mg, tabb], writes=[bk])
                S.op('act', lambda e, bk=bk: e.copy(out=mT[:, :, :], in_=vbf(bk).rearrange("p (a b) -> p a b", a=8)), reads=[bk], writes=[mT])
                for half in range(2):
                    bk = nb(); hs = slice(half * 512, (half + 1) * 512)
                    for kc in range(8):
                        S.op('pe', lambda e, bk=bk, kc=kc, hs=hs: e.matmul(bk[:, :], lhsT=mT[:, kc, :], rhs=wout[:, kc, hs], start=(kc == 0), stop=(kc == 7)),
                             reads=[mT, wout], writes=[bk])
                    S.op('act', lambda e, bk=bk, hs=hs: e.copy(out=mo[:, hs], in_=bk[:, :]), reads=[bk], writes=[mo])
                S.op('act', lambda e: e.activation(out=junk[:, :], in_=mo[:, :], func=AF.Square, accum_out=ss[:, 0:1]), reads=[mo], writes=[junk, ss])
                S.op('act', lambda e: e.activation(out=rs[:, 0:1], in_=ss[:, 0:1], func=AF.Sqrt, scale=1.0 / D, bias=1e-6), reads=[ss], writes=[rs])
                S.op('dve', lambda e: e.reciprocal(out=rs[:, 0:1], in_=rs[:, 0:1]), reads=[rs], writes=[rs])
                S.op('dve', lambda e: e.scalar_tensor_tensor(out=hh[:, :], in0=mo[:, :], scalar=rs[:, 0:1], in1=rowsB[:, :], op0=ALU.mult, op1=ALU.mult),
                     reads=[mo, rs, rowsB], writes=[hh])
                S.op('pool', lambda e, xs=xs: e.tensor_tensor(out=hh[:, :], in0=hh[:, :], in1=xs[:, :], op=ALU.add), reads=[hh, xs], writes=[hh])
                S.dma('pool', lambda e, tok0=tok0: e.dma_start(out=h_s[tok0:tok0 + 128, :], in_=hh[:, :]), 'hstore', reads=[hh])
            for ci in range(NCHUNK):
                _chunk(ci)
            S.barrier()

        stAB.close()
        if "C" in phases:
          with ExitStack() as st:
            wup = S.sb(st, [128, 8, 2 * DFF], BF16); wdn = S.sb(st, [128, 22, D], BF16)
            identC = S.sb(st, [128, 128], BF16); rowsC = S.sb(st, [128, D], F32)
            S.dma('pool', lambda e: e.dma_start(out=identC[:, :], in_=tab_d[:, T_ID:T_ID + 128]), 'wC', writes=[identC])
            S.dma('sp', lambda e: e.dma_start(out=rowsC[:, :], in_=rows_d[1:2, :].partition_broadcast(128)), 'rowsC', writes=[rowsC])
            for kc in range(8):
                S.dma('pool', lambda e, kc=kc: e.dma_start(out=wup[:, kc, :], in_=wup_d[kc * 128:(kc + 1) * 128, :]), 'wC', writes=[wup])
            for j in range(22):
                S.dma('pool', lambda e, j=j: e.dma_start(out=wdn[:, j, :], in_=wdn_d[j * 128:(j + 1) * 128, :]), 'wC', writes=[wdn])
            hb = [S.sb(st, [128, D], F32) for _ in range(2)]
            xnC2 = [S.sb(st, [128, D], BF16) for _ in range(2)]; jkC = S.sb(st, [128, D], BF16)
            ssC2 = [S.sb(st, [128, 1], F32) for _ in range(2)]; rsC2 = [S.sb(st, [128, 1], F32) for _ in range(2)]
            xnC = xnC2[0]; ssC = ssC2[0]; rsC = rsC2[0]
            hn2T = S.sb(st, [128, 8, FT], BF16)
            actT = S.sb(st, [128, 22, FT], BF16)
            ub2 = [S.sb(st, [128, FT + 2], F32) for _ in range(4)]
            acc2 = [S.sb(st, [128, FT], F32) for _ in range(4)]
            gg2 = [S.sb(st, [128, FT], BF16) for _ in range(2)]
            carry = S.sb(st, [128, 44, 2], F32)
            fo = S.sb(st, [128, D], F32)

            def _tile(ti_):
                b_i, t_i = divmod(ti_, NTILE)
                tokb = ti_ * FT
                if t_i == 0:
                    S.op('pool', lambda e: e.memset(carry[:, :, :], 0.0), writes=[carry])
                for sc in range(NSUB):
                    hbs = hb[sc % 2]
                    S.dma('sp', lambda e, sc=sc, hbs=hbs: e.dma_start(out=hbs[:, :], in_=h_s[tokb + sc * 128:tokb + (sc + 1) * 128, :]),
                          'hC%d' % (sc % 2), writes=[hbs])
                    rmsnorm_T(st, hbs, hn2T, sc * 128, G3, jkC, ssC2[sc % 2], rsC2[sc % 2], xnC2[sc % 2], idn=(identC[:, :], identC))

                def _finish(jp):
                    ag = acc2[(2 * jp) % 4]; av = acc2[(2 * jp + 1) % 4]; gg = gg2[jp % 2]
                    S.op('act', lambda e: e.activation(out=gg[:, :], in_=ag[:, :], func=AF.Gelu_apprx_tanh), reads=[ag], writes=[gg])
                    S.op('pool', lambda e: e.tensor_tensor(out=actT[:, jp, :], in0=gg[:, :], in1=av[:, :], op=ALU.mult),
                         reads=[gg, av], writes=[actT])

                for jp in range(22):
                    for which in range(2):
                        j = jp + 22 * which
                        bk = nb(); ub = ub2[(2 * jp + which) % 4]; acc = acc2[(2 * jp + which) % 4]
                        for kc in range(8):
                            S.op('pe', lambda e, bk=bk, kc=kc, j=j: e.matmul(bk[:, 0:FT], lhsT=wup[:, kc, j * 128:(j + 1) * 128], rhs=hn2T[:, kc, :],
                                                                             start=(kc == 0), stop=(kc == 7)), reads=[wup, hn2T], writes=[bk])
                        S.op('act', lambda e, bk=bk, ub=ub: e.copy(out=ub[:, 2:FT + 2], in_=bk[:, 0:FT]), reads=[bk], writes=[ub])
                        S.op('pool', lambda e, ub=ub, j=j: e.tensor_copy(out=ub[:, 0:2], in_=carry[:, j, :]), reads=[carry], writes=[ub])
                        S.op('act', lambda e, bk=bk, acc=acc, j=j: e.activation(out=acc[:, :], in_=bk[:, 0:FT], func=AF.Identity,
                                                                                 scale=cst[:, CW + 2 * 44 + j:CW + 2 * 44 + j + 1], bias=cst[:, CB + j:CB + j + 1]),
                             reads=[bk, cst], writes=[acc])
                        S.op('dve', lambda e, ub=ub, acc=acc, j=j: e.scalar_tensor_tensor(out=acc[:, :], in0=ub[:, 1:FT + 1], scalar=cst[:, CW + 44 + j:CW + 44 + j + 1],
                                                                                         in1=acc[:, :], op0=ALU.mult, op1=ALU.add), reads=[ub, acc, cst], writes=[acc])
                        S.op('dve', lambda e, ub=ub, acc=acc, j=j: e.scalar_tensor_tensor(out=acc[:, :], in0=ub[:, 0:FT], scalar=cst[:, CW + j:CW + j + 1],
                                                                                         in1=acc[:, :], op0=ALU.mult, op1=ALU.add), reads=[ub, acc, cst], writes=[acc])
                        S.op('pool', lambda e, ub=ub, j=j: e.tensor_copy(out=carry[:, j, :], in_=ub[:, FT:FT + 2]), reads=[ub], writes=[carry])
                    if jp >= 1:
                        _finish(jp - 1)
                _finish(21)
                for sc in range(NSUB):
                    hbs = hb[sc % 2]
                    S.dma('sp', lambda e, sc=sc, hbs=hbs: e.dma_start(out=hbs[:, :], in_=h_s[tokb + sc * 128:tokb + (sc + 1) * 128, :]),
                          'hC%d' % (sc % 2), writes=[hbs])
                    for half in range(2):
                        bk = nb(); hs = slice(half * 512, (half + 1) * 512)
                        for j in range(22):
                            S.op('pe', lambda e, bk=bk, j=j, sc=sc, hs=hs: e.matmul(bk[:, :], lhsT=actT[:, j, sc * 128:(sc + 1) * 128], rhs=wdn[:, j, hs],
                                                                                    start=(j == 0), stop=(j == 21)), reads=[actT, wdn], writes=[bk])
                        S.op('act', lambda e, bk=bk, hs=hs: e.copy(out=fo[:, hs], in_=bk[:, :]), reads=[bk], writes=[fo])
                    S.op('act', lambda e: e.activation(out=jkC[:, :], in_=fo[:, :], func=AF.Square, accum_out=ssC[:, 0:1]), reads=[fo], writes=[jkC, ssC])
                    S.op('act', lambda e: e.activation(out=rsC[:, 0:1], in_=ssC[:, 0:1], func=AF.Sqrt, scale=1.0 / D, bias=1e-6), reads=[ssC], writes=[rsC])
                    S.op('dve', lambda e: e.reciprocal(out=rsC[:, 0:1], in_=rsC[:, 0:1]), reads=[rsC], writes=[rsC])
                    S.op('dve', lambda e: e.scalar_tensor_tensor(out=fo[:, :], in0=fo[:, :], scalar=rsC[:, 0:1], in1=rowsC[:, :], op0=ALU.mult, op1=ALU.mult),
                         reads=[fo, rsC, rowsC], writes=[fo])
                    S.op('dve', lambda e, hbs=hbs: e.tensor_tensor(out=fo[:, :], in0=fo[:, :], in1=hbs[:, :], op=ALU.add), reads=[fo, hbs], writes=[fo])
                    S.dma('pool', lambda e, sc=sc: e.dma_start(out=out_d[tokb + sc * 128:tokb + (sc + 1) * 128, :], in_=fo[:, :]), 'ostore', reads=[fo])
            for ti_ in range(NSEQ * NTILE):
                _tile(ti_)
            S.barrier()
        S.barrier()
        S.emit()
    return nc


def host_consts(inp):
    f = np.float32
    c = np.zeros((128, NCONST), f)

    def pk(v, n):
        return np.asarray(v, f).reshape(n, 128).T
    c[:, G1:G1 + 8] = pk(inp["norm_mix_pre"][0], 8)
    c[:, MU:MU + 14] = pk(inp["rw_mu"][0], 14)
    c[:, W0:W0 + 4] = pk(inp["rw_w0"][0], 4)
    c[:, A0:A0 + 4] = pk(inp["rw_a0"][0], 4)
    c[:, KK:KK + 4] = pk(inp["rw_k_k"][0], 4)
    c[:, KA:KA + 4] = pk(inp["rw_k_a"][0], 4)
    c[:, RK:RK + 4] = pk(inp["rw_r_k"][0], 4)
    c[:, LW:LW + 4] = pk(inp["rw_lnx_w"][0], 4)
    c[:, LB:LB + 4] = pk(inp["rw_lnx_b"][0], 4)
    c[:, G3:G3 + 8] = pk(inp["norm_ffn_pre"][0], 8)
    c[:, CB:CB + 44] = pk(inp["ffn_conv_b"][0], 44)
    for tap in range(3):
        c[:, CW + tap * 44:CW + (tap + 1) * 44] = pk(inp["ffn_conv_w"][0, tap], 44)
    p = np.arange(128)
    inv = (10000.0 ** (-(np.arange(32, dtype=np.float32)) / np.float32(32))).astype(f)
    c[:, INV] = inv[p % 32] / f(2 * math.pi)
    c[:, SSC] = np.where((p % 64) < 32, -2 * math.pi, 2 * math.pi).astype(f)
    lg = np.log1p(-np.exp2(-5.0 - np.arange(8, dtype=np.float64)))
    for jc in range(4):
        hsel = 2 * jc + p // 64
        c[:, CD + jc] = np.exp(128 * lg[hsel])
        c[:, QD + jc * 128:QD + (jc + 1) * 128] = np.exp((np.arange(128)[None, :] + 1.0) * lg[hsel][:, None])
    for h in range(8):
        c[:, KDEC + h] = 0.125 * np.exp((127.0 - p) * lg[h])
    tab = np.zeros((128, NTAB), f)
    s = np.arange(128)[:, None]; t = np.arange(128)[None, :]
    strict = (t > s).astype(f); incl = (t >= s).astype(f)
    tab[:, T_MP:T_MP + 512] = np.concatenate([strict, incl, strict, incl], axis=1)
    tab[:, T_M0:T_M0 + 128] = (t < s).astype(f)
    for h in range(8):
        tab[:, T_MT + h * 128:T_MT + (h + 1) * 128] = np.where(t >= s, 0.125 * np.exp(np.maximum(t - s, 0) * lg[h]), 0.0)
    tab[:, T_BM:T_BM + 128] = ((s // 64) == (t // 64)).astype(f)
    tab[:, T_ID:T_ID + 128] = np.eye(128, dtype=f)
    tab[:, T_SW:T_SW + 128] = (t == (s ^ 32)).astype(f)
    lr = np.concatenate([np.concatenate([inp["rw_w2"][0], inp["rw_a2"][0]], axis=0), inp["rw_g2"][0]], axis=1).astype(f)
    rows = np.stack([inp["norm_mix_post"][0], inp["norm_ffn_post"][0]], axis=0).astype(f)
    return c, tab, lr, rows


_NC_CACHE = {}
STOP = [None]


def _stop(n):
    return STOP[0] is not None and n >= STOP[0]

DBG = {}


def run(inp, NSEQ, NCH, ncores, dbg=False, phases="ABC"):
    key = (NSEQ, NCH, dbg, phases, STOP[0])
    if key not in _NC_CACHE:
        _NC_CACHE[key] = build(NSEQ, NCH, dbg, phases)
    nc = _NC_CACHE[key]
    c, tab, lr, rows = host_consts(inp)
    T = NCH * 128
    shared = {
        "w_in": np.ascontiguousarray(inp["w_in"][0]), "wbrw": np.ascontiguousarray(inp["w_branch_rw"][0]),
        "wbret": np.ascontiguousarray(inp["w_branch_ret"][0]), "wout": np.ascontiguousarray(inp["w_out"][0]),
        "wup": np.ascontiguousarray(inp["ffn_w_up"][0]), "wdn": np.ascontiguousarray(inp["ffn_w_down"][0]),
        "lr": lr, "c128": c, "rows": rows, "tab": tab,
    }
    in_maps = []
    for i in range(ncores):
        xs = np.ascontiguousarray(inp["x"][i * NSEQ:(i + 1) * NSEQ, :T, :]).reshape(NSEQ * T, D)
        ps = np.ascontiguousarray(inp["positions"][i * NSEQ:(i + 1) * NSEQ, :T]).reshape(1, NSEQ * T).astype(np.int32)
        m = dict(shared); m["x"] = xs; m["pos"] = ps
        in_maps.append(m)
    res = run_bass_kernel_spmd(nc, in_maps, core_ids=list(range(ncores)))
    return res


def kernel(**inputs):
    inp = {k: np.asarray(v) for k, v in inputs.items()}
    B, T, _ = inp["x"].shape
    ncores = 8
    NSEQ = B // ncores
    res = run(inp, NSEQ, T // 128, ncores)
    out = np.concatenate([r["out"].reshape(NSEQ, T, D) for r in res.results], axis=0)
    return out.astype(np.float32)
```
